# Optimizing a Trainium2 kernel written in Bass

```python
import math
import jax
import jax.numpy as jnp
from jax import lax
import numpy as np

D_MODEL = 2048
BATCH = 16
SEQ = 256
DEPTH = 2
DEC_BATCH = 4
DEC_SEQ = 1024
PAST_LEN = 512

GRID_W = 64
SSD_D = D_MODEL // 2
SSD_P = 64
SSD_H = SSD_D // SSD_P
SSD_G = 2
SSD_N = 128
SSD_CHUNK = 128
GLA_H = 4
GLA_VW = D_MODEL // 4
GLA_DV = GLA_VW // GLA_H
GLA_DK = GLA_DV // 2
GLA_KW = GLA_H * GLA_DK
GLA_RANK = 16
GLA_TAU = 16.0
GLA_CHUNK = 64
LRU_W = D_MODEL // 4
LRU_NB = 8
LRU_BW = LRU_W // LRU_NB
LRU_C = 8.0
CONV_K = 4
CONV_PAD_L = 1
CONV_PAD_R = CONV_K - 1 - CONV_PAD_L
D_MIX = SSD_D + GLA_VW + LRU_W
SSD_CONV_CH = SSD_D + 2 * SSD_G * SSD_N
IN_SPLITS = (SSD_D, SSD_CONV_CH, SSD_H, GLA_KW, GLA_KW, GLA_VW, GLA_VW, GLA_RANK, LRU_W, LRU_W)
IN_COLS = sum(IN_SPLITS)
FF_DENSE = ((8 * D_MODEL // 3 + 127) // 128) * 128
N_EXPERTS = 8
TOP_K = 2
FF_EXPERT = 7 * D_MODEL // 2
EPS = 1e-6
F32 = jnp.float32

kernel_name = 'hybrid_ssd_gla_rglru_diffusion_step'


def _split_cols(x, sizes):
    out, start = [], 0
    for s in sizes:
        out.append(x[..., start:start + s])
        start += s
    return out


def rmsnorm(x, g):
    xf = x.astype(F32)
    y = xf * lax.rsqrt(jnp.mean(xf * xf, axis=-1, keepdims=True) + EPS)
    return (y * g.astype(F32)).astype(x.dtype)


def adaln(cvec, w, b):
    m = jax.nn.silu(cvec.astype(F32)) @ w.astype(F32) + b.astype(F32)
    return jnp.split(m[..., None, :], 6, axis=-1)


def centred_conv(x, w, b, grid_rows):
    bsz, l, ch = x.shape
    xs = x if grid_rows is None else x.reshape(bsz, grid_rows, GRID_W, ch)
    ax = xs.ndim - 2
    n = xs.shape[ax]
    pad = [(0, 0)] * xs.ndim
    pad[ax] = (CONV_PAD_L, CONV_PAD_R)
    xp = jnp.pad(xs, pad)
    w = w.astype(F32)
    y = b.astype(F32) + sum(w[k] * lax.slice_in_dim(xp, k, k + n, axis=ax) for k in range(CONV_K))
    return y.reshape(bsz, l, ch)


def segsum(a):
    cs = jnp.cumsum(a, axis=-1)
    t = a.shape[-1]
    diff = cs[..., :, None] - cs[..., None, :]
    return jnp.where(jnp.tril(jnp.ones((t, t), dtype=bool)), diff, -jnp.inf)


def ssd_chunked(x, a, bm, cm, h0):
    bsz, l, nh, hp = x.shape
    ng, ns = bm.shape[2], bm.shape[3]
    ne = nh // ng
    nc = l // SSD_CHUNK
    x = x.reshape(bsz, nc, SSD_CHUNK, ng, ne, hp)
    bm = bm.reshape(bsz, nc, SSD_CHUNK, ng, ns)
    cm = cm.reshape(bsz, nc, SSD_CHUNK, ng, ns)
    a = jnp.moveaxis(a.reshape(bsz, nc, SSD_CHUNK, ng, ne), 2, -1)
    a_cum = jnp.cumsum(a, axis=-1)
    w = jnp.einsum('bctgn,bcsgn->bcgts', cm, bm)[:, :, :, None] * jnp.exp(segsum(a))
    y = jnp.einsum('bcgets,bcsgep->bctgep', w, x)
    decay_s = jnp.exp(a_cum[..., -1:] - a_cum)
    states = jnp.einsum('bcsgn,bcges,bcsgep->bcgepn', bm, decay_s, x)
    states = jnp.concatenate([h0.reshape(bsz, 1, ng, ne, hp, ns), states], axis=1)
    a_chunk = jnp.pad(jnp.moveaxis(a_cum[..., -1], 1, -1), [(0, 0)] * 3 + [(1, 0)])
    new_states = jnp.einsum('bgezc,bcgepn->bzgepn', jnp.exp(segsum(a_chunk)), states)
    y = y + jnp.einsum('bctgn,bcgepn,bcget->bctgep', cm, new_states[:, :-1], jnp.exp(a_cum))
    return y.reshape(bsz, l, nh, hp), new_states[:, -1].reshape(bsz, nh, hp, ns)


def ssd_mixer(z, xbc, dt_raw, p, h0, grid_rows):
    bsz, l, _ = z.shape
    xbc = jax.nn.silu(centred_conv(xbc, p['ssd_conv_w'], p['ssd_conv_b'], grid_rows))
    xs, bm, cm = _split_cols(xbc, (SSD_D, SSD_G * SSD_N, SSD_G * SSD_N))
    xs = xs.reshape(bsz, l, SSD_H, SSD_P)
    bm = bm.reshape(bsz, l, SSD_G, SSD_N)
    cm = cm.reshape(bsz, l, SSD_G, SSD_N)
    y = p['ssd_D'].astype(F32)[:, None] * xs
    finals = []
    for d in range(2):
        dt = jax.nn.softplus(dt_raw + p['ssd_dt_bias'][d].astype(F32))
        a = -jnp.exp(p['ssd_A_log'][d].astype(F32)) * dt
        args = (xs * dt[..., None], a, bm, cm)
        if d == 1:
            args = tuple(jnp.flip(t, axis=1) for t in args)
        yd, fd = ssd_chunked(*args, h0[:, d])
        y = y + (jnp.flip(yd, axis=1) if d == 1 else yd)
        finals.append(fd)
    y = y.reshape(bsz, l, SSD_D) * jax.nn.silu(z)
    return rmsnorm(y, p['ssd_norm_g']), jnp.stack(finals, axis=1)


def gla_chunked(q, k, v, log_a, s0):
    bsz, l, nh, dk = q.shape
    dv = v.shape[-1]
    nc = l // GLA_CHUNK
    q, k, log_a = (t.reshape(bsz, nc, GLA_CHUNK, nh, dk) for t in (q, k, log_a))
    v = v.reshape(bsz, nc, GLA_CHUNK, nh, dv)
    bc = jnp.cumsum(log_a, axis=2)
    mask = jnp.tril(jnp.ones((GLA_CHUNK, GLA_CHUNK), dtype=bool))[:, :, None, None]
    diff = bc[:, :, :, None] - bc[:, :, None, :]
    decay = jnp.exp(jnp.where(mask, diff, -jnp.inf))
    attn = jnp.einsum('bcthk,bcshk,bctshk->bctsh', q, k, decay)
    o = jnp.einsum('bctsh,bcshv->bcthv', attn, v)
    last = bc[:, :, -1]
    ds = jnp.einsum('bcshk,bcshv->bchkv', k * jnp.exp(last[:, :, None] - bc), v)

    def step(s, inp):
        g_c, ds_c = inp
        return g_c[..., None] * s + ds_c, s

    s_fin, s_prev = lax.scan(step, s0, (jnp.moveaxis(jnp.exp(last), 1, 0), jnp.moveaxis(ds, 1, 0)))
    o = o + jnp.einsum('bcthk,cbhkv->bcthv', q * jnp.exp(bc), s_prev)
    return o.reshape(bsz, l, nh, dv), s_fin


def gla_mixer(q, k, v, g, graw, p, s0):
    bsz, l, _ = q.shape
    q = q.reshape(bsz, l, GLA_H, GLA_DK) * (GLA_DK ** -0.5)
    k = k.reshape(bsz, l, GLA_H, GLA_DK)
    v = v.reshape(bsz, l, GLA_H, GLA_DV)
    o = 0.0
    finals = []
    for d in range(2):
        logit = graw @ p['gla_gate_w'][d].astype(F32) + p['gla_gate_b'][d].astype(F32)
        log_a = jax.nn.log_sigmoid(logit).reshape(bsz, l, GLA_H, GLA_DK) / GLA_TAU
        args = (q, k, v, log_a)
        if d == 1:
            args = tuple(jnp.flip(t, axis=1) for t in args)
        od, fd = gla_chunked(*args, s0[:, d])
        o = o + (jnp.flip(od, axis=1) if d == 1 else od)
        finals.append(fd)
    o = rmsnorm(o, p['gla_norm_g']).reshape(bsz, l, GLA_VW) * jax.nn.silu(g)
    return o, jnp.stack(finals, axis=1)


def _lin_combine(left, right):
    a_l, b_l = left
    a_r, b_r = right
    return a_l * a_r, a_r * b_l + b_r


def linear_scan(a, u, h0):
    u = u.at[:, 0].add(a[:, 0] * h0)
    _, h = lax.associative_scan(_lin_combine, (a, u), axis=1)
    return h, h[:, -1]


def rglru_mixer(xb, gb, p, h0, grid_rows):
    bsz, l, _ = xb.shape
    xr = centred_conv(xb, p['lru_conv_w'], p['lru_conv_b'], grid_rows)
    xblk = xr.reshape(bsz, l, LRU_NB, LRU_BW)
    y = 0.0
    finals = []
    for d in range(2):
        r = jax.nn.sigmoid(jnp.einsum('blni,nij->blnj', xblk, p['lru_wa'][d].astype(F32)).reshape(bsz, l, LRU_W)
                           + p['lru_ba'][d].astype(F32))
        i = jax.nn.sigmoid(jnp.einsum('blni,nij->blnj', xblk, p['lru_wx'][d].astype(F32)).reshape(bsz, l, LRU_W)
                           + p['lru_bx'][d].astype(F32))
        log_a = -LRU_C * r * jax.nn.softplus(-p['lru_lambda'][d].astype(F32))
        a = jnp.exp(log_a)
        u = jnp.sqrt(-jnp.expm1(2.0 * log_a)) * (i * xr)
        if d == 1:
            a, u = jnp.flip(a, axis=1), jnp.flip(u, axis=1)
        hd, fd = linear_scan(a, u, h0[:, d])
        y = y + (jnp.flip(hd, axis=1) if d == 1 else hd)
        finals.append(fd)
    return y * jax.nn.gelu(gb), jnp.stack(finals, axis=1)


def mixer_block(h, p, init, grid_rows):
    proj = (h @ p['in_w']).astype(F32)
    z, xbc, dt_raw, q, k, v, g, graw, xb, gb = _split_cols(proj, IN_SPLITS)
    h_ssd, h_gla, h_lru = (s.astype(F32) for s in init)
    y_ssd, s_ssd = ssd_mixer(z, xbc, dt_raw, p, h_ssd, grid_rows)
    y_gla, s_gla = gla_mixer(q, k, v, g, graw, p, h_gla)
    y_lru, s_lru = rglru_mixer(xb, gb, p, h_lru, grid_rows)
    y = jnp.concatenate([y_ssd, y_gla, y_lru], axis=-1).astype(h.dtype) @ p['out_w']
    return y, (s_ssd, s_gla, s_lru)


def swiglu(x, w1, w3, w2):
    return (jax.nn.silu(x @ w1) * (x @ w3)) @ w2


def moe_swiglu(x, router, w1, w3, w2):
    bsz, l, d = x.shape
    xt = x.reshape(bsz * l, d)
    logits = xt.astype(F32) @ router.astype(F32)
    top_v, top_i = lax.top_k(logits, TOP_K)
    gates = jax.nn.softmax(top_v, axis=-1)
    combine = jnp.einsum('tk,tke->te', gates, jax.nn.one_hot(top_i, N_EXPERTS, dtype=F32))
    out = jnp.zeros((bsz * l, d), F32)
    for e in range(N_EXPERTS):
        out = out + combine[:, e:e + 1] * swiglu(xt, w1[e], w3[e], w2[e]).astype(F32)
    return out.astype(x.dtype).reshape(bsz, l, d)


def trunk_layer(x, mods, p, ffn_w, is_moe, init, grid_rows):
    sh1, sc1, g1, sh2, sc2, g2 = mods
    h = (rmsnorm(x, p['norm1_g']) * (1.0 + sc1) + sh1).astype(x.dtype)
    y, states = mixer_block(h, p, init, grid_rows)
    x = x + (g1 * y).astype(x.dtype)
    h = (rmsnorm(x, p['norm2_g']) * (1.0 + sc2) + sh2).astype(x.dtype)
    f = moe_swiglu(h, *ffn_w) if is_moe else swiglu(h, *ffn_w)
    x = x + (g2 * f).astype(x.dtype)
    return x, states


def setup_inputs(seed: int = 0) -> dict:
    key = jax.random.key(seed)
    ks = list(jax.random.split(key, 48))

    def nrm(shape, scale):
        return scale * jax.random.normal(ks.pop(), shape, F32)

    def unif(shape, lo, hi):
        return jax.random.uniform(ks.pop(), shape, F32, lo, hi)

    d = D_MODEL
    n_even = (DEPTH + 1) // 2
    n_odd = DEPTH // 2
    dt0 = jnp.exp(unif((DEPTH, 2, SSD_H), math.log(1e-3), math.log(1e-1)))
    ssd_dt_bias = dt0 + jnp.log(-jnp.expm1(-dt0))
    a0 = unif((DEPTH, 2, LRU_W), 0.9, 0.999)
    s = a0 ** (1.0 / LRU_C)
    lru_lambda = jnp.log(s) - jnp.log1p(-s)
    return {
        'x_prompt': nrm((BATCH, SEQ, d), 1.0),
        'x_sample': nrm((DEC_BATCH, DEC_SEQ, d), 1.0),
        'state_ssd': nrm((DEC_BATCH, DEPTH, 2, SSD_H, SSD_P, SSD_N), 0.2),
        'state_gla': nrm((DEC_BATCH, DEPTH, 2, GLA_H, GLA_DK, GLA_DV), 1.0),
        'state_lru': nrm((DEC_BATCH, DEPTH, 2, LRU_W), 0.5),
        'c': nrm((DEC_BATCH, d), 1.0),
        'c_ctx': nrm((d,), 1.0),
        'mod_w': nrm((DEPTH, d, 6 * d), 0.5 * d ** -0.5),
        'mod_b': nrm((DEPTH, 6 * d), 0.02),
        'norm1_g': 1.0 + nrm((DEPTH, d), 0.02),
        'norm2_g': 1.0 + nrm((DEPTH, d), 0.02),
        'in_w': nrm((DEPTH, d, IN_COLS), d ** -0.5),
        'ssd_conv_w': nrm((DEPTH, CONV_K, SSD_CONV_CH), CONV_K ** -0.5),
        'ssd_conv_b': nrm((DEPTH, SSD_CONV_CH), 0.02),
        'ssd_A_log': jnp.log(unif((DEPTH, 2, SSD_H), 1.0, 16.0)),
        'ssd_dt_bias': ssd_dt_bias,
        'ssd_D': 1.0 + nrm((DEPTH, SSD_H), 0.1),
        'ssd_norm_g': 1.0 + nrm((DEPTH, SSD_D), 0.02),
        'gla_gate_w': nrm((DEPTH, 2, GLA_RANK, GLA_KW), GLA_RANK ** -0.5),
        'gla_gate_b': nrm((DEPTH, 2, GLA_KW), 0.1),
        'gla_norm_g': 1.0 + nrm((DEPTH, GLA_DV), 0.02),
        'lru_conv_w': nrm((DEPTH, CONV_K, LRU_W), CONV_K ** -0.5),
        'lru_conv_b': nrm((DEPTH, LRU_W), 0.02),
        'lru_wa': nrm((DEPTH, 2, LRU_NB, LRU_BW, LRU_BW), LRU_BW ** -0.5),
        'lru_ba': nrm((DEPTH, 2, LRU_W), 0.02),
        'lru_wx': nrm((DEPTH, 2, LRU_NB, LRU_BW, LRU_BW), LRU_BW ** -0.5),
        'lru_bx': nrm((DEPTH, 2, LRU_W), 0.02),
        'lru_lambda': lru_lambda,
        'out_w': nrm((DEPTH, D_MIX, d), D_MIX ** -0.5),
        'ffn_w1': nrm((n_even, d, FF_DENSE), d ** -0.5),
        'ffn_w3': nrm((n_even, d, FF_DENSE), d ** -0.5),
        'ffn_w2': nrm((n_even, FF_DENSE, d), FF_DENSE ** -0.5),
        'moe_router': nrm((n_odd, d, N_EXPERTS), d ** -0.5),
        'moe_w1': nrm((n_odd, N_EXPERTS, d, FF_EXPERT), d ** -0.5),
        'moe_w3': nrm((n_odd, N_EXPERTS, d, FF_EXPERT), d ** -0.5),
        'moe_w2': nrm((n_odd, N_EXPERTS, FF_EXPERT, d), FF_EXPERT ** -0.5),
        'final_norm_g': 1.0 + nrm((d,), 0.02),
    }


def reference(x_prompt, x_sample, state_ssd, state_gla, state_lru, c, c_ctx,
              mod_w, mod_b, norm1_g, norm2_g, in_w,
              ssd_conv_w, ssd_conv_b, ssd_A_log, ssd_dt_bias, ssd_D, ssd_norm_g,
              gla_gate_w, gla_gate_b, gla_norm_g,
              lru_conv_w, lru_conv_b, lru_wa, lru_ba, lru_wx, lru_bx, lru_lambda,
              out_w, ffn_w1, ffn_w3, ffn_w2, moe_router, moe_w1, moe_w3, moe_w2, final_norm_g):
    grid_rows = x_sample.shape[1] // GRID_W
    bp = x_prompt.shape[0]
    zero_init = (jnp.zeros((bp, 2, SSD_H, SSD_P, SSD_N), F32),
                 jnp.zeros((bp, 2, GLA_H, GLA_DK, GLA_DV), F32),
                 jnp.zeros((bp, 2, LRU_W), F32))
    xp, xs = x_prompt, x_sample
    new_ssd, new_gla, new_lru = [], [], []
    for i in range(DEPTH):
        p = {
            'in_w': in_w[i], 'out_w': out_w[i], 'norm1_g': norm1_g[i], 'norm2_g': norm2_g[i],
            'ssd_conv_w': ssd_conv_w[i], 'ssd_conv_b': ssd_conv_b[i], 'ssd_A_log': ssd_A_log[i],
            'ssd_dt_bias': ssd_dt_bias[i], 'ssd_D': ssd_D[i], 'ssd_norm_g': ssd_norm_g[i],
            'gla_gate_w': gla_gate_w[i], 'gla_gate_b': gla_gate_b[i], 'gla_norm_g': gla_norm_g[i],
            'lru_conv_w': lru_conv_w[i], 'lru_conv_b': lru_conv_b[i], 'lru_wa': lru_wa[i],
            'lru_ba': lru_ba[i], 'lru_wx': lru_wx[i], 'lru_bx': lru_bx[i], 'lru_lambda': lru_lambda[i],
        }
        j = i // 2
        is_moe = (i % 2 == 1)
        if is_moe:
            ffn_w = (moe_router[j], moe_w1[j], moe_w3[j], moe_w2[j])
        else:
            ffn_w = (ffn_w1[j], ffn_w3[j], ffn_w2[j])
        xp, (s_ssd, s_gla, s_lru) = trunk_layer(xp, adaln(c_ctx, mod_w[i], mod_b[i]), p, ffn_w, is_moe,
                                                zero_init, None)
        new_ssd.append(s_ssd)
        new_gla.append(s_gla)
        new_lru.append(s_lru)
        xs, _ = trunk_layer(xs, adaln(c, mod_w[i], mod_b[i]), p, ffn_w, is_moe,
                            (state_ssd[:, i], state_gla[:, i], state_lru[:, i]), grid_rows)
    y_prompt = rmsnorm(xp, final_norm_g)
    y_sample = rmsnorm(xs, final_norm_g)
    return (y_prompt, y_sample, jnp.stack(new_ssd, axis=1), jnp.stack(new_gla, axis=1), jnp.stack(new_lru, axis=1))
```

```python
from contextlib import ExitStack
import numpy as np
import concourse.bass as bass
import concourse.mybir as mybir
from concourse.bass_utils import run_bass_kernel_spmd

F32 = mybir.dt.float32
BF16 = mybir.dt.bfloat16
AF = mybir.ActivationFunctionType
ALU = mybir.AluOpType
AX = mybir.AxisListType

P = 128
T = 1024
D = 2048
DT = 16
NCH = 8
DEPTH = 2
IN_COLS = 5152
FF_DENSE = 5504
FF_EXP = 7168
NEXP = 8
EPS = 1e-6
ND = 48
ARENA0 = 16512
KB = 1024
import os
STOPX = int(os.environ.get('STOPX', '0'))
STOP_AFTER = None
C_Z, C_XBC, C_DT, C_Q, C_K, C_V, C_G, C_GRAW, C_XB, C_GB = 0, 1024, 2560, 2576, 2832, 3088, 3600, 4112, 4128, 4640


class FW:
    def __init__(self, nc):
        self.nc = nc
        self.es = ExitStack()
        self.eng = {'pe': nc.tensor, 'dve': nc.vector, 'act': nc.scalar, 'pool': nc.gpsimd, 'sp': nc.sync}
        self.sem = {k: self.es.enter_context(nc.semaphore("s_" + k)) for k in ['pe', 'dve', 'act', 'pool']}
        self.cnt = {k: 0 for k in self.sem}
        self.dsem = [self.es.enter_context(nc.semaphore("d%d" % i)) for i in range(ND)]
        self.dcnt = [0] * ND
        self.dnext = 0
        self.seen = {e: {} for e in self.eng}
        self.lastw = {}
        self.readers = {}
        self.nbuf = 0

    def ps(self, shape, dtype=F32, name=None):
        self.nbuf += 1
        return self.es.enter_context(self.nc.psum_tensor(name or ("p%d" % self.nbuf), list(shape), dtype))

    def _semh(self, sk):
        return self.sem[sk[1]] if sk[0] == 'e' else self.dsem[sk[1]]

    def _need(self, E, deps):
        for sk, v in deps:
            if sk[0] == 'd':
                v = 16 * self.dcnt[sk[1]]
            if sk == ('e', 'pe') and E == 'pe':
                continue
            if self.seen[E].get(sk, 0) >= v:
                continue
            self.eng[E].wait_ge(self._semh(sk), v)
            self.seen[E][sk] = v

    def _deps(self, reads, writes):
        deps = []
        for r in reads:
            lw = self.lastw.get(r)
            if lw:
                deps.append(lw)
        for w in writes:
            lw = self.lastw.get(w)
            if lw:
                deps.append(lw)
            deps.extend(self.readers.get(w, {}).items())
        return deps

    def _commit(self, key, val, reads, writes):
        for r in reads:
            self.readers.setdefault(r, {})[key] = val
        for w in writes:
            self.lastw[w] = (key, val)
            self.readers[w] = {}

    def op(self, E, fn, reads=(), writes=(), inc=True):
        self._need(E, self._deps(reads, writes))
        ins = fn(self.eng[E])
        val = self.cnt[E] + 1
        if inc:
            ins.then_inc(self.sem[E], 1)
            self.cnt[E] = val
        self._commit(('e', E), val, reads, writes)

    def dma(self, Q, out, in_, reads=(), writes=()):
        self._need(Q, self._deps(reads, writes))
        i = self.dnext
        self.dnext = (i + 1) % ND
        self.eng[Q].dma_start(out=out, in_=in_).then_inc(self.dsem[i], 16)
        self.dcnt[i] += 1
        self._commit(('d', i), 16 * self.dcnt[i], reads, writes)

    def barrier(self):
        for E in self.eng:
            deps = [(('e', k), self.cnt[k]) for k in self.sem if self.cnt[k] and k != E]
            deps += [(('d', i), 16 * self.dcnt[i]) for i in range(ND) if self.dcnt[i]]
            self._need(E, deps)

    def finish(self):
        for i in range(ND):
            if self.dcnt[i]:
                self.nc.sync.wait_ge(self.dsem[i], 16 * self.dcnt[i])
        for k in self.sem:
            if self.cnt[k]:
                self.nc.sync.wait_ge(self.sem[k], self.cnt[k])
        self.es.close()


class Arena:
    uid = 0

    def __init__(self, nc, base, size):
        self.nc, self.base, self.size, self.off = nc, base, size, 0

    def alloc(self, shape, dtype, name):
        n = 1
        for s in shape[1:]:
            n *= s
        nbytes = (n * (4 if dtype == F32 else 2) + 31) // 32 * 32
        assert self.off + nbytes <= self.size, (name, self.off, nbytes, self.size)
        Arena.uid += 1
        t = self.nc.alloc_sbuf_tensor_at("%s_%d" % (name, Arena.uid), list(shape), dtype, offset=self.base + self.off)
        self.off += nbytes
        return t

    def reset(self):
        self.off = 0


def _param_layout():
    off = {}
    n = 0

    def add(name, w):
        nonlocal n
        off[name] = (n, w)
        n += w
    add('cvec', DT)
    add('final_g', DT)
    add('flag', 1)
    for i in range(DEPTH):
        add('modb%d' % i, 96)
        add('n1g%d' % i, DT)
        add('n2g%d' % i, DT)
        add('lru_cw%d' % i, 16)
        add('lru_cb%d' % i, 4)
        for d in range(2):
            add('lru_ba%d%d' % (i, d), 4)
            add('lru_bx%d%d' % (i, d), 4)
            add('lru_lam%d%d' % (i, d), 4)
            add('lru_h0%d%d' % (i, d), 4)
        add('ssd_cw%d' % i, 48)
        add('ssd_cb%d' % i, 12)
        add('ssd_ng%d' % i, 8)
        add('ssd_D%d' % i, 16)
        for d in range(2):
            add('ssd_dtb%d%d' % (i, d), 16)
            add('ssd_alog%d%d' % (i, d), 16)
        add('gla_ng%d' % i, 1)
    return off, n


PAR_OFF, NPAR = _param_layout()


def build_program():
    nc = bass.Bass("TRN2", target_bir_lowering=False)
    fw = FW(nc)

    def din(name, shape, dt=F32):
        return nc.dram_tensor(name, list(shape), dt, kind="ExternalInput").ap()

    def dout(name, shape, dt=F32):
        return nc.dram_tensor(name, list(shape), dt, kind="ExternalOutput").ap()

    xT_in = din("xT", [D, T])
    par_in = din("par", [P, NPAR])
    cst_in = din("cst", [P, 4, P])
    msk_in = din("msk", [P, 3, T], BF16)
    mod_w = din("mod_w", [DEPTH, D, 6 * D])
    in_w = din("in_w", [DEPTH, D, IN_COLS])
    out_w = din("out_w", [DEPTH, D, D])
    w1 = din("ffn_w1", [D, FF_DENSE])
    w3 = din("ffn_w3", [D, FF_DENSE])
    w2 = din("ffn_w2", [FF_DENSE, D])
    rt_in = din("router", [D, NEXP])
    full = STOP_AFTER is None
    mw1 = din("moe_w1", [NEXP, D, FF_EXP]) if full else None
    mw3 = din("moe_w3", [NEXP, D, FF_EXP]) if full else None
    mw2 = din("moe_w2", [NEXP, FF_EXP, D]) if full else None
    gw_in = din("gla_gw", [DEPTH, 2, 32, 256])
    lw_in = din("lru_w", [DEPTH, 2, 2, 4, P, P])
    ssd_h0 = din("ssd_h0", [DEPTH, 2, P, 1024])
    gla_h0 = din("gla_h0", [DEPTH, 2, P, 256])
    yT_out = dout("yT", [D, T])
    o_ssd = dout("o_ssd", [4, DEPTH, 2, 1024, P])
    o_gla = dout("o_gla", [4, DEPTH, 2, P, 256])
    o_lru = dout("o_lru", [DEPTH, P, 32])
    dbg_ym = dout("dbg_ym", [P, DT, T], BF16) if STOP_AFTER else None
    xspill = nc.dram_tensor("xspill", [D, T], F32, kind="Internal").ap()

    b = ARENA0
    A_sm = Arena(nc, b, 11 * KB); b += 11 * KB
    A_h = Arena(nc, b, 32 * KB); b += 32 * KB
    A_ra = Arena(nc, b, 32 * KB); b += 32 * KB
    U_BASE = b
    A_u = Arena(nc, b, 42 * KB); b += 42 * KB
    A_x = Arena(nc, b, 64 * KB); b += 64 * KB
    A_nt = Arena(nc, b, 26 * KB); b += 26 * KB
    assert b <= 229376 - 64, b
    A_mix = Arena(nc, U_BASE + 32 * KB, 74 * KB)
    A_ffn = Arena(nc, U_BASE, 42 * KB)

    par = A_sm.alloc([P, NPAR], F32, "par")
    cst = A_sm.alloc([P, 4, P], F32, "cst")
    ident_b = A_sm.alloc([P, P], BF16, "identb")
    ones_b = A_sm.alloc([P, P], BF16, "onesb")
    mfb = A_sm.alloc([P, 2, P], BF16, "mfb")
    mods = [A_sm.alloc([P, 96], F32, "mods%d" % i) for i in range(DEPTH)]
    coef = [A_sm.alloc([P, 6, DT], F32, "coef%d" % i) for i in range(DEPTH)]
    csil = A_sm.alloc([P, DT], F32, "csil")
    epsb = A_sm.alloc([P, 1], F32, "epsb")
    oneb = A_sm.alloc([P, 1], F32, "oneb")
    lruF = A_sm.alloc([P, 32], F32, "lruF")
    rt = A_sm.alloc([P, DT, NEXP], F32, "rt")
    lg = A_sm.alloc([P, NCH, NEXP], F32, "lg")
    mx = A_sm.alloc([P, NCH, 8], F32, "mx")
    gt = A_sm.alloc([P, NCH, 4], F32, "gt")
    cb = A_sm.alloc([P, NCH, NEXP], F32, "cb")
    ident_f, Mf, Mb, ones_f = cst[:, 0, :], cst[:, 1, :], cst[:, 2, :], cst[:, 3, :]

    hT = A_h.alloc([P, DT, T], BF16, "hT")
    ring = [A_ra.alloc([P, DT, 512], BF16, "ring%d" % i) for i in range(2)]
    ymix = A_u.alloc([P, DT, T], BF16, "ymix")
    rstd = A_nt.alloc([P, T], F32, "rstd")
    tmpf = [A_nt.alloc([P, T], F32, "tmpf%d" % i) for i in range(3)]
    sqb = [A_nt.alloc([P, T], BF16, "sqb%d" % i) for i in range(2)]
    msk = A_nt.alloc([P, 3, T], BF16, "msk")
    psb = [fw.ps([P, 512], F32, "psb%d" % i) for i in range(8)]
    PSK = ['ps%d' % i for i in range(8)]

    def pcol(name, j=0, w=1):
        o, _ = PAR_OFF[name]
        return par[:, o + j:o + j + w]

    def V(fn, reads=(), writes=()):
        fw.op('dve', fn, reads, writes)

    def Aop(fn, reads=(), writes=()):
        fw.op('act', fn, reads, writes)

    def MM(out, lhsT, rhs, start, stop, reads, writes, inc=None):
        fw.op('pe', lambda e: e.matmul(out, lhsT, rhs, start=start, stop=stop), reads, writes,
              inc=(stop if inc is None else inc))

    fw.dma('sp', par[:], par_in, writes=['par'])
    for q in range(4):
        fw.dma('sp', cst[:, q, :], cst_in[:, q, :], writes=['cst'])
    for q in range(3):
        fw.dma('sp', msk[:, q, :], msk_in[:, q, :], writes=['msk'])
    for kt in range(DT):
        fw.dma('sp', rt[:, kt, :], rt_in[kt * P:(kt + 1) * P, :], writes=['rt'])
    V(lambda e: e.tensor_copy(out=ident_b[:], in_=ident_f), ['cst'], ['identb'])
    V(lambda e: e.memset(ones_b[:], 1.0), [], ['onesb'])
    V(lambda e: e.tensor_copy(out=mfb[:], in_=cst[:, 1:3, :]), ['cst'], ['mfb'])
    V(lambda e: e.memset(epsb[:], EPS), [], ['epsb'])
    V(lambda e: e.memset(oneb[:], 1.0), [], ['oneb'])
    Aop(lambda e: e.activation(out=csil[:], in_=pcol('cvec', 0, DT), func=AF.Silu), ['par'], ['csil'])

    MC = 384
    mwb = [A_x.alloc([P, DT, MC], F32, "mwb%d" % i) for i in range(2)]
    kk = 0
    for i in range(DEPTH):
        for ch in range(6 * D // MC):
            bb = kk % 2
            kk += 1
            for kt in range(DT):
                fw.dma('sp', mwb[bb][:, kt, :], mod_w[i, kt * P:(kt + 1) * P, ch * MC:(ch + 1) * MC], writes=[('mwb', bb, kt)])
            for mm in range(MC // P):
                m = ch * (MC // P) + mm
                for kt in range(DT):
                    MM(psb[0][:, m:m + 1], mwb[bb][:, kt, mm * P:(mm + 1) * P], csil[:, kt:kt + 1], kt == 0, kt == DT - 1,
                       [('mwb', bb, kt), 'csil'], ['ps0'])
        V(lambda e, i=i: e.tensor_tensor(out=mods[i][:], in0=psb[0][:, 0:96], in1=pcol('modb%d' % i, 0, 96), op=ALU.add),
          ['par'], [('mods', i), 'ps0'])
        for half, (ng, sh_i, sc_i, g_i) in enumerate([('n1g%d' % i, 0, 1, 2), ('n2g%d' % i, 3, 4, 5)]):
            V(lambda e, i=i, half=half, ng=ng, sc_i=sc_i: e.scalar_tensor_tensor(
                out=coef[i][:, 3 * half + 0, :], in0=mods[i][:, sc_i * DT:(sc_i + 1) * DT], scalar=1.0,
                in1=pcol(ng, 0, DT), op0=ALU.add, op1=ALU.mult), [('mods', i), 'par'], ['coefs'])
            V(lambda e, i=i, half=half, sh_i=sh_i: e.tensor_copy(
                out=coef[i][:, 3 * half + 1, :], in_=mods[i][:, sh_i * DT:(sh_i + 1) * DT]), [('mods', i)], ['coefs'])
            V(lambda e, i=i, half=half, g_i=g_i: e.tensor_copy(
                out=coef[i][:, 3 * half + 2, :], in_=mods[i][:, g_i * DT:(g_i + 1) * DT]), [('mods', i)], ['coefs'])
    fw.barrier()
    A_x.reset()
    xT = A_x.alloc([P, DT, T], F32, "xT")
    for dt in range(DT):
        fw.dma('sp', xT[:, dt, :], xT_in[dt * P:(dt + 1) * P, :], writes=[('x', dt)])

    def rms_stats(src_fn, ntiles, key_fn, width):
        for j in range(ntiles):
            bb = j % 2
            Aop(lambda e, j=j, bb=bb: e.activation(out=sqb[bb][:], in_=src_fn(j), func=AF.Square), [key_fn(j)], [('sqb', bb)])
            for tc in range(2):
                MM(psb[tc][:], ones_b[:], sqb[bb][:, tc * 512:(tc + 1) * 512], j == 0, j == ntiles - 1,
                   [('sqb', bb), 'onesb'], [PSK[tc]], inc=True)
        for tc in range(2):
            Aop(lambda e, tc=tc: e.activation(out=tmpf[0][:, tc * 512:(tc + 1) * 512], in_=psb[tc][:], func=AF.Sqrt,
                                              scale=1.0 / width, bias=epsb[:]), ['epsb'], [('tmpf', 0), PSK[tc]])
        V(lambda e: e.reciprocal(out=rstd[:], in_=tmpf[0][:]), [('tmpf', 0)], ['rstd'])

    def norm_to_hT(i, half):
        rms_stats(lambda j: xT[:, j, :], DT, lambda j: ('x', j), D)
        for dt in range(DT):
            bb = 1 + dt % 2
            V(lambda e, dt=dt, bb=bb: e.tensor_tensor(out=tmpf[bb][:], in0=xT[:, dt, :], in1=rstd[:], op=ALU.mult),
              [('x', dt), 'rstd'], [('tmpf', bb)])
            V(lambda e, dt=dt, bb=bb: e.tensor_scalar(out=hT[:, dt, :], in0=tmpf[bb][:], scalar1=coef[i][:, 3 * half, dt:dt + 1],
                                                      scalar2=coef[i][:, 3 * half + 1, dt:dt + 1], op0=ALU.mult, op1=ALU.add),
              [('tmpf', bb), 'coefs'], [('h', dt)])

    rstate = {'k': 0}

    lq = {'items': [], 'pos': 0, 'slots': []}

    def _issue_load(k):
        if k < len(lq['items']) and k == len(lq['slots']):
            src2d, c0, ncols = lq['items'][k]
            s = ring[(lq['base'] + k) % 2]
            for kt in range(DT):
                fw.dma('pool', s[:, kt, 0:ncols], src2d[kt * P:(kt + 1) * P, c0:c0 + ncols], writes=[('rg', id(s), kt)])
            lq['slots'].append(s)

    def plan_loads(items):
        lq['items'], lq['pos'], lq['slots'], lq['base'] = list(items), 0, [], rstate['k']
        rstate['k'] += len(items)
        _issue_load(0)

    def load_cols(src2d, c0, ncols, prefetch=True):
        k = lq['pos']
        assert lq['items'][k][1] == c0 and lq['items'][k][2] == ncols, (k, lq['items'][k], c0, ncols)
        lq['pos'] += 1
        _issue_load(k)
        if prefetch:
            _issue_load(k + 1)
        return lq['slots'][k]

    def proj_fm(wslot, cofs, m, pi):
        for tc in range(2):
            for kt in range(DT):
                MM(psb[pi + tc][0:m, :], wslot[:, kt, cofs:cofs + m], hT[:, kt, tc * 512:(tc + 1) * 512], kt == 0, kt == DT - 1,
                   [('rg', id(wslot), kt), ('h', kt)], [PSK[pi + tc]])

    def proj_tm(wslot, cofs, n, tt, pi):
        for kt in range(DT):
            MM(psb[pi][:, 0:n], hT[:, kt, tt * P:(tt + 1) * P], wslot[:, kt, cofs:cofs + n], kt == 0, kt == DT - 1,
               [('rg', id(wslot), kt), ('h', kt)], [PSK[pi]])

    def evac_fm(pi, dst_fn, wkeys, func=AF.Copy, m=P):
        for tc in range(2):
            Aop(lambda e, tc=tc: e.activation(out=dst_fn(tc), in_=psb[pi + tc][0:m, :], func=func), [], list(wkeys) + [PSK[pi + tc]])

    def conv_tile(skey, dst_fn, cw, cb_, j):
        src, acc, tmp = tmpf[0], tmpf[1], tmpf[2]
        V(lambda e: e.tensor_scalar(out=acc[:], in0=src[:], scalar1=pcol(cw, 4 * j + 1), scalar2=pcol(cb_, j), op0=ALU.mult, op1=ALU.add),
          [skey, 'par'], [('tmpf', 1)])
        for (k, lo, hi, sh, mi) in [(0, 1, T, -1, 0), (2, 0, T - 1, 1, 1), (3, 0, T - 2, 2, 2)]:
            V(lambda e, lo=lo, hi=hi, sh=sh, mi=mi: e.tensor_tensor(out=tmp[:, lo:hi], in0=src[:, lo + sh:hi + sh], in1=msk[:, mi, lo:hi], op=ALU.mult),
              [skey, 'msk'], [('tmpf', 2)])
            V(lambda e, lo=lo, hi=hi, k=k: e.scalar_tensor_tensor(out=acc[:, lo:hi], in0=tmp[:, lo:hi], scalar=pcol(cw, 4 * j + k),
                                                                  in1=acc[:, lo:hi], op0=ALU.mult, op1=ALU.add),
              [('tmpf', 2), 'par'], [('tmpf', 1)])
        dst_fn(acc)

    def lru_mixer(i):
        A_mix.reset()
        if STOP_AFTER:
            plan_loads([q_ for j_ in range(4) for q_ in [(in_w[i], C_XB + j_ * P, P), (in_w[i], C_GB + j_ * P, P)]])
        inw = in_w[i]
        wbd = A_mix.alloc([P, 2, 2, P], BF16, "lruw")
        xr = A_mix.alloc([P, T], F32, "xr")
        xrb = A_mix.alloc([P, T], BF16, "xrb")
        gl = A_mix.alloc([P, T], F32, "gl")
        gx = A_mix.alloc([P, T], F32, "gx")
        hs = A_mix.alloc([P, T], F32, "hs")
        rr = A_mix.alloc([P, T], F32, "rr")
        ii = A_mix.alloc([P, T], F32, "ii")
        aa = A_mix.alloc([P, T], F32, "aa")
        uu = A_mix.alloc([P, T], F32, "uu")
        hh = A_mix.alloc([P, T], F32, "hh")
        cc = A_mix.alloc([P, 2], F32, "cc")
        for j in range(4):
            wx = load_cols(inw, C_XB + j * P, P)
            for d in range(2):
                for ax in range(2):
                    fw.dma('pool', wbd[:, d, ax, :], lw_in[i, d, ax, j], writes=[('lruw', d, ax)])
            proj_fm(wx, 0, P, 2)
            evac_fm(2, lambda tc: tmpf[0][:, tc * 512:(tc + 1) * 512], [('tmpf', 0)])

            def fin(acc):
                V(lambda e: e.tensor_copy(out=xr[:], in_=acc[:]), [('tmpf', 1)], ['xr'])
                V(lambda e: e.tensor_copy(out=xrb[:], in_=acc[:]), [('tmpf', 1)], ['xrb'])
            conv_tile(('tmpf', 0), fin, 'lru_cw%d' % i, 'lru_cb%d' % i, j)
            wg = load_cols(inw, C_GB + j * P, P)
            proj_fm(wg, 0, P, 4)
            evac_fm(4, lambda tc: gx[:, tc * 512:(tc + 1) * 512], ['gx'])
            V(lambda e: e.tensor_tensor(out=gl[:], in0=gx[:], in1=gx[:], op=ALU.mult), ['gx'], ['gl'])
            V(lambda e: e.tensor_scalar(out=gl[:], in0=gl[:], scalar1=0.044715, scalar2=1.0, op0=ALU.mult, op1=ALU.add), [], ['gl'])
            V(lambda e: e.tensor_tensor(out=gl[:], in0=gl[:], in1=gx[:], op=ALU.mult), ['gx'], ['gl'])
            Aop(lambda e: e.activation(out=gl[:], in_=gl[:], func=AF.Sigmoid, scale=1.5957691216057308), [], ['gl'])
            V(lambda e: e.tensor_tensor(out=gl[:], in0=gl[:], in1=gx[:], op=ALU.mult), ['gx'], ['gl'])
            for d in range(2):
                Aop(lambda e, d=d: e.activation(out=cc[:, 0:1], in_=pcol('lru_lam%d%d' % (i, d), j), func=AF.Exp, scale=-1.0), ['par'], ['cc'])
                Aop(lambda e: e.activation(out=cc[:, 0:1], in_=cc[:, 0:1], func=AF.Ln, bias=oneb[:]), ['oneb'], ['cc'])
                V(lambda e: e.tensor_scalar(out=cc[:, 1:2], in0=cc[:, 0:1], scalar1=-8.0, scalar2=None, op0=ALU.mult), [], ['cc'])
                for ax, (dst, dk, bn, ps0) in enumerate([(rr, 'rr', 'lru_ba%d%d' % (i, d), 2), (ii, 'ii', 'lru_bx%d%d' % (i, d), 4)]):
                    for tc in range(2):
                        MM(psb[ps0 + tc][:], wbd[:, d, ax, :], xrb[:, tc * 512:(tc + 1) * 512], True, True,
                           [('lruw', d, ax), 'xrb'], [PSK[ps0 + tc]])
                        Aop(lambda e, tc=tc, dst=dst, bn=bn, ps0=ps0: e.activation(
                            out=dst[:, tc * 512:(tc + 1) * 512], in_=psb[ps0 + tc][:], func=AF.Sigmoid, bias=pcol(bn, j)),
                            ['par'], [dk, PSK[ps0 + tc]])
                Aop(lambda e: e.activation(out=aa[:], in_=rr[:], func=AF.Exp, scale=cc[:, 1:2]), ['rr', 'cc'], ['aa'])
                V(lambda e: e.tensor_tensor(out=uu[:], in0=aa[:], in1=aa[:], op=ALU.mult), ['aa'], ['uu'])
                V(lambda e: e.tensor_scalar(out=uu[:], in0=uu[:], scalar1=-1.0, scalar2=1.0, op0=ALU.mult, op1=ALU.add), [], ['uu'])
                V(lambda e: e.tensor_scalar(out=uu[:], in0=uu[:], scalar1=0.0, scalar2=None, op0=ALU.max), [], ['uu'])
                Aop(lambda e: e.activation(out=uu[:], in_=uu[:], func=AF.Sqrt), [], ['uu'])
                V(lambda e: e.tensor_tensor(out=uu[:], in0=uu[:], in1=ii[:], op=ALU.mult), ['ii'], ['uu'])
                V(lambda e: e.tensor_tensor(out=uu[:], in0=uu[:], in1=xr[:], op=ALU.mult), ['xr'], ['uu'])
                av = aa[:].rearrange("p (s l) -> p s l", l=256)
                if d == 0:
                    V(lambda e: e.tensor_scalar(out=av[:, 1:4, 0:1], in0=av[:, 1:4, 0:1], scalar1=pcol('flag'), scalar2=None, op0=ALU.mult),
                      ['par'], ['aa'])
                    V(lambda e, d=d: e.tensor_tensor_scan(out=hh[:], data0=aa[:], data1=uu[:], initial=pcol('lru_h0%d%d' % (i, d), j),
                                                          op0=ALU.mult, op1=ALU.add), ['aa', 'uu', 'par'], ['hh'])
                    V(lambda e: e.tensor_copy(out=hs[:], in_=hh[:]), ['hh'], ['hs'])
                else:
                    V(lambda e: e.tensor_scalar(out=av[:, 0:3, 255:256], in0=av[:, 0:3, 255:256], scalar1=pcol('flag'), scalar2=None, op0=ALU.mult),
                      ['par'], ['aa'])
                    V(lambda e, d=d: e.tensor_tensor_scan(out=hh[:, ::-1], data0=aa[:, ::-1], data1=uu[:, ::-1],
                                                          initial=pcol('lru_h0%d%d' % (i, d), j), op0=ALU.mult, op1=ALU.add),
                      ['aa', 'uu', 'par'], ['hh'])
                    V(lambda e: e.tensor_tensor(out=hs[:], in0=hs[:], in1=hh[:], op=ALU.add), ['hh'], ['hs'])
                hv = hh[:].rearrange("p (s l) -> p s l", l=256)
                col = 255 if d == 0 else 0
                fv = lruF[:].rearrange("p (j s d) -> p j s d", j=4, s=4)
                V(lambda e, d=d, col=col, j=j: e.tensor_copy(out=fv[:, j, :, d:d + 1], in_=hv[:, :, col:col + 1]), ['hh'], ['lruF'])
            V(lambda e, j=j: e.tensor_tensor(out=ymix[:, 12 + j, :], in0=hs[:], in1=gl[:], op=ALU.mult), ['hs', 'gl'], [('ym', 12 + j)])
        fw.dma('sp', o_lru[i], lruF[:], reads=['lruF'])

    def gla_mixer(i):
        A_mix.reset()
        if STOP_AFTER:
            plan_loads([(in_w[i], C_Q, 512), (in_w[i], C_G, 512), (in_w[i], C_V, 512), (in_w[i], C_GRAW, 16)])
        inw = in_w[i]
        qT = A_mix.alloc([P, 2, T], F32, "qT")
        kT = A_mix.alloc([P, 2, T], F32, "kT")
        vtm = A_mix.alloc([P, NCH, 512], BF16, "vtm")
        gT_ = A_mix.alloc([P, 4, T], BF16, "gTs")
        grA = A_mix.alloc([32, T], F32, "grA")
        gw = A_mix.alloc([32, 256], F32, "gw")
        ltm = A_mix.alloc([P, NCH, 256], F32, "ltm")
        oT = A_mix.alloc([P, 4, T], F32, "oT")
        eb = A_mix.alloc([P, 2, P], F32, "eb")
        enb = A_mix.alloc([P, 2, P], F32, "enb")
        qt = A_mix.alloc([P, 2, P], BF16, "qt")
        kt_ = A_mix.alloc([P, 2, P], BF16, "kt")
        qtm = A_mix.alloc([P, 4, P], BF16, "qtm")
        ktm = A_mix.alloc([P, 256], BF16, "ktm")
        atT = A_mix.alloc([P, 4, P], BF16, "atT")
        S = A_mix.alloc([P, 2, P], F32, "S")
        Sb = A_mix.alloc([P, 2, P], BF16, "Sb")
        stg = [A_mix.alloc([P, 2, P], F32, "stg%d" % q) for q in range(2)]
        w = load_cols(inw, C_Q, 512)
        for t2 in range(2):
            for (dst, cofs, nm) in [(qT, 0, 'qT'), (kT, 256, 'kT')]:
                proj_fm(w, cofs + t2 * P, P, 2)
                evac_fm(2, lambda tc, dst=dst, t2=t2: dst[:, t2, tc * 512:(tc + 1) * 512], [nm])
        w = load_cols(inw, C_G, 512)
        for t4 in range(4):
            proj_fm(w, t4 * P, P, 4)
            evac_fm(4, lambda tc, t4=t4: gT_[:, t4, tc * 512:(tc + 1) * 512], [('gTs', t4)], func=AF.Silu)
        w = load_cols(inw, C_V, 512)
        for tt in range(NCH):
            pi = 6 + tt % 2
            proj_tm(w, 0, 512, tt, pi)
            V(lambda e, tt=tt, pi=pi: e.tensor_copy(out=vtm[:, tt, :], in_=psb[pi][:]), [], [('vtm', tt), PSK[pi]])
        w = load_cols(inw, C_GRAW, 16)
        V(lambda e: e.memset(grA[:], 1.0), [], ['grA'])
        proj_fm(w, 0, 16, 2)
        evac_fm(2, lambda tc: grA[0:16, tc * 512:(tc + 1) * 512], ['grA'], m=16)
        V(lambda e: e.memset(oT[:], 0.0), [], ['oT'])
        V(lambda e: e.memset(qtm[:], 0.0), [], ['qtm'])
        if STOPX == 1:
            return
        for d in range(2):
            fw.dma('sp', gw[:], gw_in[i, d], writes=['gw'])
            for tt in range(NCH):
                pi = 6 + tt % 2
                MM(psb[pi][:, 0:256], grA[0:32, tt * P:(tt + 1) * P], gw[0:32, :], True, True, ['grA', 'gw'], [PSK[pi]])
                Aop(lambda e, tt=tt, pi=pi: e.activation(out=ltm[:, tt, :], in_=psb[pi][:, 0:256], func=AF.Exp, scale=-1.0), [], [('ltm', tt), PSK[pi]])
                Aop(lambda e, tt=tt: e.activation(out=ltm[:, tt, :], in_=ltm[:, tt, :], func=AF.Ln, bias=oneb[:]), ['oneb'], [('ltm', tt)])
            if STOPX == 2:
                return
            fw.dma('sp', S[:].rearrange("p a b -> p (a b)"), gla_h0[i, d], writes=['S'])
            V(lambda e: e.tensor_copy(out=Sb[:], in_=S[:]), ['S'], ['Sb'])
            Mm = Mf if d == 0 else Mb
            order = list(range(NCH)) if d == 0 else list(range(NCH - 1, -1, -1))
            for step, c in enumerate(order):
                ts = slice(c * P, (c + 1) * P)
                last = P - 1 if d == 0 else 0
                if step > 0 and step % 2 == 0:
                    V(lambda e: e.tensor_scalar(out=S[:], in0=S[:], scalar1=pcol('flag'), scalar2=None, op0=ALU.mult), ['par'], ['S'])
                    V(lambda e: e.tensor_copy(out=Sb[:], in_=S[:]), ['S'], ['Sb'])
                for t2 in range(2):
                    MM(psb[2][:, t2 * P:(t2 + 1) * P], ltm[:, c, t2 * P:(t2 + 1) * P], Mm, True, True, [('ltm', c), 'cst'], [PSK[2]])
                Aop(lambda e: e.activation(out=eb[:].rearrange("p a b -> p (a b)"), in_=psb[2][:, 0:256], func=AF.Exp, scale=-1.0 / 16), [], ['eb', PSK[2]])
                Aop(lambda e: e.activation(out=enb[:].rearrange("p a b -> p (a b)"), in_=psb[2][:, 0:256], func=AF.Exp, scale=1.0 / 16), [], ['enb', PSK[2]])
                V(lambda e, ts=ts: e.scalar_tensor_tensor(out=qt[:], in0=qT[:, :, ts], scalar=0.125, in1=eb[:], op0=ALU.mult, op1=ALU.mult),
                  ['qT', 'eb'], ['qt'])
                V(lambda e, ts=ts: e.tensor_tensor(out=kt_[:], in0=kT[:, :, ts], in1=enb[:], op=ALU.mult), ['kT', 'enb'], ['kt'])
                for h in range(4):
                    rs = slice((h % 2) * 64, (h % 2) * 64 + 64)
                    V(lambda e, h=h, rs=rs: e.tensor_copy(out=qtm[rs, h, :], in_=qt[rs, h // 2, :]), ['qt'], ['qtm'])
                if STOPX == 3:
                    return
                for t2 in range(2):
                    MM(psb[3][:, t2 * P:(t2 + 1) * P], kt_[:, t2, :], ident_b[:], True, True, ['kt', 'identb'], [PSK[3]])
                V(lambda e: e.tensor_copy(out=ktm[:], in_=psb[3][:, 0:256]), [], ['ktm', PSK[3]])
                for h in range(4):
                    rs = slice((h % 2) * 64, (h % 2) * 64 + 64)
                    MM(psb[4][:, h * P:(h + 1) * P], kt_[:, h // 2, :], qtm[:, h, :], True, True, ['kt', 'qtm'], [PSK[4]])
                V(lambda e, d=d: e.tensor_tensor(out=atT[:], in0=psb[4][:].rearrange("p (h t) -> p h t", h=4),
                                                 in1=mfb[:, d, :].unsqueeze(1).to_broadcast([P, 4, P]), op=ALU.mult), ['mfb'], ['atT', PSK[4]])
                if STOPX == 4:
                    return
                for h in range(4):
                    rs = slice((h % 2) * 64, (h % 2) * 64 + 64)
                    MM(psb[5][:, h * P:(h + 1) * P], vtm[:, c, h * P:(h + 1) * P], atT[:, h, :], True, False, [('vtm', c), 'atT'], [PSK[5]], inc=False)
                    MM(psb[5][:, h * P:(h + 1) * P], Sb[:, h // 2, :], qtm[:, h, :], False, True, ['Sb', 'qtm'], [PSK[5]], inc=True)
                V(lambda e, ts=ts: e.tensor_tensor(out=oT[:, :, ts], in0=oT[:, :, ts], in1=psb[5][:].rearrange("p (h t) -> p h t", h=4), op=ALU.add),
                  [], ['oT', PSK[5]])
                if STOPX == 5:
                    return
                for t2 in range(2):
                    MM(psb[6][:, 0:256], ktm[:, t2 * P:(t2 + 1) * P], vtm[:, c, t2 * 256:(t2 + 1) * 256], True, True, ['ktm', ('vtm', c)], [PSK[6]])
                    for hl in range(2):
                        rs = slice(hl * 64, hl * 64 + 64)
                        V(lambda e, t2=t2, hl=hl, rs=rs: e.tensor_tensor(out=S[rs, t2, :], in0=S[rs, t2, :], in1=psb[6][rs, hl * P:(hl + 1) * P], op=ALU.add),
                          [], ['S', PSK[6]])
                    V(lambda e, t2=t2, last=last: e.tensor_scalar(out=S[:, t2, :], in0=S[:, t2, :], scalar1=eb[:, t2, last:last + 1], scalar2=None, op0=ALU.mult),
                      ['eb'], ['S'])
                V(lambda e: e.tensor_copy(out=Sb[:], in_=S[:]), ['S'], ['Sb'])
                if step % 2 == 1:
                    seg = c // 2
                    q = (step // 2) % 2
                    V(lambda e, q=q: e.tensor_copy(out=stg[q][:], in_=S[:]), ['S'], [('stg', q)])
                    fw.dma('sp', o_gla[seg, i, d], stg[q][:].rearrange("p a b -> p (a b)"), reads=[('stg', q)])
        for h in range(4):
            bb = h % 2
            Aop(lambda e, h=h, bb=bb: e.activation(out=sqb[bb][:], in_=oT[:, h, :], func=AF.Square), ['oT'], [('sqb', bb)])
            for tc in range(2):
                MM(psb[tc][:], ones_b[:], sqb[bb][:, tc * 512:(tc + 1) * 512], True, True, [('sqb', bb), 'onesb'], [PSK[tc]])
                Aop(lambda e, tc=tc: e.activation(out=tmpf[0][:, tc * 512:(tc + 1) * 512], in_=psb[tc][:], func=AF.Sqrt,
                                                  scale=1.0 / P, bias=epsb[:]), ['epsb'], [('tmpf', 0), PSK[tc]])
            V(lambda e: e.reciprocal(out=tmpf[1][:], in_=tmpf[0][:]), [('tmpf', 0)], [('tmpf', 1)])
            V(lambda e, h=h: e.tensor_tensor(out=tmpf[2][:], in0=oT[:, h, :], in1=tmpf[1][:], op=ALU.mult), ['oT', ('tmpf', 1)], [('tmpf', 2)])
            V(lambda e, h=h: e.scalar_tensor_tensor(out=ymix[:, 8 + h, :], in0=tmpf[2][:], scalar=pcol('gla_ng%d' % i), in1=gT_[:, h, :],
                                                    op0=ALU.mult, op1=ALU.mult), [('tmpf', 2), 'par', ('gTs', h)], [('ym', 8 + h)])

    def ssd_mixer(i):
        A_mix.reset()
        if STOP_AFTER:
            plan_loads([(in_w[i], C_XBC + j_ * P, P) for j_ in range(12)] + [(in_w[i], C_DT, 16), (in_w[i], C_Z, 512), (in_w[i], C_Z + 512, 512)])
        inw = in_w[i]
        xtm = A_mix.alloc([P, NCH, 1024], BF16, "xtm")
        BCT = A_mix.alloc([P, 4, T], BF16, "BCT")
        Btm = A_mix.alloc([P, NCH, 256], BF16, "Btm")
        yf = A_mix.alloc([P, NCH, 1024], BF16, "yf")
        dtr = A_mix.alloc([P, NCH, 16], F32, "dtr")
        dtd = A_mix.alloc([P, NCH, 16], F32, "dtd")
        ad = A_mix.alloc([P, NCH, 16], F32, "ad")
        nA = A_mix.alloc([P, 16], F32, "nA")
        abc = A_mix.alloc([P, 16, P], F32, "abc")
        DTm = A_mix.alloc([P, 16, P], F32, "DTm")
        WT = A_mix.alloc([P, 16, P], BF16, "WT")
        ST = A_mix.alloc([P, 1024], F32, "ST")
        STb = A_mix.alloc([P, 1024], BF16, "STb")
        sm = A_mix.alloc([P, 4, 16], F32, "ssm")
        rrep, yc, stg = tmpf[1], tmpf[2], tmpf[0]
        xdt, xdd = sqb[0], sqb[1]
        for j in range(12):
            w = load_cols(inw, C_XBC + j * P, P)
            proj_fm(w, 0, P, 2)
            evac_fm(2, lambda tc: tmpf[0][:, tc * 512:(tc + 1) * 512], [('tmpf', 0)])
            if j < 8:
                def fin(acc, j=j):
                    Aop(lambda e: e.activation(out=sqb[0][:], in_=acc[:], func=AF.Silu), [('tmpf', 1)], [('sqb', 0)])
                    for c in range(NCH):
                        pi = 4 + c % 2
                        MM(psb[pi][:, 0:P], sqb[0][:, c * P:(c + 1) * P], ident_b[:], True, True, [('sqb', 0), 'identb'], [PSK[pi]])
                        V(lambda e, c=c, pi=pi: e.tensor_copy(out=xtm[:, c, j * P:(j + 1) * P], in_=psb[pi][:, 0:P]), [], [('xtm', c), PSK[pi]])
            else:
                def fin(acc, j=j):
                    Aop(lambda e: e.activation(out=BCT[:, j - 8, :], in_=acc[:], func=AF.Silu), [('tmpf', 1)], [('BCT', j - 8)])
                    if j < 10:
                        for c in range(NCH):
                            pi = 4 + c % 2
                            MM(psb[pi][:, 0:P], BCT[:, j - 8, c * P:(c + 1) * P], ident_b[:], True, True, [('BCT', j - 8), 'identb'], [PSK[pi]])
                            V(lambda e, c=c, pi=pi: e.tensor_copy(out=Btm[:, c, (j - 8) * P:(j - 7) * P], in_=psb[pi][:, 0:P]), [], [('Btm', c), PSK[pi]])
            conv_tile(('tmpf', 0), fin, 'ssd_cw%d' % i, 'ssd_cb%d' % i, j)
        w = load_cols(inw, C_DT, 16)
        for tt in range(NCH):
            pi = 6 + tt % 2
            proj_tm(w, 0, 16, tt, pi)
            V(lambda e, tt=tt, pi=pi: e.tensor_copy(out=dtr[:, tt, :], in_=psb[pi][:, 0:16]), [], ['dtr', PSK[pi]])
        for d in (1, 0):
            if d == 0:
                wz = [load_cols(inw, C_Z, 512), load_cols(inw, C_Z + 512, 512, prefetch=False)]
            V(lambda e, d=d: e.tensor_tensor(out=dtd[:], in0=dtr[:], in1=pcol('ssd_dtb%d%d' % (i, d), 0, 16).unsqueeze(1).to_broadcast([P, NCH, 16]), op=ALU.add),
              ['dtr', 'par'], ['dtd'])
            Aop(lambda e: e.activation(out=dtd[:], in_=dtd[:], func=AF.Exp), [], ['dtd'])
            Aop(lambda e: e.activation(out=dtd[:], in_=dtd[:], func=AF.Ln, bias=oneb[:]), ['oneb'], ['dtd'])
            Aop(lambda e, d=d: e.activation(out=nA[:], in_=pcol('ssd_alog%d%d' % (i, d), 0, 16), func=AF.Exp), ['par'], ['nA'])
            V(lambda e: e.scalar_tensor_tensor(out=ad[:], in0=dtd[:], scalar=-1.0, in1=nA[:].unsqueeze(1).to_broadcast([P, NCH, 16]), op0=ALU.mult, op1=ALU.mult),
              ['dtd', 'nA'], ['ad'])
            fw.dma('sp', ST[:], ssd_h0[i, d], writes=['ST'])
            V(lambda e: e.tensor_copy(out=STb[:], in_=ST[:]), ['ST'], ['STb'])
            Mm = Mf if d == 0 else Mb
            order = list(range(NCH)) if d == 0 else list(range(NCH - 1, -1, -1))
            for step, c in enumerate(order):
                ts = slice(c * P, (c + 1) * P)
                if step > 0 and step % 2 == 0:
                    V(lambda e: e.tensor_scalar(out=ST[:], in0=ST[:], scalar1=pcol('flag'), scalar2=None, op0=ALU.mult), ['par'], ['ST'])
                    V(lambda e: e.tensor_copy(out=STb[:], in_=ST[:]), ['ST'], ['STb'])
                for hf in range(2):
                    V(lambda e, hf=hf, c=c: e.tensor_tensor(out=rrep[:].rearrange("p (h t) -> p h t", h=8),
                                                            in0=ad[:, c, hf * 8:(hf + 1) * 8].unsqueeze(2).to_broadcast([P, 8, P]),
                                                            in1=ident_f.unsqueeze(1).to_broadcast([P, 8, P]), op=ALU.mult), ['ad', 'cst'], [('tmpf', 1)])
                    for q in range(2):
                        MM(psb[2 + q][:], ones_f, rrep[:, q * 512:(q + 1) * 512], True, True, [('tmpf', 1), 'cst'], [PSK[2 + q]])
                        Aop(lambda e, hf=hf, q=q: e.activation(out=abc[:, hf * 8 + q * 4:hf * 8 + q * 4 + 4, :].rearrange("p h t -> p (h t)"),
                                                               in_=psb[2 + q][:], func=AF.Exp), [], ['abc', PSK[2 + q]])
                MM(psb[4][:, 0:16], Mm, ad[:, c, :], True, True, ['cst', 'ad'], [PSK[4]])
                Aop(lambda e: e.activation(out=sm[:, 3, :], in_=psb[4][:, 0:16], func=AF.Exp), [], ['sm3', PSK[4]])
                MM(psb[4][:, 16:32], ones_f, ad[:, c, :], True, True, ['cst', 'ad'], [PSK[4]])
                Aop(lambda e: e.activation(out=sm[:, 1, :], in_=psb[4][:, 16:32], func=AF.Exp), [], ['sm1', PSK[4]])
                edge = 0 if d == 0 else P - 1
                V(lambda e, edge=edge: e.memset(abc[:, :, edge:edge + 1], 0.0), [], ['abc'])
                a2 = abc[:].rearrange("p h t -> p (h t)")
                d2 = DTm[:].rearrange("p h t -> p (h t)")
                V(lambda e: e.tensor_copy(out=DTm[:], in_=ident_f.unsqueeze(1).to_broadcast([P, 16, P])), ['cst'], ['DTm'])
                if d == 0:
                    V(lambda e: e.tensor_tensor_scan(out=d2, data0=a2, data1=d2, initial=0.0, op0=ALU.mult, op1=ALU.add), ['abc'], ['DTm'])
                else:
                    V(lambda e: e.tensor_tensor_scan(out=d2[:, ::-1], data0=a2[:, ::-1], data1=d2[:, ::-1], initial=0.0, op0=ALU.mult, op1=ALU.add),
                      ['abc'], ['DTm'])
                for g in range(2):
                    MM(psb[5][:, g * P:(g + 1) * P], BCT[:, g, ts], BCT[:, 2 + g, ts], True, True, [('BCT', g), ('BCT', 2 + g)], [PSK[5]])
                for g in range(2):
                    V(lambda e, g=g: e.tensor_tensor(out=WT[:, g * 8:(g + 1) * 8, :], in0=DTm[:, g * 8:(g + 1) * 8, :],
                                                     in1=psb[5][:, g * P:(g + 1) * P].unsqueeze(1).to_broadcast([P, 8, P]), op=ALU.mult),
                      ['DTm'], ['WT', PSK[5]])
                V(lambda e, c=c: e.tensor_tensor(out=xdt[:].rearrange("p (h q) -> p h q", h=16), in0=xtm[:, c, :].rearrange("p (h q) -> p h q", h=16),
                                                 in1=dtd[:, c, :].unsqueeze(2).to_broadcast([P, 16, 64]), op=ALU.mult), [('xtm', c), 'dtd'], [('sqb', 0)])
                for h in range(16):
                    MM(psb[6 + h // 8][:, (h % 8) * 64:(h % 8) * 64 + 64], WT[:, h, :], xdt[:, h * 64:(h + 1) * 64], True, True,
                       ['WT', ('sqb', 0)], [PSK[6 + h // 8]])
                for g in range(2):
                    MM(psb[2 + g][:], BCT[:, 2 + g, ts], STb[:, g * 512:(g + 1) * 512], True, True, [('BCT', 2 + g), 'STb'], [PSK[2 + g]])
                for g in range(2):
                    V(lambda e, g=g: e.tensor_tensor(out=yc[:, g * 512:(g + 1) * 512].rearrange("p (h q) -> p h q", h=8),
                                                     in0=psb[2 + g][:].rearrange("p (h q) -> p h q", h=8),
                                                     in1=sm[:, 3, g * 8:(g + 1) * 8].unsqueeze(2).to_broadcast([P, 8, 64]), op=ALU.mult),
                      ['sm3'], [('tmpf', 2), PSK[2 + g]])
                    V(lambda e, g=g: e.tensor_tensor(out=yc[:, g * 512:(g + 1) * 512], in0=yc[:, g * 512:(g + 1) * 512], in1=psb[6 + g][:], op=ALU.add),
                      [], [('tmpf', 2), PSK[6 + g]])
                ecol = P - 1 if d == 0 else 0
                V(lambda e, ecol=ecol: e.tensor_tensor(out=xdd[:].rearrange("p (h q) -> p h q", h=16), in0=xdt[:].rearrange("p (h q) -> p h q", h=16),
                                                       in1=DTm[:, :, ecol:ecol + 1].to_broadcast([P, 16, 64]), op=ALU.mult), [('sqb', 0), 'DTm'], [('sqb', 1)])
                for g in range(2):
                    MM(psb[4 + g][:], Btm[:, c, g * P:(g + 1) * P], xdd[:, g * 512:(g + 1) * 512], True, True, [('Btm', c), ('sqb', 1)], [PSK[4 + g]])
                V(lambda e: e.tensor_tensor(out=ST[:].rearrange("p (h q) -> p h q", h=16), in0=ST[:].rearrange("p (h q) -> p h q", h=16),
                                            in1=sm[:, 1, :].unsqueeze(2).to_broadcast([P, 16, 64]), op=ALU.mult), ['sm1'], ['ST'])
                for g in range(2):
                    V(lambda e, g=g: e.tensor_tensor(out=ST[:, g * 512:(g + 1) * 512], in0=ST[:, g * 512:(g + 1) * 512], in1=psb[4 + g][:], op=ALU.add),
                      [], ['ST', PSK[4 + g]])
                V(lambda e: e.tensor_copy(out=STb[:], in_=ST[:]), ['ST'], ['STb'])
                if step % 2 == 1:
                    seg = c // 2
                    for q in range(8):
                        MM(psb[2 + q % 2][:, 0:P], ST[:, q * P:(q + 1) * P], ident_f, True, True, ['ST', 'cst'], [PSK[2 + q % 2]])
                        V(lambda e, q=q: e.tensor_copy(out=stg[:, q * P:(q + 1) * P], in_=psb[2 + q % 2][:, 0:P]), [], [('tmpf', 0), PSK[2 + q % 2]])
                    for q in range(8):
                        fw.dma('sp', o_ssd[seg, i, d, q * P:(q + 1) * P, :], stg[:, q * P:(q + 1) * P], reads=[('tmpf', 0)])
                if d == 1:
                    V(lambda e, c=c: e.tensor_copy(out=yf[:, c, :], in_=yc[:]), [('tmpf', 2)], [('yf', c)])
                else:
                    V(lambda e, c=c: e.tensor_tensor(out=yc[:], in0=yc[:], in1=yf[:, c, :], op=ALU.add), [('yf', c)], [('tmpf', 2)])
                    V(lambda e, c=c: e.tensor_tensor(out=xdd[:].rearrange("p (h q) -> p h q", h=16), in0=xtm[:, c, :].rearrange("p (h q) -> p h q", h=16),
                                                     in1=pcol('ssd_D%d' % i, 0, 16).unsqueeze(2).to_broadcast([P, 16, 64]), op=ALU.mult),
                      [('xtm', c), 'par'], [('sqb', 1)])
                    V(lambda e: e.tensor_tensor(out=yc[:], in0=yc[:], in1=xdd[:], op=ALU.add), [('sqb', 1)], [('tmpf', 2)])
                    for zq in range(2):
                        proj_tm(wz[zq], 0, 512, c, 2 + zq)
                        Aop(lambda e, zq=zq: e.activation(out=tmpf[0][:, zq * 512:(zq + 1) * 512], in_=psb[2 + zq][:], func=AF.Silu),
                            [], [('tmpf', 0), PSK[2 + zq]])
                    V(lambda e: e.tensor_tensor(out=yc[:], in0=yc[:], in1=tmpf[0][:], op=ALU.mult), [('tmpf', 0)], [('tmpf', 2)])
                    Aop(lambda e: e.activation(out=tmpf[0][:], in_=yc[:], func=AF.Square), [('tmpf', 2)], [('tmpf', 0)])
                    V(lambda e: e.reduce_sum(out=sm[:, 0, 0:1], in_=tmpf[0][:], axis=AX.X), [('tmpf', 0)], ['sm0'])
                    Aop(lambda e: e.activation(out=sm[:, 0, 1:2], in_=sm[:, 0, 0:1], func=AF.Sqrt, scale=1.0 / 1024, bias=epsb[:]), ['epsb'], ['sm0'])
                    V(lambda e: e.reciprocal(out=sm[:, 0, 2:3], in_=sm[:, 0, 1:2]), [], ['sm0'])
                    V(lambda e: e.tensor_scalar(out=xdd[:], in0=yc[:], scalar1=sm[:, 0, 2:3], scalar2=None, op0=ALU.mult), [('tmpf', 2), 'sm0'], [('sqb', 1)])
                    for q in range(8):
                        pi = 4 + q % 2
                        MM(psb[pi][:, 0:P], xdd[:, q * P:(q + 1) * P], ident_b[:], True, True, [('sqb', 1), 'identb'], [PSK[pi]])
                        V(lambda e, q=q, ts=ts, pi=pi: e.tensor_scalar(out=ymix[:, q, ts], in0=psb[pi][:, 0:P], scalar1=pcol('ssd_ng%d' % i, q),
                                                                       scalar2=None, op0=ALU.mult), ['par'], [('ym', q), PSK[pi]])

    def ffn_groups(groups, g2cols, slots, gT, silb):
        sl = [0]
        k0 = rstate['k']
        rstate['k'] += 3 * len(groups)

        def slot_of(gi, which):
            return slots[(k0 + 3 * gi + which) % 4]

        def issue_w1(gi):
            w1_, w3_, w2_, g0, J, _, _ = groups[gi]
            s1 = slot_of(gi, 0)
            for kt in range(DT):
                fw.dma('pool', s1[:, kt, 0:J * P], w1_[kt * P:(kt + 1) * P, g0 * P:(g0 + J) * P], writes=[('rg', id(s1), kt)])

        def issue_w3w2(gi):
            w1_, w3_, w2_, g0, J, _, _ = groups[gi]
            s3, s2 = slot_of(gi, 1), slot_of(gi, 2)
            for kt in range(DT):
                fw.dma('pool', s3[:, kt, 0:J * P], w3_[kt * P:(kt + 1) * P, g0 * P:(g0 + J) * P], writes=[('rg', id(s3), kt)])
            w2v = s2[:].rearrange("p a b -> p (a b)").rearrange("p (j d) -> p j d", j=4)
            for j in range(J):
                for q in range(4):
                    fw.dma('pool', w2v[:, j, q * 512:(q + 1) * 512], w2_[(g0 + j) * P:(g0 + j + 1) * P, q * 512:(q + 1) * 512],
                           writes=[('rg', id(s2), 4 * j + q)])

        issue_w1(0)
        issue_w3w2(0)
        for gi, (w1_, w3_, w2_, g0, J, comb_fn, pre_fn) in enumerate(groups):
            s1, s3, s2 = slot_of(gi, 0), slot_of(gi, 1), slot_of(gi, 2)
            w2v = s2[:].rearrange("p a b -> p (a b)").rearrange("p (j d) -> p j d", j=4)
            if gi + 1 < len(groups):
                issue_w1(gi + 1)
            if pre_fn is not None:
                pre_fn()
            for j in range(J):
                for tc in range(2):
                    a_, b_ = 2 + 2 * (sl[0] % 2), 3 + 2 * (sl[0] % 2)
                    sb_ = sl[0] % 2
                    sl[0] += 1
                    for kt in range(DT):
                        MM(psb[a_][:], s1[:, kt, j * P:(j + 1) * P], hT[:, kt, tc * 512:(tc + 1) * 512], kt == 0, kt == DT - 1,
                           [('rg', id(s1), kt), ('h', kt)], [PSK[a_]])
                    for kt in range(DT):
                        MM(psb[b_][:], s3[:, kt, j * P:(j + 1) * P], hT[:, kt, tc * 512:(tc + 1) * 512], kt == 0, kt == DT - 1,
                           [('rg', id(s3), kt), ('h', kt)], [PSK[b_]])
                    Aop(lambda e, a_=a_, sb_=sb_: e.activation(out=silb[sb_][:], in_=psb[a_][:], func=AF.Silu), [], [('silb', sb_), PSK[a_]])
                    if comb_fn is None:
                        V(lambda e, b_=b_, sb_=sb_, j=j, tc=tc: e.tensor_tensor(out=gT[:, j, tc * 512:(tc + 1) * 512], in0=silb[sb_][:], in1=psb[b_][:], op=ALU.mult),
                          [('silb', sb_)], [('gT', j, tc), PSK[b_]])
                    else:
                        V(lambda e, b_=b_, sb_=sb_: e.tensor_tensor(out=silb[sb_][:], in0=silb[sb_][:], in1=psb[b_][:], op=ALU.mult),
                          [], [('silb', sb_), PSK[b_]])
                        V(lambda e, sb_=sb_, j=j, tc=tc: e.tensor_tensor(out=gT[:, j, tc * 512:(tc + 1) * 512], in0=silb[sb_][:], in1=comb_fn(tc), op=ALU.mult),
                          [('silb', sb_), 'rstd'], [('gT', j, tc)])
            if gi + 1 < len(groups):
                issue_w3w2(gi + 1)
            for dt in range(DT):
                for tc in range(2):
                    o_ = 6 + (sl[0] % 2)
                    sl[0] += 1
                    for j in range(J):
                        MM(psb[o_][:], w2v[:, j, dt * P:(dt + 1) * P], gT[:, j, tc * 512:(tc + 1) * 512], j == 0, j == J - 1,
                           [('rg', id(s2), 4 * j + dt // 4), ('gT', j, tc)], [PSK[o_]])
                    V(lambda e, dt=dt, tc=tc, o_=o_: e.scalar_tensor_tensor(
                        out=xT[:, dt, tc * 512:(tc + 1) * 512], in0=psb[o_][:], scalar=g2cols[dt],
                        in1=xT[:, dt, tc * 512:(tc + 1) * 512], op0=ALU.mult, op1=ALU.add), ['coefs'], [('x', dt), PSK[o_]])

    def make_groups(w1_, w3_, w2_, FF, comb_fn=None, pre_fn=None):
        out = []
        for g0 in range(0, FF // P, 4):
            out.append((w1_, w3_, w2_, g0, min(4, FF // P - g0), comb_fn, pre_fn if g0 == 0 else None))
        return out

    def moe_layer(i, g2cols, slots, gTb, silb):
        comb = rstd
        cbT = tmpf[1]
        sel = tmpf[2]
        for dt in range(DT):
            bb = 1 + dt % 2
            V(lambda e, dt=dt, bb=bb: e.tensor_tensor(out=tmpf[bb][:], in0=xT[:, dt, :], in1=rstd[:], op=ALU.mult), [('x', dt), 'rstd'], [('tmpf', bb)])
            V(lambda e, dt=dt, bb=bb: e.tensor_scalar(out=tmpf[bb][:], in0=tmpf[bb][:], scalar1=coef[i][:, 3, dt:dt + 1], scalar2=coef[i][:, 4, dt:dt + 1],
                                                      op0=ALU.mult, op1=ALU.add), ['coefs'], [('tmpf', bb)])
            for tt in range(NCH):
                MM(psb[tt][:, 0:NEXP], tmpf[bb][:, tt * P:(tt + 1) * P], rt[:, dt, :], dt == 0, dt == DT - 1,
                   [('tmpf', bb), 'rt'], [PSK[tt]], inc=True)
        for tt in range(NCH):
            V(lambda e, tt=tt: e.tensor_copy(out=lg[:, tt, :], in_=psb[tt][:, 0:NEXP]), [], ['lg', PSK[tt]])
        for tt in range(NCH):
            V(lambda e, tt=tt: e.max(out=mx[:, tt, :], in_=lg[:, tt, :]), ['lg'], ['mx'])
        V(lambda e: e.tensor_tensor(out=gt[:, :, 0:1], in0=mx[:, :, 1:2], in1=mx[:, :, 0:1], op=ALU.subtract), ['mx'], ['gt'])
        Aop(lambda e: e.activation(out=gt[:, :, 1:2], in_=gt[:, :, 0:1], func=AF.Exp), [], ['gt'])
        V(lambda e: e.tensor_scalar(out=gt[:, :, 1:2], in0=gt[:, :, 1:2], scalar1=1.0, scalar2=None, op0=ALU.add), [], ['gt'])
        V(lambda e: e.reciprocal(out=gt[:, :, 2:3], in_=gt[:, :, 1:2]), [], ['gt'])
        V(lambda e: e.tensor_scalar(out=gt[:, :, 3:4], in0=gt[:, :, 2:3], scalar1=-1.0, scalar2=1.0, op0=ALU.mult, op1=ALU.add), [], ['gt'])
        V(lambda e: e.tensor_tensor(out=cb[:], in0=lg[:], in1=mx[:, :, 0:1].to_broadcast([P, NCH, NEXP]), op=ALU.is_equal), ['lg', 'mx'], ['cb'])
        V(lambda e: e.tensor_tensor(out=cb[:], in0=cb[:], in1=gt[:, :, 2:3].to_broadcast([P, NCH, NEXP]), op=ALU.mult), ['gt'], ['cb'])
        V(lambda e: e.tensor_tensor(out=lg[:], in0=lg[:], in1=mx[:, :, 1:2].to_broadcast([P, NCH, NEXP]), op=ALU.is_equal), ['mx'], ['lg'])
        V(lambda e: e.tensor_tensor(out=lg[:], in0=lg[:], in1=gt[:, :, 3:4].to_broadcast([P, NCH, NEXP]), op=ALU.mult), ['gt'], ['lg'])
        V(lambda e: e.tensor_tensor(out=cb[:], in0=cb[:], in1=lg[:], op=ALU.add), ['lg'], ['cb'])
        if os.environ.get('MOEDBG'):
            for nm_, t_, k_ in [('dbg_cb', cb, 'cb'), ('dbg_mx', mx, 'mx')]:
                fw.dma('sp', dout(nm_, [P, NCH, 8]), t_[:], reads=[k_])
        for hf in range(2):
            for q in range(4):
                MM(psb[1][0:NEXP, q * P:(q + 1) * P], cb[:, hf * 4 + q, :], ident_f, True, True, ['cb', 'cst'], [PSK[1]])
            V(lambda e, hf=hf: e.tensor_copy(out=cbT[0:NEXP, hf * 512:(hf + 1) * 512], in_=psb[1][0:NEXP, :]), [], [('tmpf', 1), PSK[1]])
        V(lambda e: e.tensor_copy(out=sel[0:NEXP, :].rearrange("p (a b) -> p a b", a=NEXP),
                                  in_=cst[0:NEXP, 0, 0:NEXP].unsqueeze(2).to_broadcast([NEXP, NEXP, P])), ['cst'], [('tmpf', 2)])
        groups = []
        for ex in range(NEXP):
            def pre(ex=ex):
                for tc in range(2):
                    MM(psb[tc][:], sel[0:NEXP, ex * P:(ex + 1) * P], cbT[0:NEXP, tc * 512:(tc + 1) * 512], True, True, [('tmpf', 2), ('tmpf', 1)], [PSK[tc]])
                    V(lambda e, tc=tc: e.tensor_copy(out=comb[:, tc * 512:(tc + 1) * 512], in_=psb[tc][:]), [], ['rstd', PSK[tc]])
            groups += make_groups(mw1[ex], mw3[ex], mw2[ex], FF_EXP, lambda tc: comb[:, tc * 512:(tc + 1) * 512], pre)
        ffn_groups(groups, g2cols, slots, gTb, silb)

    def stop_here():
        fw.barrier()
        for kt in range(DT):
            fw.dma('sp', dbg_ym[:, kt, :], ymix[:, kt, :], reads=[('ym', kt)])
        for dt in range(DT):
            fw.dma('sp', yT_out[dt * P:(dt + 1) * P, :], xT[:, dt, :], reads=[('x', dt)])
        fw.finish()
        return nc

    for i in range(DEPTH):
        plan = []
        for j in range(4):
            plan += [(in_w[i], C_XB + j * P, P), (in_w[i], C_GB + j * P, P)]
        plan += [(in_w[i], C_Q, 512), (in_w[i], C_G, 512), (in_w[i], C_V, 512), (in_w[i], C_GRAW, 16)]
        plan += [(in_w[i], C_XBC + j * P, P) for j in range(12)] + [(in_w[i], C_DT, 16), (in_w[i], C_Z, 512), (in_w[i], C_Z + 512, 512)]
        plan += [(out_w[i], q * 512, 512) for q in range(4)]
        if not STOP_AFTER:
            plan_loads(plan)
        norm_to_hT(i, 0)
        for dt in range(DT):
            fw.dma('sp', xspill[dt * P:(dt + 1) * P, :], xT[:, dt, :], reads=[('x', dt)])
        fw.barrier()
        for nm, fn in [('lru', lru_mixer), ('gla', gla_mixer), ('ssd', ssd_mixer)]:
            if STOP_AFTER and STOP_AFTER[1] == i and STOP_AFTER[0] in ('lru', 'gla', 'ssd') and STOP_AFTER[0] != nm and STOP_AFTER[2:] == ('only',):
                continue
            fn(i)
            fw.barrier()
            if STOP_AFTER == (nm, i) or STOP_AFTER == (nm, i, 'only'):
                for dt in range(DT):
                    fw.dma('sp', xT[:, dt, :], xspill[dt * P:(dt + 1) * P, :], writes=[('x', dt)])
                return stop_here()
        for dt in range(DT):
            fw.dma('sp', xT[:, dt, :], xspill[dt * P:(dt + 1) * P, :], writes=[('x', dt)])
        if STOP_AFTER:
            plan_loads([(out_w[i], q * 512, 512) for q in range(4)])
        for dt4 in range(4):
            w = load_cols(out_w[i], dt4 * 512, 512)
            for dq in range(4):
                dt = dt4 * 4 + dq
                for tc in range(2):
                    o_ = 6 + tc
                    for kt in range(DT):
                        MM(psb[o_][:], w[:, kt, dq * P:(dq + 1) * P], ymix[:, kt, tc * 512:(tc + 1) * 512], kt == 0, kt == DT - 1,
                           [('rg', id(w), kt), ('ym', kt)], [PSK[o_]])
                    V(lambda e, dt=dt, tc=tc, o_=o_: e.scalar_tensor_tensor(
                        out=xT[:, dt, tc * 512:(tc + 1) * 512], in0=psb[o_][:], scalar=coef[i][:, 2, dt:dt + 1],
                        in1=xT[:, dt, tc * 512:(tc + 1) * 512], op0=ALU.mult, op1=ALU.add), ['coefs'], [('x', dt), PSK[o_]])
        if STOP_AFTER == ('mix', i):
            return stop_here()
        norm_to_hT(i, 1)
        fw.barrier()
        A_ffn.reset()
        slots = ring + [A_ffn.alloc([P, DT, 512], BF16, "ringB%d" % q) for q in range(2)]
        gTb = A_ffn.alloc([P, 4, T], BF16, "gT")
        silb = [A_ffn.alloc([P, 512], BF16, "silb%d" % q) for q in range(2)]
        g2cols = [coef[i][:, 5, q:q + 1] for q in range(DT)]
        if i % 2 == 0:
            ffn_groups(make_groups(w1, w3, w2, FF_DENSE), g2cols, slots, gTb, silb)
        else:
            moe_layer(i, g2cols, slots, gTb, silb)
        fw.barrier()
        if STOP_AFTER == ('ffn', i):
            return stop_here()

    rms_stats(lambda j: xT[:, j, :], DT, lambda j: ('x', j), D)
    for dt in range(DT):
        bb = 1 + dt % 2
        V(lambda e, dt=dt, bb=bb: e.tensor_tensor(out=tmpf[bb][:], in0=xT[:, dt, :], in1=rstd[:], op=ALU.mult), [('x', dt), 'rstd'], [('tmpf', bb)])
        V(lambda e, dt=dt, bb=bb: e.tensor_scalar(out=tmpf[bb][:], in0=tmpf[bb][:], scalar1=pcol('final_g', dt), scalar2=None, op0=ALU.mult),
          ['par'], [('tmpf', bb)])
        fw.dma('sp', yT_out[dt * P:(dt + 1) * P, :], tmpf[bb][:], reads=[('tmpf', bb)])
    fw.finish()
    return nc


def _host_params(core, inp):
    par = np.zeros((P, NPAR), np.float32)

    def put(name, vec):
        o, w = PAR_OFF[name]
        par[:, o:o + w] = np.asarray(vec, np.float32).reshape(w, P).T

    def rep(name, vec):
        o, w = PAR_OFF[name]
        par[:, o:o + w] = np.asarray(vec, np.float32).reshape(1, w)
    sample = core < 4
    put('cvec', inp['c'][core] if sample else inp['c_ctx'])
    put('final_g', inp['final_norm_g'])
    rep('flag', [1.0 if sample else 0.0])
    for i in range(DEPTH):
        put('modb%d' % i, inp['mod_b'][i])
        put('n1g%d' % i, inp['norm1_g'][i])
        put('n2g%d' % i, inp['norm2_g'][i])
        o, w = PAR_OFF['lru_cw%d' % i]
        par[:, o:o + w] = inp['lru_conv_w'][i].reshape(4, 4, P).transpose(2, 1, 0).reshape(P, 16)
        put('lru_cb%d' % i, inp['lru_conv_b'][i])
        for d in range(2):
            put('lru_ba%d%d' % (i, d), inp['lru_ba'][i, d])
            put('lru_bx%d%d' % (i, d), inp['lru_bx'][i, d])
            put('lru_lam%d%d' % (i, d), inp['lru_lambda'][i, d])
            put('lru_h0%d%d' % (i, d), inp['state_lru'][core, i, d] if sample else np.zeros(512, np.float32))
        o, w = PAR_OFF['ssd_cw%d' % i]
        par[:, o:o + w] = inp['ssd_conv_w'][i].reshape(4, 12, P).transpose(2, 1, 0).reshape(P, 48)
        put('ssd_cb%d' % i, inp['ssd_conv_b'][i])
        put('ssd_ng%d' % i, inp['ssd_norm_g'][i])
        rep('ssd_D%d' % i, inp['ssd_D'][i])
        for d in range(2):
            rep('ssd_dtb%d%d' % (i, d), inp['ssd_dt_bias'][i, d])
            rep('ssd_alog%d%d' % (i, d), inp['ssd_A_log'][i, d])
        put('gla_ng%d' % i, inp['gla_norm_g'][i])
    return par


def _host_consts(core):
    import ml_dtypes
    s = np.arange(P)
    cst = np.zeros((P, 4, P), np.float32)
    cst[:, 0] = np.eye(P)
    cst[:, 1] = (s[:, None] <= s[None, :])
    cst[:, 2] = (s[:, None] >= s[None, :])
    cst[:, 3] = 1.0
    L = 64 if core < 4 else 256
    t = np.arange(T)
    m = np.stack([(t % L != 0), (t % L != L - 1), (t % L < L - 2)], 0).astype(np.float32)
    msk = np.broadcast_to(m[None], (P, 3, T)).astype(ml_dtypes.bfloat16)
    return cst, np.ascontiguousarray(msk)


def make_in_maps(inp, cores=range(8)):
    inp = {k: np.asarray(v) for k, v in inp.items()}
    gw = np.zeros((DEPTH, 2, 32, 256), np.float32)
    gw[:, :, 0:16] = inp['gla_gate_w']
    gw[:, :, 16] = inp['gla_gate_b']
    lw = np.zeros((DEPTH, 2, 2, 4, P, P), np.float32)
    for ax, nm in enumerate(['lru_wa', 'lru_wx']):
        wv = inp[nm]
        for j in range(4):
            lw[:, :, ax, j, 0:64, 0:64] = wv[:, :, 2 * j]
            lw[:, :, ax, j, 64:128, 64:128] = wv[:, :, 2 * j + 1]
    maps = []
    for core in cores:
        sample = core < 4
        x = inp['x_sample'][core] if sample else inp['x_prompt'][4 * (core - 4):4 * (core - 3)].reshape(T, D)
        cst, msk = _host_consts(core)
        if sample:
            sh = inp['state_ssd'][core]
            ssd_h0 = np.ascontiguousarray(sh.reshape(DEPTH, 2, 1024, P).transpose(0, 1, 3, 2))
            gh = inp['state_gla'][core]
            gla_h0 = np.ascontiguousarray(gh.reshape(DEPTH, 2, 2, 2, 64, P).transpose(0, 1, 3, 4, 2, 5).reshape(DEPTH, 2, P, 256))
        else:
            ssd_h0 = np.zeros((DEPTH, 2, P, 1024), np.float32)
            gla_h0 = np.zeros((DEPTH, 2, P, 256), np.float32)
        maps.append({
            "xT": np.ascontiguousarray(x.T), "par": _host_params(core, inp), "cst": cst, "msk": msk,
            "mod_w": inp['mod_w'], "in_w": inp['in_w'], "out_w": inp['out_w'],
            "ffn_w1": inp['ffn_w1'][0], "ffn_w3": inp['ffn_w3'][0], "ffn_w2": inp['ffn_w2'][0],
            "router": inp['moe_router'][0], "moe_w1": inp['moe_w1'][0], "moe_w3": inp['moe_w3'][0], "moe_w2": inp['moe_w2'][0],
            "gla_gw": gw, "lru_w": lw, "ssd_h0": ssd_h0, "gla_h0": gla_h0,
        })
    return maps


def assemble(results):
    ys = [np.ascontiguousarray(r["yT"].T) for r in results]
    y_sample = np.stack(ys[:4], 0)
    y_prompt = np.concatenate(ys[4:], 0).reshape(16, 256, D)
    pr = results[4:]
    new_ssd = np.concatenate([r["o_ssd"].reshape(4, DEPTH, 2, 16, 64, P) for r in pr], 0)
    new_gla = np.concatenate([r["o_gla"].reshape(4, DEPTH, 2, 2, 64, 2, P).transpose(0, 1, 2, 5, 3, 4, 6).reshape(4, DEPTH, 2, 4, 64, P)
                              for r in pr], 0)
    new_lru = np.concatenate([r["o_lru"].reshape(DEPTH, P, 4, 4, 2).transpose(3, 0, 4, 2, 1).reshape(4, DEPTH, 2, 512) for r in pr], 0)
    return (y_prompt, y_sample, np.ascontiguousarray(new_ssd), np.ascontiguousarray(new_gla), np.ascontiguousarray(new_lru))


def kernel(**inputs):
    nc = build_program()
    in_maps = make_in_maps(inputs)
    res = run_bass_kernel_spmd(nc, in_maps, core_ids=list(range(8)))
    return assemble(res.results)
```

```python
from contextlib import ExitStack
import numpy as np
import concourse.bass as bass
import concourse.mybir as mybir
from concourse.bass_utils import run_bass_kernel_spmd

F32 = mybir.dt.float32
BF16 = mybir.dt.bfloat16
AF = mybir.ActivationFunctionType
ALU = mybir.AluOpType
AX = mybir.AxisListType

P = 128
T = 1024
D = 2048
DT = 16
NCH = 8
DEPTH = 2
IN_COLS = 5152
FF_DENSE = 5504
FF_EXP = 7168
NEXP = 8
EPS = 1e-6
ND = 48
ARENA0 = 16512
KB = 1024
import os
STOPX = int(os.environ.get('STOPX', '0'))
STOP_AFTER = None
C_Z, C_XBC, C_DT, C_Q, C_K, C_V, C_G, C_GRAW, C_XB, C_GB = 0, 1024, 2560, 2576, 2832, 3088, 3600, 4112, 4128, 4640


class FW:
    def __init__(self, nc):
        self.nc = nc
        self.es = ExitStack()
        self.eng = {'pe': nc.tensor, 'dve': nc.vector, 'act': nc.scalar, 'pool': nc.gpsimd, 'sp': nc.sync}
        self.sem = {k: self.es.enter_context(nc.semaphore("s_" + k)) for k in ['pe', 'dve', 'act', 'pool']}
        self.cnt = {k: 0 for k in self.sem}
        self.dsem = [self.es.enter_context(nc.semaphore("d%d" % i)) for i in range(ND)]
        self.dcnt = [0] * ND
        self.dnext = 0
        self.seen = {e: {} for e in self.eng}
        self.lastw = {}
        self.readers = {}
        self.nbuf = 0

    def ps(self, shape, dtype=F32, name=None):
        self.nbuf += 1
        return self.es.enter_context(self.nc.psum_tensor(name or ("p%d" % self.nbuf), list(shape), dtype))

    def _semh(self, sk):
        return self.sem[sk[1]] if sk[0] == 'e' else self.dsem[sk[1]]

    def _need(self, E, deps):
        for sk, v in deps:
            if sk[0] == 'd':
                v = 16 * self.dcnt[sk[1]]
            if sk == ('e', 'pe') and E == 'pe':
                continue
            if self.seen[E].get(sk, 0) >= v:
                continue
            self.eng[E].wait_ge(self._semh(sk), v)
            self.seen[E][sk] = v

    def _deps(self, reads, writes):
        deps = []
        for r in reads:
            lw = self.lastw.get(r)
            if lw:
                deps.append(lw)
        for w in writes:
            lw = self.lastw.get(w)
            if lw:
                deps.append(lw)
            deps.extend(self.readers.get(w, {}).items())
        return deps

    def _commit(self, key, val, reads, writes):
        for r in reads:
            self.readers.setdefault(r, {})[key] = val
        for w in writes:
            self.lastw[w] = (key, val)
            self.readers[w] = {}

    def op(self, E, fn, reads=(), writes=(), inc=True):
        self._need(E, self._deps(reads, writes))
        ins = fn(self.eng[E])
        val = self.cnt[E] + 1
        if inc:
            ins.then_inc(self.sem[E], 1)
            self.cnt[E] = val
        self._commit(('e', E), val, reads, writes)

    def dma(self, Q, out, in_, reads=(), writes=()):
        self._need(Q, self._deps(reads, writes))
        i = self.dnext
        self.dnext = (i + 1) % ND
        self.eng[Q].dma_start(out=out, in_=in_).then_inc(self.dsem[i], 16)
        self.dcnt[i] += 1
        self._commit(('d', i), 16 * self.dcnt[i], reads, writes)

    def barrier(self):
        for E in self.eng:
            deps = [(('e', k), self.cnt[k]) for k in self.sem if self.cnt[k] and k != E]
            deps += [(('d', i), 16 * self.dcnt[i]) for i in range(ND) if self.dcnt[i]]
            self._need(E, deps)

    def finish(self):
        for i in range(ND):
            if self.dcnt[i]:
                self.nc.sync.wait_ge(self.dsem[i], 16 * self.dcnt[i])
        for k in self.sem:
            if self.cnt[k]:
                self.nc.sync.wait_ge(self.sem[k], self.cnt[k])
        self.es.close()


class Arena:
    uid = 0

    def __init__(self, nc, base, size):
        self.nc, self.base, self.size, self.off = nc, base, size, 0

    def alloc(self, shape, dtype, name):
        n = 1
        for s in shape[1:]:
            n *= s
        nbytes = (n * (4 if dtype == F32 else 2) + 31) // 32 * 32
        assert self.off + nbytes <= self.size, (name, self.off, nbytes, self.size)
        Arena.uid += 1
        t = self.nc.alloc_sbuf_tensor_at("%s_%d" % (name, Arena.uid), list(shape), dtype, offset=self.base + self.off)
        self.off += nbytes
        return t

    def reset(self):
        self.off = 0


def _param_layout():
    off = {}
    n = 0

    def add(name, w):
        nonlocal n
        off[name] = (n, w)
        n += w
    add('cvec', DT)
    add('final_g', DT)
    add('flag', 1)
    for i in range(DEPTH):
        add('modb%d' % i, 96)
        add('n1g%d' % i, DT)
        add('n2g%d' % i, DT)
        add('lru_cw%d' % i, 16)
        add('lru_cb%d' % i, 4)
        for d in range(2):
            add('lru_ba%d%d' % (i, d), 4)
            add('lru_bx%d%d' % (i, d), 4)
            add('lru_lam%d%d' % (i, d), 4)
            add('lru_h0%d%d' % (i, d), 4)
        add('ssd_cw%d' % i, 48)
        add('ssd_cb%d' % i, 12)
        add('ssd_ng%d' % i, 8)
        add('ssd_D%d' % i, 16)
        for d in range(2):
            add('ssd_dtb%d%d' % (i, d), 16)
            add('ssd_alog%d%d' % (i, d), 16)
        add('gla_ng%d' % i, 1)
    return off, n


PAR_OFF, NPAR = _param_layout()


def build_program():
    nc = bass.Bass("TRN2", target_bir_lowering=False)
    fw = FW(nc)

    def din(name, shape, dt=F32):
        return nc.dram_tensor(name, list(shape), dt, kind="ExternalInput").ap()

    def dout(name, shape, dt=F32):
        return nc.dram_tensor(name, list(shape), dt, kind="ExternalOutput").ap()

    xT_in = din("xT", [D, T])
    par_in = din("par", [P, NPAR])
    cst_in = din("cst", [P, 4, P])
    msk_in = din("msk", [P, 3, T], BF16)
    mod_w = din("mod_w", [DEPTH, D, 6 * D])
    in_w = din("in_w", [DEPTH, D, IN_COLS])
    out_w = din("out_w", [DEPTH, D, D])
    w1 = din("ffn_w1", [D, FF_DENSE])
    w3 = din("ffn_w3", [D, FF_DENSE])
    w2 = din("ffn_w2", [FF_DENSE, D])
    rt_in = din("router", [D, NEXP])
    full = STOP_AFTER is None
    mw1 = din("moe_w1", [NEXP, D, FF_EXP]) if full else None
    mw3 = din("moe_w3", [NEXP, D, FF_EXP]) if full else None
    mw2 = din("moe_w2", [NEXP, FF_EXP, D]) if full else None
    gw_in = din("gla_gw", [DEPTH, 2, 32, 256])
    lw_in = din("lru_w", [DEPTH, 2, 2, 4, P, P])
    ssd_h0 = din("ssd_h0", [DEPTH, 2, P, 1024])
    gla_h0 = din("gla_h0", [DEPTH, 2, P, 256])
    yT_out = dout("yT", [D, T])
    o_ssd = dout("o_ssd", [4, DEPTH, 2, 1024, P])
    o_gla = dout("o_gla", [4, DEPTH, 2, P, 256])
    o_lru = dout("o_lru", [DEPTH, P, 32])
    dbg_ym = dout("dbg_ym", [P, DT, T], BF16) if STOP_AFTER else None
    xspill = nc.dram_tensor("xspill", [D, T], F32, kind="Internal").ap()

    b = ARENA0
    A_sm = Arena(nc, b, 11 * KB); b += 11 * KB
    A_h = Arena(nc, b, 32 * KB); b += 32 * KB
    A_ra = Arena(nc, b, 32 * KB); b += 32 * KB
    U_BASE = b
    A_u = Arena(nc, b, 42 * KB); b += 42 * KB
    A_x = Arena(nc, b, 64 * KB); b += 64 * KB
    A_nt = Arena(nc, b, 26 * KB); b += 26 * KB
    assert b <= 229376 - 64, b
    A_mix = Arena(nc, U_BASE + 32 * KB, 74 * KB)
    A_ffn = Arena(nc, U_BASE, 42 * KB)

    par = A_sm.alloc([P, NPAR], F32, "par")
    cst = A_sm.alloc([P, 4, P], F32, "cst")
    ident_b = A_sm.alloc([P, P], BF16, "identb")
    ones_b = A_sm.alloc([P, P], BF16, "onesb")
    mfb = A_sm.alloc([P, 2, P], BF16, "mfb")
    mods = [A_sm.alloc([P, 96], F32, "mods%d" % i) for i in range(DEPTH)]
    coef = [A_sm.alloc([P, 6, DT], F32, "coef%d" % i) for i in range(DEPTH)]
    csil = A_sm.alloc([P, DT], F32, "csil")
    epsb = A_sm.alloc([P, 1], F32, "epsb")
    oneb = A_sm.alloc([P, 1], F32, "oneb")
    lruF = A_sm.alloc([P, 32], F32, "lruF")
    rt = A_sm.alloc([P, DT, NEXP], F32, "rt")
    lg = A_sm.alloc([P, NCH, NEXP], F32, "lg")
    mx = A_sm.alloc([P, NCH, 8], F32, "mx")
    gt = A_sm.alloc([P, NCH, 4], F32, "gt")
    cb = A_sm.alloc([P, NCH, NEXP], F32, "cb")
    ident_f, Mf, Mb, ones_f = cst[:, 0, :], cst[:, 1, :], cst[:, 2, :], cst[:, 3, :]

    hT = A_h.alloc([P, DT, T], BF16, "hT")
    ring = [A_ra.alloc([P, DT, 512], BF16, "ring%d" % i) for i in range(2)]
    ymix = A_u.alloc([P, DT, T], BF16, "ymix")
    rstd = A_nt.alloc([P, T], F32, "rstd")
    tmpf = [A_nt.alloc([P, T], F32, "tmpf%d" % i) for i in range(3)]
    sqb = [A_nt.alloc([P, T], BF16, "sqb%d" % i) for i in range(2)]
    msk = A_nt.alloc([P, 3, T], BF16, "msk")
    psb = [fw.ps([P, 512], F32, "psb%d" % i) for i in range(8)]
    PSK = ['ps%d' % i for i in range(8)]

    def pcol(name, j=0, w=1):
        o, _ = PAR_OFF[name]
        return par[:, o + j:o + j + w]

    def V(fn, reads=(), writes=()):
        fw.op('dve', fn, reads, writes)

    def Aop(fn, reads=(), writes=()):
        fw.op('act', fn, reads, writes)

    def MM(out, lhsT, rhs, start, stop, reads, writes, inc=None):
        fw.op('pe', lambda e: e.matmul(out, lhsT, rhs, start=start, stop=stop), reads, writes,
              inc=(stop if inc is None else inc))

    fw.dma('sp', par[:], par_in, writes=['par'])
    for q in range(4):
        fw.dma('sp', cst[:, q, :], cst_in[:, q, :], writes=['cst'])
    for q in range(3):
        fw.dma('sp', msk[:, q, :], msk_in[:, q, :], writes=['msk'])
    for kt in range(DT):
        fw.dma('sp', rt[:, kt, :], rt_in[kt * P:(kt + 1) * P, :], writes=['rt'])
    V(lambda e: e.tensor_copy(out=ident_b[:], in_=ident_f), ['cst'], ['identb'])
    V(lambda e: e.memset(ones_b[:], 1.0), [], ['onesb'])
    V(lambda e: e.tensor_copy(out=mfb[:], in_=cst[:, 1:3, :]), ['cst'], ['mfb'])
    V(lambda e: e.memset(epsb[:], EPS), [], ['epsb'])
    V(lambda e: e.memset(oneb[:], 1.0), [], ['oneb'])
    Aop(lambda e: e.activation(out=csil[:], in_=pcol('cvec', 0, DT), func=AF.Silu), ['par'], ['csil'])

    MC = 384
    mwb = [A_x.alloc([P, DT, MC], F32, "mwb%d" % i) for i in range(2)]
    kk = 0
    for i in range(DEPTH):
        for ch in range(6 * D // MC):
            bb = kk % 2
            kk += 1
            for kt in range(DT):
                fw.dma('sp', mwb[bb][:, kt, :], mod_w[i, kt * P:(kt + 1) * P, ch * MC:(ch + 1) * MC], writes=[('mwb', bb, kt)])
            for mm in range(MC // P):
                m = ch * (MC // P) + mm
                for kt in range(DT):
                    MM(psb[0][:, m:m + 1], mwb[bb][:, kt, mm * P:(mm + 1) * P], csil[:, kt:kt + 1], kt == 0, kt == DT - 1,
                       [('mwb', bb, kt), 'csil'], ['ps0'])
        V(lambda e, i=i: e.tensor_tensor(out=mods[i][:], in0=psb[0][:, 0:96], in1=pcol('modb%d' % i, 0, 96), op=ALU.add),
          ['par'], [('mods', i), 'ps0'])
        for half, (ng, sh_i, sc_i, g_i) in enumerate([('n1g%d' % i, 0, 1, 2), ('n2g%d' % i, 3, 4, 5)]):
            V(lambda e, i=i, half=half, ng=ng, sc_i=sc_i: e.scalar_tensor_tensor(
                out=coef[i][:, 3 * half + 0, :], in0=mods[i][:, sc_i * DT:(sc_i + 1) * DT], scalar=1.0,
                in1=pcol(ng, 0, DT), op0=ALU.add, op1=ALU.mult), [('mods', i), 'par'], ['coefs'])
            V(lambda e, i=i, half=half, sh_i=sh_i: e.tensor_copy(
                out=coef[i][:, 3 * half + 1, :], in_=mods[i][:, sh_i * DT:(sh_i + 1) * DT]), [('mods', i)], ['coefs'])
            V(lambda e, i=i, half=half, g_i=g_i: e.tensor_copy(
                out=coef[i][:, 3 * half + 2, :], in_=mods[i][:, g_i * DT:(g_i + 1) * DT]), [('mods', i)], ['coefs'])
    fw.barrier()
    A_x.reset()
    xT = A_x.alloc([P, DT, T], F32, "xT")
    for dt in range(DT):
        fw.dma('sp', xT[:, dt, :], xT_in[dt * P:(dt + 1) * P, :], writes=[('x', dt)])

    def rms_stats(src_fn, ntiles, key_fn, width):
        for j in range(ntiles):
            bb = j % 2
            Aop(lambda e, j=j, bb=bb: e.activation(out=sqb[bb][:], in_=src_fn(j), func=AF.Square), [key_fn(j)], [('sqb', bb)])
            for tc in range(2):
                MM(psb[tc][:], ones_b[:], sqb[bb][:, tc * 512:(tc + 1) * 512], j == 0, j == ntiles - 1,
                   [('sqb', bb), 'onesb'], [PSK[tc]], inc=True)
        for tc in range(2):
            Aop(lambda e, tc=tc: e.activation(out=tmpf[0][:, tc * 512:(tc + 1) * 512], in_=psb[tc][:], func=AF.Sqrt,
                                              scale=1.0 / width, bias=epsb[:]), ['epsb'], [('tmpf', 0), PSK[tc]])
        V(lambda e: e.reciprocal(out=rstd[:], in_=tmpf[0][:]), [('tmpf', 0)], ['rstd'])

    def norm_to_hT(i, half):
        rms_stats(lambda j: xT[:, j, :], DT, lambda j: ('x', j), D)
        for dt in range(DT):
            bb = 1 + dt % 2
            V(lambda e, dt=dt, bb=bb: e.tensor_tensor(out=tmpf[bb][:], in0=xT[:, dt, :], in1=rstd[:], op=ALU.mult),
              [('x', dt), 'rstd'], [('tmpf', bb)])
            V(lambda e, dt=dt, bb=bb: e.tensor_scalar(out=hT[:, dt, :], in0=tmpf[bb][:], scalar1=coef[i][:, 3 * half, dt:dt + 1],
                                                      scalar2=coef[i][:, 3 * half + 1, dt:dt + 1], op0=ALU.mult, op1=ALU.add),
              [('tmpf', bb), 'coefs'], [('h', dt)])

    rstate = {'k': 0}

    lq = {'items': [], 'pos': 0, 'slots': []}

    def _issue_load(k):
        if k < len(lq['items']) and k == len(lq['slots']):
            src2d, c0, ncols = lq['items'][k]
            s = ring[(lq['base'] + k) % 2]
            for kt in range(DT):
                fw.dma('pool', s[:, kt, 0:ncols], src2d[kt * P:(kt + 1) * P, c0:c0 + ncols], writes=[('rg', id(s), kt)])
            lq['slots'].append(s)

    def plan_loads(items):
        lq['items'], lq['pos'], lq['slots'], lq['base'] = list(items), 0, [], rstate['k']
        rstate['k'] += len(items)
        _issue_load(0)

    def load_cols(src2d, c0, ncols, prefetch=True):
        k = lq['pos']
        assert lq['items'][k][1] == c0 and lq['items'][k][2] == ncols, (k, lq['items'][k], c0, ncols)
        lq['pos'] += 1
        _issue_load(k)
        if prefetch:
            _issue_load(k + 1)
        return lq['slots'][k]

    def proj_fm(wslot, cofs, m, pi):
        for tc in range(2):
            for kt in range(DT):
                MM(psb[pi + tc][0:m, :], wslot[:, kt, cofs:cofs + m], hT[:, kt, tc * 512:(tc + 1) * 512], kt == 0, kt == DT - 1,
                   [('rg', id(wslot), kt), ('h', kt)], [PSK[pi + tc]])

    def proj_tm(wslot, cofs, n, tt, pi):
        for kt in range(DT):
            MM(psb[pi][:, 0:n], hT[:, kt, tt * P:(tt + 1) * P], wslot[:, kt, cofs:cofs + n], kt == 0, kt == DT - 1,
               [('rg', id(wslot), kt), ('h', kt)], [PSK[pi]])

    def evac_fm(pi, dst_fn, wkeys, func=AF.Copy, m=P):
        for tc in range(2):
            Aop(lambda e, tc=tc: e.activation(out=dst_fn(tc), in_=psb[pi + tc][0:m, :], func=func), [], list(wkeys) + [PSK[pi + tc]])

    def conv_tile(skey, dst_fn, cw, cb_, j):
        src, acc, tmp = tmpf[0], tmpf[1], tmpf[2]
        V(lambda e: e.tensor_scalar(out=acc[:], in0=src[:], scalar1=pcol(cw, 4 * j + 1), scalar2=pcol(cb_, j), op0=ALU.mult, op1=ALU.add),
          [skey, 'par'], [('tmpf', 1)])
        for (k, lo, hi, sh, mi) in [(0, 1, T, -1, 0), (2, 0, T - 1, 1, 1), (3, 0, T - 2, 2, 2)]:
            V(lambda e, lo=lo, hi=hi, sh=sh, mi=mi: e.tensor_tensor(out=tmp[:, lo:hi], in0=src[:, lo + sh:hi + sh], in1=msk[:, mi, lo:hi], op=ALU.mult),
              [skey, 'msk'], [('tmpf', 2)])
            V(lambda e, lo=lo, hi=hi, k=k: e.scalar_tensor_tensor(out=acc[:, lo:hi], in0=tmp[:, lo:hi], scalar=pcol(cw, 4 * j + k),
                                                                  in1=acc[:, lo:hi], op0=ALU.mult, op1=ALU.add),
              [('tmpf', 2), 'par'], [('tmpf', 1)])
        dst_fn(acc)

    def lru_mixer(i):
        A_mix.reset()
        if STOP_AFTER:
            plan_loads([q_ for j_ in range(4) for q_ in [(in_w[i], C_XB + j_ * P, P), (in_w[i], C_GB + j_ * P, P)]])
        inw = in_w[i]
        wbd = A_mix.alloc([P, 2, 2, P], BF16, "lruw")
        xr = A_mix.alloc([P, T], F32, "xr")
        xrb = A_mix.alloc([P, T], BF16, "xrb")
        gl = A_mix.alloc([P, T], F32, "gl")
        gx = A_mix.alloc([P, T], F32, "gx")
        hs = A_mix.alloc([P, T], F32, "hs")
        rr = A_mix.alloc([P, T], F32, "rr")
        ii = A_mix.alloc([P, T], F32, "ii")
        aa = A_mix.alloc([P, T], F32, "aa")
        uu = A_mix.alloc([P, T], F32, "uu")
        hh = A_mix.alloc([P, T], F32, "hh")
        cc = A_mix.alloc([P, 2], F32, "cc")
        for j in range(4):
            wx = load_cols(inw, C_XB + j * P, P)
            for d in range(2):
                for ax in range(2):
                    fw.dma('pool', wbd[:, d, ax, :], lw_in[i, d, ax, j], writes=[('lruw', d, ax)])
            proj_fm(wx, 0, P, 2)
            evac_fm(2, lambda tc: tmpf[0][:, tc * 512:(tc + 1) * 512], [('tmpf', 0)])

            def fin(acc):
                V(lambda e: e.tensor_copy(out=xr[:], in_=acc[:]), [('tmpf', 1)], ['xr'])
                V(lambda e: e.tensor_copy(out=xrb[:], in_=acc[:]), [('tmpf', 1)], ['xrb'])
            conv_tile(('tmpf', 0), fin, 'lru_cw%d' % i, 'lru_cb%d' % i, j)
            wg = load_cols(inw, C_GB + j * P, P)
            proj_fm(wg, 0, P, 4)
            evac_fm(4, lambda tc: gx[:, tc * 512:(tc + 1) * 512], ['gx'])
            V(lambda e: e.tensor_tensor(out=gl[:], in0=gx[:], in1=gx[:], op=ALU.mult), ['gx'], ['gl'])
            V(lambda e: e.tensor_scalar(out=gl[:], in0=gl[:], scalar1=0.044715, scalar2=1.0, op0=ALU.mult, op1=ALU.add), [], ['gl'])
            V(lambda e: e.tensor_tensor(out=gl[:], in0=gl[:], in1=gx[:], op=ALU.mult), ['gx'], ['gl'])
            Aop(lambda e: e.activation(out=gl[:], in_=gl[:], func=AF.Sigmoid, scale=1.5957691216057308), [], ['gl'])
            V(lambda e: e.tensor_tensor(out=gl[:], in0=gl[:], in1=gx[:], op=ALU.mult), ['gx'], ['gl'])
            for d in range(2):
                Aop(lambda e, d=d: e.activation(out=cc[:, 0:1], in_=pcol('lru_lam%d%d' % (i, d), j), func=AF.Exp, scale=-1.0), ['par'], ['cc'])
                Aop(lambda e: e.activation(out=cc[:, 0:1], in_=cc[:, 0:1], func=AF.Ln, bias=oneb[:]), ['oneb'], ['cc'])
                V(lambda e: e.tensor_scalar(out=cc[:, 1:2], in0=cc[:, 0:1], scalar1=-8.0, scalar2=None, op0=ALU.mult), [], ['cc'])
                for ax, (dst, dk, bn, ps0) in enumerate([(rr, 'rr', 'lru_ba%d%d' % (i, d), 2), (ii, 'ii', 'lru_bx%d%d' % (i, d), 4)]):
                    for tc in range(2):
                        MM(psb[ps0 + tc][:], wbd[:, d, ax, :], xrb[:, tc * 512:(tc + 1) * 512], True, True,
                           [('lruw', d, ax), 'xrb'], [PSK[ps0 + tc]])
                        Aop(lambda e, tc=tc, dst=dst, bn=bn, ps0=ps0: e.activation(
                            out=dst[:, tc * 512:(tc + 1) * 512], in_=psb[ps0 + tc][:], func=AF.Sigmoid, bias=pcol(bn, j)),
                            ['par'], [dk, PSK[ps0 + tc]])
                Aop(lambda e: e.activation(out=aa[:], in_=rr[:], func=AF.Exp, scale=cc[:, 1:2]), ['rr', 'cc'], ['aa'])
                V(lambda e: e.tensor_tensor(out=uu[:], in0=aa[:], in1=aa[:], op=ALU.mult), ['aa'], ['uu'])
                V(lambda e: e.tensor_scalar(out=uu[:], in0=uu[:], scalar1=-1.0, scalar2=1.0, op0=ALU.mult, op1=ALU.add), [], ['uu'])
                V(lambda e: e.tensor_scalar(out=uu[:], in0=uu[:], scalar1=0.0, scalar2=None, op0=ALU.max), [], ['uu'])
                Aop(lambda e: e.activation(out=uu[:], in_=uu[:], func=AF.Sqrt), [], ['uu'])
                V(lambda e: e.tensor_tensor(out=uu[:], in0=uu[:], in1=ii[:], op=ALU.mult), ['ii'], ['uu'])
                V(lambda e: e.tensor_tensor(out=uu[:], in0=uu[:], in1=xr[:], op=ALU.mult), ['xr'], ['uu'])
                av = aa[:].rearrange("p (s l) -> p s l", l=256)
                if d == 0:
                    V(lambda e: e.tensor_scalar(out=av[:, 1:4, 0:1], in0=av[:, 1:4, 0:1], scalar1=pcol('flag'), scalar2=None, op0=ALU.mult),
                      ['par'], ['aa'])
                    V(lambda e, d=d: e.tensor_tensor_scan(out=hh[:], data0=aa[:], data1=uu[:], initial=pcol('lru_h0%d%d' % (i, d), j),
                                                          op0=ALU.mult, op1=ALU.add), ['aa', 'uu', 'par'], ['hh'])
                    V(lambda e: e.tensor_copy(out=hs[:], in_=hh[:]), ['hh'], ['hs'])
                else:
                    V(lambda e: e.tensor_scalar(out=av[:, 0:3, 255:256], in0=av[:, 0:3, 255:256], scalar1=pcol('flag'), scalar2=None, op0=ALU.mult),
                      ['par'], ['aa'])
                    V(lambda e, d=d: e.tensor_tensor_scan(out=hh[:, ::-1], data0=aa[:, ::-1], data1=uu[:, ::-1],
                                                          initial=pcol('lru_h0%d%d' % (i, d), j), op0=ALU.mult, op1=ALU.add),
                      ['aa', 'uu', 'par'], ['hh'])
                    V(lambda e: e.tensor_tensor(out=hs[:], in0=hs[:], in1=hh[:], op=ALU.add), ['hh'], ['hs'])
                hv = hh[:].rearrange("p (s l) -> p s l", l=256)
                col = 255 if d == 0 else 0
                fv = lruF[:].rearrange("p (j s d) -> p j s d", j=4, s=4)
                V(lambda e, d=d, col=col, j=j: e.tensor_copy(out=fv[:, j, :, d:d + 1], in_=hv[:, :, col:col + 1]), ['hh'], ['lruF'])
            V(lambda e, j=j: e.tensor_tensor(out=ymix[:, 12 + j, :], in0=hs[:], in1=gl[:], op=ALU.mult), ['hs', 'gl'], [('ym', 12 + j)])
        fw.dma('sp', o_lru[i], lruF[:], reads=['lruF'])

    def gla_mixer(i):
        A_mix.reset()
        if STOP_AFTER:
            plan_loads([(in_w[i], C_Q, 512), (in_w[i], C_G, 512), (in_w[i], C_V, 512), (in_w[i], C_GRAW, 16)])
        inw = in_w[i]
        qT = A_mix.alloc([P, 2, T], F32, "qT")
        kT = A_mix.alloc([P, 2, T], F32, "kT")
        vtm = A_mix.alloc([P, NCH, 512], BF16, "vtm")
        gT_ = A_mix.alloc([P, 4, T], BF16, "gTs")
        grA = A_mix.alloc([32, T], F32, "grA")
        gw = A_mix.alloc([32, 256], F32, "gw")
        ltm = A_mix.alloc([P, NCH, 256], F32, "ltm")
        oT = A_mix.alloc([P, 4, T], F32, "oT")
        eb = A_mix.alloc([P, 2, P], F32, "eb")
        enb = A_mix.alloc([P, 2, P], F32, "enb")
        qt = A_mix.alloc([P, 2, P], BF16, "qt")
        kt_ = A_mix.alloc([P, 2, P], BF16, "kt")
        qtm = A_mix.alloc([P, 4, P], BF16, "qtm")
        ktm = A_mix.alloc([P, 256], BF16, "ktm")
        atT = A_mix.alloc([P, 4, P], BF16, "atT")
        S = A_mix.alloc([P, 2, P], F32, "S")
        Sb = A_mix.alloc([P, 2, P], BF16, "Sb")
        stg = [A_mix.alloc([P, 2, P], F32, "stg%d" % q) for q in range(2)]
        w = load_cols(inw, C_Q, 512)
        for t2 in range(2):
            for (dst, cofs, nm) in [(qT, 0, 'qT'), (kT, 256, 'kT')]:
                proj_fm(w, cofs + t2 * P, P, 2)
                evac_fm(2, lambda tc, dst=dst, t2=t2: dst[:, t2, tc * 512:(tc + 1) * 512], [nm])
        w = load_cols(inw, C_G, 512)
        for t4 in range(4):
            proj_fm(w, t4 * P, P, 4)
            evac_fm(4, lambda tc, t4=t4: gT_[:, t4, tc * 512:(tc + 1) * 512], [('gTs', t4)], func=AF.Silu)
        w = load_cols(inw, C_V, 512)
        for tt in range(NCH):
            pi = 6 + tt % 2
            proj_tm(w, 0, 512, tt, pi)
            V(lambda e, tt=tt, pi=pi: e.tensor_copy(out=vtm[:, tt, :], in_=psb[pi][:]), [], [('vtm', tt), PSK[pi]])
        w = load_cols(inw, C_GRAW, 16)
        V(lambda e: e.memset(grA[:], 1.0), [], ['grA'])
        proj_fm(w, 0, 16, 2)
        evac_fm(2, lambda tc: grA[0:16, tc * 512:(tc + 1) * 512], ['grA'], m=16)
        V(lambda e: e.memset(oT[:], 0.0), [], ['oT'])
        V(lambda e: e.memset(qtm[:], 0.0), [], ['qtm'])
        if STOPX == 1:
            return
        for d in range(2):
            fw.dma('sp', gw[:], gw_in[i, d], writes=['gw'])
            for tt in range(NCH):
                pi = 6 + tt % 2
                MM(psb[pi][:, 0:256], grA[0:32, tt * P:(tt + 1) * P], gw[0:32, :], True, True, ['grA', 'gw'], [PSK[pi]])
                Aop(lambda e, tt=tt, pi=pi: e.activation(out=ltm[:, tt, :], in_=psb[pi][:, 0:256], func=AF.Exp, scale=-1.0), [], [('ltm', tt), PSK[pi]])
                Aop(lambda e, tt=tt: e.activation(out=ltm[:, tt, :], in_=ltm[:, tt, :], func=AF.Ln, bias=oneb[:]), ['oneb'], [('ltm', tt)])
            if STOPX == 2:
                return
            fw.dma('sp', S[:].rearrange("p a b -> p (a b)"), gla_h0[i, d], writes=['S'])
            V(lambda e: e.tensor_copy(out=Sb[:], in_=S[:]), ['S'], ['Sb'])
            Mm = Mf if d == 0 else Mb
            order = list(range(NCH)) if d == 0 else list(range(NCH - 1, -1, -1))
            for step, c in enumerate(order):
                ts = slice(c * P, (c + 1) * P)
                last = P - 1 if d == 0 else 0
                if step > 0 and step % 2 == 0:
                    V(lambda e: e.tensor_scalar(out=S[:], in0=S[:], scalar1=pcol('flag'), scalar2=None, op0=ALU.mult), ['par'], ['S'])
                    V(lambda e: e.tensor_copy(out=Sb[:], in_=S[:]), ['S'], ['Sb'])
                for t2 in range(2):
                    MM(psb[2][:, t2 * P:(t2 + 1) * P], ltm[:, c, t2 * P:(t2 + 1) * P], Mm, True, True, [('ltm', c), 'cst'], [PSK[2]])
                Aop(lambda e: e.activation(out=eb[:].rearrange("p a b -> p (a b)"), in_=psb[2][:, 0:256], func=AF.Exp, scale=-1.0 / 16), [], ['eb', PSK[2]])
                Aop(lambda e: e.activation(out=enb[:].rearrange("p a b -> p (a b)"), in_=psb[2][:, 0:256], func=AF.Exp, scale=1.0 / 16), [], ['enb', PSK[2]])
                V(lambda e, ts=ts: e.scalar_tensor_tensor(out=qt[:], in0=qT[:, :, ts], scalar=0.125, in1=eb[:], op0=ALU.mult, op1=ALU.mult),
                  ['qT', 'eb'], ['qt'])
                V(lambda e, ts=ts: e.tensor_tensor(out=kt_[:], in0=kT[:, :, ts], in1=enb[:], op=ALU.mult), ['kT', 'enb'], ['kt'])
                for h in range(4):
                    rs = slice((h % 2) * 64, (h % 2) * 64 + 64)
                    V(lambda e, h=h, rs=rs: e.tensor_copy(out=qtm[rs, h, :], in_=qt[rs, h // 2, :]), ['qt'], ['qtm'])
                if STOPX == 3:
                    return
                for t2 in range(2):
                    MM(psb[3][:, t2 * P:(t2 + 1) * P], kt_[:, t2, :], ident_b[:], True, True, ['kt', 'identb'], [PSK[3]])
                V(lambda e: e.tensor_copy(out=ktm[:], in_=psb[3][:, 0:256]), [], ['ktm', PSK[3]])
                for h in range(4):
                    rs = slice((h % 2) * 64, (h % 2) * 64 + 64)
                    MM(psb[4][:, h * P:(h + 1) * P], kt_[:, h // 2, :], qtm[:, h, :], True, True, ['kt', 'qtm'], [PSK[4]])
                V(lambda e, d=d: e.tensor_tensor(out=atT[:], in0=psb[4][:].rearrange("p (h t) -> p h t", h=4),
                                                 in1=mfb[:, d, :].unsqueeze(1).to_broadcast([P, 4, P]), op=ALU.mult), ['mfb'], ['atT', PSK[4]])
                if STOPX == 4:
                    return
                for h in range(4):
                    rs = slice((h % 2) * 64, (h % 2) * 64 + 64)
                    MM(psb[5][:, h * P:(h + 1) * P], vtm[:, c, h * P:(h + 1) * P], atT[:, h, :], True, False, [('vtm', c), 'atT'], [PSK[5]], inc=False)
                    MM(psb[5][:, h * P:(h + 1) * P], Sb[:, h // 2, :], qtm[:, h, :], False, True, ['Sb', 'qtm'], [PSK[5]], inc=True)
                V(lambda e, ts=ts: e.tensor_tensor(out=oT[:, :, ts], in0=oT[:, :, ts], in1=psb[5][:].rearrange("p (h t) -> p h t", h=4), op=ALU.add),
                  [], ['oT', PSK[5]])
                if STOPX == 5:
                    return
                for t2 in range(2):
                    MM(psb[6][:, 0:256], ktm[:, t2 * P:(t2 + 1) * P], vtm[:, c, t2 * 256:(t2 + 1) * 256], True, True, ['ktm', ('vtm', c)], [PSK[6]])
                    for hl in range(2):
                        rs = slice(hl * 64, hl * 64 + 64)
                        V(lambda e, t2=t2, hl=hl, rs=rs: e.tensor_tensor(out=S[rs, t2, :], in0=S[rs, t2, :], in1=psb[6][rs, hl * P:(hl + 1) * P], op=ALU.add),
                          [], ['S', PSK[6]])
                    V(lambda e, t2=t2, last=last: e.tensor_scalar(out=S[:, t2, :], in0=S[:, t2, :], scalar1=eb[:, t2, last:last + 1], scalar2=None, op0=ALU.mult),
                      ['eb'], ['S'])
                V(lambda e: e.tensor_copy(out=Sb[:], in_=S[:]), ['S'], ['Sb'])
                if step % 2 == 1:
                    seg = c // 2
                    q = (step // 2) % 2
                    V(lambda e, q=q: e.tensor_copy(out=stg[q][:], in_=S[:]), ['S'], [('stg', q)])
                    fw.dma('sp', o_gla[seg, i, d], stg[q][:].rearrange("p a b -> p (a b)"), reads=[('stg', q)])
        for h in range(4):
            bb = h % 2
            Aop(lambda e, h=h, bb=bb: e.activation(out=sqb[bb][:], in_=oT[:, h, :], func=AF.Square), ['oT'], [('sqb', bb)])
            for tc in range(2):
                MM(psb[tc][:], ones_b[:], sqb[bb][:, tc * 512:(tc + 1) * 512], True, True, [('sqb', bb), 'onesb'], [PSK[tc]])
                Aop(lambda e, tc=tc: e.activation(out=tmpf[0][:, tc * 512:(tc + 1) * 512], in_=psb[tc][:], func=AF.Sqrt,
                                                  scale=1.0 / P, bias=epsb[:]), ['epsb'], [('tmpf', 0), PSK[tc]])
            V(lambda e: e.reciprocal(out=tmpf[1][:], in_=tmpf[0][:]), [('tmpf', 0)], [('tmpf', 1)])
            V(lambda e, h=h: e.tensor_tensor(out=tmpf[2][:], in0=oT[:, h, :], in1=tmpf[1][:], op=ALU.mult), ['oT', ('tmpf', 1)], [('tmpf', 2)])
            V(lambda e, h=h: e.scalar_tensor_tensor(out=ymix[:, 8 + h, :], in0=tmpf[2][:], scalar=pcol('gla_ng%d' % i), in1=gT_[:, h, :],
                                                    op0=ALU.mult, op1=ALU.mult), [('tmpf', 2), 'par', ('gTs', h)], [('ym', 8 + h)])

    def ssd_mixer(i):
        A_mix.reset()
        if STOP_AFTER:
            plan_loads([(in_w[i], C_XBC + j_ * P, P) for j_ in range(12)] + [(in_w[i], C_DT, 16), (in_w[i], C_Z, 512), (in_w[i], C_Z + 512, 512)])
        inw = in_w[i]
        xtm = A_mix.alloc([P, NCH, 1024], BF16, "xtm")
        BCT = A_mix.alloc([P, 4, T], BF16, "BCT")
        Btm = A_mix.alloc([P, NCH, 256], BF16, "Btm")
        yf = A_mix.alloc([P, NCH, 1024], BF16, "yf")
        dtr = A_mix.alloc([P, NCH, 16], F32, "dtr")
        dtd = A_mix.alloc([P, NCH, 16], F32, "dtd")
        ad = A_mix.alloc([P, NCH, 16], F32, "ad")
        nA = A_mix.alloc([P, 16], F32, "nA")
        abc = A_mix.alloc([P, 16, P], F32, "abc")
        DTm = A_mix.alloc([P, 16, P], F32, "DTm")
        WT = A_mix.alloc([P, 16, P], BF16, "WT")
        ST = A_mix.alloc([P, 1024], F32, "ST")
        STb = A_mix.alloc([P, 1024], BF16, "STb")
        sm = A_mix.alloc([P, 4, 16], F32, "ssm")
        rrep, yc, stg = tmpf[1], tmpf[2], tmpf[0]
        xdt, xdd = sqb[0], sqb[1]
        for j in range(12):
            w = load_cols(inw, C_XBC + j * P, P)
            proj_fm(w, 0, P, 2)
            evac_fm(2, lambda tc: tmpf[0][:, tc * 512:(tc + 1) * 512], [('tmpf', 0)])
            if j < 8:
                def fin(acc, j=j):
                    Aop(lambda e: e.activation(out=sqb[0][:], in_=acc[:], func=AF.Silu), [('tmpf', 1)], [('sqb', 0)])
                    for c in range(NCH):
                        pi = 4 + c % 2
                        MM(psb[pi][:, 0:P], sqb[0][:, c * P:(c + 1) * P], ident_b[:], True, True, [('sqb', 0), 'identb'], [PSK[pi]])
                        V(lambda e, c=c, pi=pi: e.tensor_copy(out=xtm[:, c, j * P:(j + 1) * P], in_=psb[pi][:, 0:P]), [], [('xtm', c), PSK[pi]])
            else:
                def fin(acc, j=j):
                    Aop(lambda e: e.activation(out=BCT[:, j - 8, :], in_=acc[:], func=AF.Silu), [('tmpf', 1)], [('BCT', j - 8)])
                    if j < 10:
                        for c in range(NCH):
                            pi = 4 + c % 2
                            MM(psb[pi][:, 0:P], BCT[:, j - 8, c * P:(c + 1) * P], ident_b[:], True, True, [('BCT', j - 8), 'identb'], [PSK[pi]])
                            V(lambda e, c=c, pi=pi: e.tensor_copy(out=Btm[:, c, (j - 8) * P:(j - 7) * P], in_=psb[pi][:, 0:P]), [], [('Btm', c), PSK[pi]])
            conv_tile(('tmpf', 0), fin, 'ssd_cw%d' % i, 'ssd_cb%d' % i, j)
        w = load_cols(inw, C_DT, 16)
        for tt in range(NCH):
            pi = 6 + tt % 2
            proj_tm(w, 0, 16, tt, pi)
            V(lambda e, tt=tt, pi=pi: e.tensor_copy(out=dtr[:, tt, :], in_=psb[pi][:, 0:16]), [], ['dtr', PSK[pi]])
        for d in (1, 0):
            if d == 0:
                wz = [load_cols(inw, C_Z, 512), load_cols(inw, C_Z + 512, 512, prefetch=False)]
            V(lambda e, d=d: e.tensor_tensor(out=dtd[:], in0=dtr[:], in1=pcol('ssd_dtb%d%d' % (i, d), 0, 16).unsqueeze(1).to_broadcast([P, NCH, 16]), op=ALU.add),
              ['dtr', 'par'], ['dtd'])
            Aop(lambda e: e.activation(out=dtd[:], in_=dtd[:], func=AF.Exp), [], ['dtd'])
            Aop(lambda e: e.activation(out=dtd[:], in_=dtd[:], func=AF.Ln, bias=oneb[:]), ['oneb'], ['dtd'])
            Aop(lambda e, d=d: e.activation(out=nA[:], in_=pcol('ssd_alog%d%d' % (i, d), 0, 16), func=AF.Exp), ['par'], ['nA'])
            V(lambda e: e.scalar_tensor_tensor(out=ad[:], in0=dtd[:], scalar=-1.0, in1=nA[:].unsqueeze(1).to_broadcast([P, NCH, 16]), op0=ALU.mult, op1=ALU.mult),
              ['dtd', 'nA'], ['ad'])
            fw.dma('sp', ST[:], ssd_h0[i, d], writes=['ST'])
            V(lambda e: e.tensor_copy(out=STb[:], in_=ST[:]), ['ST'], ['STb'])
            Mm = Mf if d == 0 else Mb
            order = list(range(NCH)) if d == 0 else list(range(NCH - 1, -1, -1))
            for step, c in enumerate(order):
                ts = slice(c * P, (c + 1) * P)
                if step > 0 and step % 2 == 0:
                    V(lambda e: e.tensor_scalar(out=ST[:], in0=ST[:], scalar1=pcol('flag'), scalar2=None, op0=ALU.mult), ['par'], ['ST'])
                    V(lambda e: e.tensor_copy(out=STb[:], in_=ST[:]), ['ST'], ['STb'])
                for hf in range(2):
                    V(lambda e, hf=hf, c=c: e.tensor_tensor(out=rrep[:].rearrange("p (h t) -> p h t", h=8),
                                                            in0=ad[:, c, hf * 8:(hf + 1) * 8].unsqueeze(2).to_broadcast([P, 8, P]),
                                                            in1=ident_f.unsqueeze(1).to_broadcast([P, 8, P]), op=ALU.mult), ['ad', 'cst'], [('tmpf', 1)])
                    for q in range(2):
                        MM(psb[2 + q][:], ones_f, rrep[:, q * 512:(q + 1) * 512], True, True, [('tmpf', 1), 'cst'], [PSK[2 + q]])
                        Aop(lambda e, hf=hf, q=q: e.activation(out=abc[:, hf * 8 + q * 4:hf * 8 + q * 4 + 4, :].rearrange("p h t -> p (h t)"),
                                                               in_=psb[2 + q][:], func=AF.Exp), [], ['abc', PSK[2 + q]])
                MM(psb[4][:, 0:16], Mm, ad[:, c, :], True, True, ['cst', 'ad'], [PSK[4]])
                Aop(lambda e: e.activation(out=sm[:, 3, :], in_=psb[4][:, 0:16], func=AF.Exp), [], ['sm3', PSK[4]])
                MM(psb[4][:, 16:32], ones_f, ad[:, c, :], True, True, ['cst', 'ad'], [PSK[4]])
                Aop(lambda e: e.activation(out=sm[:, 1, :], in_=psb[4][:, 16:32], func=AF.Exp), [], ['sm1', PSK[4]])
                edge = 0 if d == 0 else P - 1
                V(lambda e, edge=edge: e.memset(abc[:, :, edge:edge + 1], 0.0), [], ['abc'])
                a2 = abc[:].rearrange("p h t -> p (h t)")
                d2 = DTm[:].rearrange("p h t -> p (h t)")
                V(lambda e: e.tensor_copy(out=DTm[:], in_=ident_f.unsqueeze(1).to_broadcast([P, 16, P])), ['cst'], ['DTm'])
                if d == 0:
                    V(lambda e: e.tensor_tensor_scan(out=d2, data0=a2, data1=d2, initial=0.0, op0=ALU.mult, op1=ALU.add), ['abc'], ['DTm'])
                else:
                    V(lambda e: e.tensor_tensor_scan(out=d2[:, ::-1], data0=a2[:, ::-1], data1=d2[:, ::-1], initial=0.0, op0=ALU.mult, op1=ALU.add),
                      ['abc'], ['DTm'])
                for g in range(2):
                    MM(psb[5][:, g * P:(g + 1) * P], BCT[:, g, ts], BCT[:, 2 + g, ts], True, True, [('BCT', g), ('BCT', 2 + g)], [PSK[5]])
                for g in range(2):
                    V(lambda e, g=g: e.tensor_tensor(out=WT[:, g * 8:(g + 1) * 8, :], in0=DTm[:, g * 8:(g + 1) * 8, :],
                                                     in1=psb[5][:, g * P:(g + 1) * P].unsqueeze(1).to_broadcast([P, 8, P]), op=ALU.mult),
                      ['DTm'], ['WT', PSK[5]])
                V(lambda e, c=c: e.tensor_tensor(out=xdt[:].rearrange("p (h q) -> p h q", h=16), in0=xtm[:, c, :].rearrange("p (h q) -> p h q", h=16),
                                                 in1=dtd[:, c, :].unsqueeze(2).to_broadcast([P, 16, 64]), op=ALU.mult), [('xtm', c), 'dtd'], [('sqb', 0)])
                for h in range(16):
                    MM(psb[6 + h // 8][:, (h % 8) * 64:(h % 8) * 64 + 64], WT[:, h, :], xdt[:, h * 64:(h + 1) * 64], True, True,
                       ['WT', ('sqb', 0)], [PSK[6 + h // 8]])
                for g in range(2):
                    MM(psb[2 + g][:], BCT[:, 2 + g, ts], STb[:, g * 512:(g + 1) * 512], True, True, [('BCT', 2 + g), 'STb'], [PSK[2 + g]])
                for g in range(2):
                    V(lambda e, g=g: e.tensor_tensor(out=yc[:, g * 512:(g + 1) * 512].rearrange("p (h q) -> p h q", h=8),
                                                     in0=psb[2 + g][:].rearrange("p (h q) -> p h q", h=8),
                                                     in1=sm[:, 3, g * 8:(g + 1) * 8].unsqueeze(2).to_broadcast([P, 8, 64]), op=ALU.mult),
                      ['sm3'], [('tmpf', 2), PSK[2 + g]])
                    V(lambda e, g=g: e.tensor_tensor(out=yc[:, g * 512:(g + 1) * 512], in0=yc[:, g * 512:(g + 1) * 512], in1=psb[6 + g][:], op=ALU.add),
                      [], [('tmpf', 2), PSK[6 + g]])
                ecol = P - 1 if d == 0 else 0
                V(lambda e, ecol=ecol: e.tensor_tensor(out=xdd[:].rearrange("p (h q) -> p h q", h=16), in0=xdt[:].rearrange("p (h q) -> p h q", h=16),
                                                       in1=DTm[:, :, ecol:ecol + 1].to_broadcast([P, 16, 64]), op=ALU.mult), [('sqb', 0), 'DTm'], [('sqb', 1)])
                for g in range(2):
                    MM(psb[4 + g][:], Btm[:, c, g * P:(g + 1) * P], xdd[:, g * 512:(g + 1) * 512], True, True, [('Btm', c), ('sqb', 1)], [PSK[4 + g]])
                V(lambda e: e.tensor_tensor(out=ST[:].rearrange("p (h q) -> p h q", h=16), in0=ST[:].rearrange("p (h q) -> p h q", h=16),
                                            in1=sm[:, 1, :].unsqueeze(2).to_broadcast([P, 16, 64]), op=ALU.mult), ['sm1'], ['ST'])
                for g in range(2):
                    V(lambda e, g=g: e.tensor_tensor(out=ST[:, g * 512:(g + 1) * 512], in0=ST[:, g * 512:(g + 1) * 512], in1=psb[4 + g][:], op=ALU.add),
                      [], ['ST', PSK[4 + g]])
                V(lambda e: e.tensor_copy(out=STb[:], in_=ST[:]), ['ST'], ['STb'])
                if step % 2 == 1:
                    seg = c // 2
                    for q in range(8):
                        MM(psb[2 + q % 2][:, 0:P], ST[:, q * P:(q + 1) * P], ident_f, True, True, ['ST', 'cst'], [PSK[2 + q % 2]])
                        V(lambda e, q=q: e.tensor_copy(out=stg[:, q * P:(q + 1) * P], in_=psb[2 + q % 2][:, 0:P]), [], [('tmpf', 0), PSK[2 + q % 2]])
                    for q in range(8):
                        fw.dma('sp', o_ssd[seg, i, d, q * P:(q + 1) * P, :], stg[:, q * P:(q + 1) * P], reads=[('tmpf', 0)])
                if d == 1:
                    V(lambda e, c=c: e.tensor_copy(out=yf[:, c, :], in_=yc[:]), [('tmpf', 2)], [('yf', c)])
                else:
                    V(lambda e, c=c: e.tensor_tensor(out=yc[:], in0=yc[:], in1=yf[:, c, :], op=ALU.add), [('yf', c)], [('tmpf', 2)])
                    V(lambda e, c=c: e.tensor_tensor(out=xdd[:].rearrange("p (h q) -> p h q", h=16), in0=xtm[:, c, :].rearrange("p (h q) -> p h q", h=16),
                                                     in1=pcol('ssd_D%d' % i, 0, 16).unsqueeze(2).to_broadcast([P, 16, 64]), op=ALU.mult),
                      [('xtm', c), 'par'], [('sqb', 1)])
                    V(lambda e: e.tensor_tensor(out=yc[:], in0=yc[:], in1=xdd[:], op=ALU.add), [('sqb', 1)], [('tmpf', 2)])
                    for zq in range(2):
                        proj_tm(wz[zq], 0, 512, c, 2 + zq)
                        Aop(lambda e, zq=zq: e.activation(out=tmpf[0][:, zq * 512:(zq + 1) * 512], in_=psb[2 + zq][:], func=AF.Silu),
                            [], [('tmpf', 0), PSK[2 + zq]])
                    V(lambda e: e.tensor_tensor(out=yc[:], in0=yc[:], in1=tmpf[0][:], op=ALU.mult), [('tmpf', 0)], [('tmpf', 2)])
                    Aop(lambda e: e.activation(out=tmpf[0][:], in_=yc[:], func=AF.Square), [('tmpf', 2)], [('tmpf', 0)])
                    V(lambda e: e.reduce_sum(out=sm[:, 0, 0:1], in_=tmpf[0][:], axis=AX.X), [('tmpf', 0)], ['sm0'])
                    Aop(lambda e: e.activation(out=sm[:, 0, 1:2], in_=sm[:, 0, 0:1], func=AF.Sqrt, scale=1.0 / 1024, bias=epsb[:]), ['epsb'], ['sm0'])
                    V(lambda e: e.reciprocal(out=sm[:, 0, 2:3], in_=sm[:, 0, 1:2]), [], ['sm0'])
                    V(lambda e: e.tensor_scalar(out=xdd[:], in0=yc[:], scalar1=sm[:, 0, 2:3], scalar2=None, op0=ALU.mult), [('tmpf', 2), 'sm0'], [('sqb', 1)])
                    for q in range(8):
                        pi = 4 + q % 2
                        MM(psb[pi][:, 0:P], xdd[:, q * P:(q + 1) * P], ident_b[:], True, True, [('sqb', 1), 'identb'], [PSK[pi]])
                        V(lambda e, q=q, ts=ts, pi=pi: e.tensor_scalar(out=ymix[:, q, ts], in0=psb[pi][:, 0:P], scalar1=pcol('ssd_ng%d' % i, q),
                                                                       scalar2=None, op0=ALU.mult), ['par'], [('ym', q), PSK[pi]])

    def ffn_groups(groups, g2cols, slots, gT, silb):
        sl = [0]
        k0 = rstate['k']
        rstate['k'] += 3 * len(groups)

        def slot_of(gi, which):
            return slots[(k0 + 3 * gi + which) % 4]

        def issue_w1(gi):
            w1_, w3_, w2_, g0, J, _, _ = groups[gi]
            s1 = slot_of(gi, 0)
            for kt in range(DT):
                fw.dma('pool', s1[:, kt, 0:J * P], w1_[kt * P:(kt + 1) * P, g0 * P:(g0 + J) * P], writes=[('rg', id(s1), kt)])

        def issue_w3w2(gi):
            w1_, w3_, w2_, g0, J, _, _ = groups[gi]
            s3, s2 = slot_of(gi, 1), slot_of(gi, 2)
            for kt in range(DT):
                fw.dma('pool', s3[:, kt, 0:J * P], w3_[kt * P:(kt + 1) * P, g0 * P:(g0 + J) * P], writes=[('rg', id(s3), kt)])
            w2v = s2[:].rearrange("p a b -> p (a b)").rearrange("p (j d) -> p j d", j=4)
            for j in range(J):
                for q in range(4):
                    fw.dma('pool', w2v[:, j, q * 512:(q + 1) * 512], w2_[(g0 + j) * P:(g0 + j + 1) * P, q * 512:(q + 1) * 512],
                           writes=[('rg', id(s2), 4 * j + q)])

        issue_w1(0)
        issue_w3w2(0)
        for gi, (w1_, w3_, w2_, g0, J, comb_fn, pre_fn) in enumerate(groups):
            s1, s3, s2 = slot_of(gi, 0), slot_of(gi, 1), slot_of(gi, 2)
            w2v = s2[:].rearrange("p a b -> p (a b)").rearrange("p (j d) -> p j d", j=4)
            if gi + 1 < len(groups):
                issue_w1(gi + 1)
            if pre_fn is not None:
                pre_fn()
            for j in range(J):
                for tc in range(2):
                    a_, b_ = 2 + 2 * (sl[0] % 2), 3 + 2 * (sl[0] % 2)
                    sb_ = sl[0] % 2
                    sl[0] += 1
                    for kt in range(DT):
                        MM(psb[a_][:], s1[:, kt, j * P:(j + 1) * P], hT[:, kt, tc * 512:(tc + 1) * 512], kt == 0, kt == DT - 1,
                           [('rg', id(s1), kt), ('h', kt)], [PSK[a_]])
                    for kt in range(DT):
                        MM(psb[b_][:], s3[:, kt, j * P:(j + 1) * P], hT[:, kt, tc * 512:(tc + 1) * 512], kt == 0, kt == DT - 1,
                           [('rg', id(s3), kt), ('h', kt)], [PSK[b_]])
                    Aop(lambda e, a_=a_, sb_=sb_: e.activation(out=silb[sb_][:], in_=psb[a_][:], func=AF.Silu), [], [('silb', sb_), PSK[a_]])
                    if comb_fn is None:
                        V(lambda e, b_=b_, sb_=sb_, j=j, tc=tc: e.tensor_tensor(out=gT[:, j, tc * 512:(tc + 1) * 512], in0=silb[sb_][:], in1=psb[b_][:], op=ALU.mult),
                          [('silb', sb_)], [('gT', j, tc), PSK[b_]])
                    else:
                        V(lambda e, b_=b_, sb_=sb_: e.tensor_tensor(out=silb[sb_][:], in0=silb[sb_][:], in1=psb[b_][:], op=ALU.mult),
                          [], [('silb', sb_), PSK[b_]])
                        V(lambda e, sb_=sb_, j=j, tc=tc: e.tensor_tensor(out=gT[:, j, tc * 512:(tc + 1) * 512], in0=silb[sb_][:], in1=comb_fn(tc), op=ALU.mult),
                          [('silb', sb_), 'rstd'], [('gT', j, tc)])
            if gi + 1 < len(groups):
                issue_w3w2(gi + 1)
            for dt in range(DT):
                for tc in range(2):
                    o_ = (6, 7, 0, 1)[sl[0] % 4]
                    sl[0] += 1
                    for j in range(J):
                        MM(psb[o_][:], w2v[:, j, dt * P:(dt + 1) * P], gT[:, j, tc * 512:(tc + 1) * 512], j == 0, j == J - 1,
                           [('rg', id(s2), 4 * j + dt // 4), ('gT', j, tc)], [PSK[o_]])
                    V(lambda e, dt=dt, tc=tc, o_=o_: e.scalar_tensor_tensor(
                        out=xT[:, dt, tc * 512:(tc + 1) * 512], in0=psb[o_][:], scalar=g2cols[dt],
                        in1=xT[:, dt, tc * 512:(tc + 1) * 512], op0=ALU.mult, op1=ALU.add), ['coefs'], [('x', dt), PSK[o_]])

    def make_groups(w1_, w3_, w2_, FF, comb_fn=None, pre_fn=None):
        out = []
        for g0 in range(0, FF // P, 4):
            out.append((w1_, w3_, w2_, g0, min(4, FF // P - g0), comb_fn, pre_fn if g0 == 0 else None))
        return out

    def moe_layer(i, g2cols, slots, gTb, silb):
        comb = rstd
        cbT = tmpf[1]
        sel = tmpf[2]
        for dt in range(DT):
            bb = 1 + dt % 2
            V(lambda e, dt=dt, bb=bb: e.tensor_tensor(out=tmpf[bb][:], in0=xT[:, dt, :], in1=rstd[:], op=ALU.mult), [('x', dt), 'rstd'], [('tmpf', bb)])
            V(lambda e, dt=dt, bb=bb: e.tensor_scalar(out=tmpf[bb][:], in0=tmpf[bb][:], scalar1=coef[i][:, 3, dt:dt + 1], scalar2=coef[i][:, 4, dt:dt + 1],
                                                      op0=ALU.mult, op1=ALU.add), ['coefs'], [('tmpf', bb)])
            for tt in range(NCH):
                MM(psb[tt][:, 0:NEXP], tmpf[bb][:, tt * P:(tt + 1) * P], rt[:, dt, :], dt == 0, dt == DT - 1,
                   [('tmpf', bb), 'rt'], [PSK[tt]], inc=True)
        for tt in range(NCH):
            V(lambda e, tt=tt: e.tensor_copy(out=lg[:, tt, :], in_=psb[tt][:, 0:NEXP]), [], ['lg', PSK[tt]])
        for tt in range(NCH):
            V(lambda e, tt=tt: e.max(out=mx[:, tt, :], in_=lg[:, tt, :]), ['lg'], ['mx'])
        V(lambda e: e.tensor_tensor(out=gt[:, :, 0:1], in0=mx[:, :, 1:2], in1=mx[:, :, 0:1], op=ALU.subtract), ['mx'], ['gt'])
        Aop(lambda e: e.activation(out=gt[:, :, 1:2], in_=gt[:, :, 0:1], func=AF.Exp), [], ['gt'])
        V(lambda e: e.tensor_scalar(out=gt[:, :, 1:2], in0=gt[:, :, 1:2], scalar1=1.0, scalar2=None, op0=ALU.add), [], ['gt'])
        V(lambda e: e.reciprocal(out=gt[:, :, 2:3], in_=gt[:, :, 1:2]), [], ['gt'])
        V(lambda e: e.tensor_scalar(out=gt[:, :, 3:4], in0=gt[:, :, 2:3], scalar1=-1.0, scalar2=1.0, op0=ALU.mult, op1=ALU.add), [], ['gt'])
        V(lambda e: e.tensor_tensor(out=cb[:], in0=lg[:], in1=mx[:, :, 0:1].to_broadcast([P, NCH, NEXP]), op=ALU.is_equal), ['lg', 'mx'], ['cb'])
        V(lambda e: e.tensor_tensor(out=cb[:], in0=cb[:], in1=gt[:, :, 2:3].to_broadcast([P, NCH, NEXP]), op=ALU.mult), ['gt'], ['cb'])
        V(lambda e: e.tensor_tensor(out=lg[:], in0=lg[:], in1=mx[:, :, 1:2].to_broadcast([P, NCH, NEXP]), op=ALU.is_equal), ['mx'], ['lg'])
        V(lambda e: e.tensor_tensor(out=lg[:], in0=lg[:], in1=gt[:, :, 3:4].to_broadcast([P, NCH, NEXP]), op=ALU.mult), ['gt'], ['lg'])
        V(lambda e: e.tensor_tensor(out=cb[:], in0=cb[:], in1=lg[:], op=ALU.add), ['lg'], ['cb'])
        if os.environ.get('MOEDBG'):
            for nm_, t_, k_ in [('dbg_cb', cb, 'cb'), ('dbg_mx', mx, 'mx')]:
                fw.dma('sp', dout(nm_, [P, NCH, 8]), t_[:], reads=[k_])
        for hf in range(2):
            for q in range(4):
                MM(psb[1][0:NEXP, q * P:(q + 1) * P], cb[:, hf * 4 + q, :], ident_f, True, True, ['cb', 'cst'], [PSK[1]])
            V(lambda e, hf=hf: e.tensor_copy(out=cbT[0:NEXP, hf * 512:(hf + 1) * 512], in_=psb[1][0:NEXP, :]), [], [('tmpf', 1), PSK[1]])
        V(lambda e: e.tensor_copy(out=sel[0:NEXP, :].rearrange("p (a b) -> p a b", a=NEXP),
                                  in_=cst[0:NEXP, 0, 0:NEXP].unsqueeze(2).to_broadcast([NEXP, NEXP, P])), ['cst'], [('tmpf', 2)])
        groups = []
        for ex in range(NEXP):
            def pre(ex=ex):
                for tc in range(2):
                    MM(psb[tc][:], sel[0:NEXP, ex * P:(ex + 1) * P], cbT[0:NEXP, tc * 512:(tc + 1) * 512], True, True, [('tmpf', 2), ('tmpf', 1)], [PSK[tc]])
                    V(lambda e, tc=tc: e.tensor_copy(out=comb[:, tc * 512:(tc + 1) * 512], in_=psb[tc][:]), [], ['rstd', PSK[tc]])
            groups += make_groups(mw1[ex], mw3[ex], mw2[ex], FF_EXP, lambda tc: comb[:, tc * 512:(tc + 1) * 512], pre)
        ffn_groups(groups, g2cols, slots, gTb, silb)

    def stop_here():
        fw.barrier()
        for kt in range(DT):
            fw.dma('sp', dbg_ym[:, kt, :], ymix[:, kt, :], reads=[('ym', kt)])
        for dt in range(DT):
            fw.dma('sp', yT_out[dt * P:(dt + 1) * P, :], xT[:, dt, :], reads=[('x', dt)])
        fw.finish()
        return nc

    for i in range(DEPTH):
        plan = []
        for j in range(4):
            plan += [(in_w[i], C_XB + j * P, P), (in_w[i], C_GB + j * P, P)]
        plan += [(in_w[i], C_Q, 512), (in_w[i], C_G, 512), (in_w[i], C_V, 512), (in_w[i], C_GRAW, 16)]
        plan += [(in_w[i], C_XBC + j * P, P) for j in range(12)] + [(in_w[i], C_DT, 16), (in_w[i], C_Z, 512), (in_w[i], C_Z + 512, 512)]
        plan += [(out_w[i], q * 512, 512) for q in range(4)]
        if not STOP_AFTER:
            plan_loads(plan)
        norm_to_hT(i, 0)
        for dt in range(DT):
            fw.dma('sp', xspill[dt * P:(dt + 1) * P, :], xT[:, dt, :], reads=[('x', dt)])
        fw.barrier()
        for nm, fn in [('lru', lru_mixer), ('gla', gla_mixer), ('ssd', ssd_mixer)]:
            if STOP_AFTER and STOP_AFTER[1] == i and STOP_AFTER[0] in ('lru', 'gla', 'ssd') and STOP_AFTER[0] != nm and STOP_AFTER[2:] == ('only',):
                continue
            fn(i)
            fw.barrier()
            if STOP_AFTER == (nm, i) or STOP_AFTER == (nm, i, 'only'):
                for dt in range(DT):
                    fw.dma('sp', xT[:, dt, :], xspill[dt * P:(dt + 1) * P, :], writes=[('x', dt)])
                return stop_here()
        for dt in range(DT):
            fw.dma('sp', xT[:, dt, :], xspill[dt * P:(dt + 1) * P, :], writes=[('x', dt)])
        if STOP_AFTER:
            plan_loads([(out_w[i], q * 512, 512) for q in range(4)])
        for dt4 in range(4):
            w = load_cols(out_w[i], dt4 * 512, 512)
            for dq in range(4):
                dt = dt4 * 4 + dq
                for tc in range(2):
                    o_ = 6 + tc
                    for kt in range(DT):
                        MM(psb[o_][:], w[:, kt, dq * P:(dq + 1) * P], ymix[:, kt, tc * 512:(tc + 1) * 512], kt == 0, kt == DT - 1,
                           [('rg', id(w), kt), ('ym', kt)], [PSK[o_]])
                    V(lambda e, dt=dt, tc=tc, o_=o_: e.scalar_tensor_tensor(
                        out=xT[:, dt, tc * 512:(tc + 1) * 512], in0=psb[o_][:], scalar=coef[i][:, 2, dt:dt + 1],
                        in1=xT[:, dt, tc * 512:(tc + 1) * 512], op0=ALU.mult, op1=ALU.add), ['coefs'], [('x', dt), PSK[o_]])
        if STOP_AFTER == ('mix', i):
            return stop_here()
        norm_to_hT(i, 1)
        fw.barrier()
        A_ffn.reset()
        slots = ring + [A_ffn.alloc([P, DT, 512], BF16, "ringB%d" % q) for q in range(2)]
        gTb = A_ffn.alloc([P, 4, T], BF16, "gT")
        silb = [A_ffn.alloc([P, 512], BF16, "silb%d" % q) for q in range(2)]
        g2cols = [coef[i][:, 5, q:q + 1] for q in range(DT)]
        if i % 2 == 0:
            ffn_groups(make_groups(w1, w3, w2, FF_DENSE), g2cols, slots, gTb, silb)
        else:
            moe_layer(i, g2cols, slots, gTb, silb)
        fw.barrier()
        if STOP_AFTER == ('ffn', i):
            return stop_here()

    rms_stats(lambda j: xT[:, j, :], DT, lambda j: ('x', j), D)
    for dt in range(DT):
        bb = 1 + dt % 2
        V(lambda e, dt=dt, bb=bb: e.tensor_tensor(out=tmpf[bb][:], in0=xT[:, dt, :], in1=rstd[:], op=ALU.mult), [('x', dt), 'rstd'], [('tmpf', bb)])
        V(lambda e, dt=dt, bb=bb: e.tensor_scalar(out=tmpf[bb][:], in0=tmpf[bb][:], scalar1=pcol('final_g', dt), scalar2=None, op0=ALU.mult),
          ['par'], [('tmpf', bb)])
        fw.dma('sp', yT_out[dt * P:(dt + 1) * P, :], tmpf[bb][:], reads=[('tmpf', bb)])
    fw.finish()
    return nc


def _host_params(core, inp):
    par = np.zeros((P, NPAR), np.float32)

    def put(name, vec):
        o, w = PAR_OFF[name]
        par[:, o:o + w] = np.asarray(vec, np.float32).reshape(w, P).T

    def rep(name, vec):
        o, w = PAR_OFF[name]
        par[:, o:o + w] = np.asarray(vec, np.float32).reshape(1, w)
    sample = core < 4
    put('cvec', inp['c'][core] if sample else inp['c_ctx'])
    put('final_g', inp['final_norm_g'])
    rep('flag', [1.0 if sample else 0.0])
    for i in range(DEPTH):
        put('modb%d' % i, inp['mod_b'][i])
        put('n1g%d' % i, inp['norm1_g'][i])
        put('n2g%d' % i, inp['norm2_g'][i])
        o, w = PAR_OFF['lru_cw%d' % i]
        par[:, o:o + w] = inp['lru_conv_w'][i].reshape(4, 4, P).transpose(2, 1, 0).reshape(P, 16)
        put('lru_cb%d' % i, inp['lru_conv_b'][i])
        for d in range(2):
            put('lru_ba%d%d' % (i, d), inp['lru_ba'][i, d])
            put('lru_bx%d%d' % (i, d), inp['lru_bx'][i, d])
            put('lru_lam%d%d' % (i, d), inp['lru_lambda'][i, d])
            put('lru_h0%d%d' % (i, d), inp['state_lru'][core, i, d] if sample else np.zeros(512, np.float32))
        o, w = PAR_OFF['ssd_cw%d' % i]
        par[:, o:o + w] = inp['ssd_conv_w'][i].reshape(4, 12, P).transpose(2, 1, 0).reshape(P, 48)
        put('ssd_cb%d' % i, inp['ssd_conv_b'][i])
        put('ssd_ng%d' % i, inp['ssd_norm_g'][i])
        rep('ssd_D%d' % i, inp['ssd_D'][i])
        for d in range(2):
            rep('ssd_dtb%d%d' % (i, d), inp['ssd_dt_bias'][i, d])
            rep('ssd_alog%d%d' % (i, d), inp['ssd_A_log'][i, d])
        put('gla_ng%d' % i, inp['gla_norm_g'][i])
    return par


def _host_consts(core):
    import ml_dtypes
    s = np.arange(P)
    cst = np.zeros((P, 4, P), np.float32)
    cst[:, 0] = np.eye(P)
    cst[:, 1] = (s[:, None] <= s[None, :])
    cst[:, 2] = (s[:, None] >= s[None, :])
    cst[:, 3] = 1.0
    L = 64 if core < 4 else 256
    t = np.arange(T)
    m = np.stack([(t % L != 0), (t % L != L - 1), (t % L < L - 2)], 0).astype(np.float32)
    msk = np.broadcast_to(m[None], (P, 3, T)).astype(ml_dtypes.bfloat16)
    return cst, np.ascontiguousarray(msk)


def make_in_maps(inp, cores=range(8)):
    inp = {k: np.asarray(v) for k, v in inp.items()}
    gw = np.zeros((DEPTH, 2, 32, 256), np.float32)
    gw[:, :, 0:16] = inp['gla_gate_w']
    gw[:, :, 16] = inp['gla_gate_b']
    lw = np.zeros((DEPTH, 2, 2, 4, P, P), np.float32)
    for ax, nm in enumerate(['lru_wa', 'lru_wx']):
        wv = inp[nm]
        for j in range(4):
            lw[:, :, ax, j, 0:64, 0:64] = wv[:, :, 2 * j]
            lw[:, :, ax, j, 64:128, 64:128] = wv[:, :, 2 * j + 1]
    maps = []
    for core in cores:
        sample = core < 4
        x = inp['x_sample'][core] if sample else inp['x_prompt'][4 * (core - 4):4 * (core - 3)].reshape(T, D)
        cst, msk = _host_consts(core)
        if sample:
            sh = inp['state_ssd'][core]
            ssd_h0 = np.ascontiguousarray(sh.reshape(DEPTH, 2, 1024, P).transpose(0, 1, 3, 2))
            gh = inp['state_gla'][core]
            gla_h0 = np.ascontiguousarray(gh.reshape(DEPTH, 2, 2, 2, 64, P).transpose(0, 1, 3, 4, 2, 5).reshape(DEPTH, 2, P, 256))
        else:
            ssd_h0 = np.zeros((DEPTH, 2, P, 1024), np.float32)
            gla_h0 = np.zeros((DEPTH, 2, P, 256), np.float32)
        maps.append({
            "xT": np.ascontiguousarray(x.T), "par": _host_params(core, inp), "cst": cst, "msk": msk,
            "mod_w": inp['mod_w'], "in_w": inp['in_w'], "out_w": inp['out_w'],
            "ffn_w1": inp['ffn_w1'][0], "ffn_w3": inp['ffn_w3'][0], "ffn_w2": inp['ffn_w2'][0],
            "router": inp['moe_router'][0], "moe_w1": inp['moe_w1'][0], "moe_w3": inp['moe_w3'][0], "moe_w2": inp['moe_w2'][0],
            "gla_gw": gw, "lru_w": lw, "ssd_h0": ssd_h0, "gla_h0": gla_h0,
        })
    return maps


def assemble(results):
    ys = [np.ascontiguousarray(r["yT"].T) for r in results]
    y_sample = np.stack(ys[:4], 0)
    y_prompt = np.concatenate(ys[4:], 0).reshape(16, 256, D)
    pr = results[4:]
    new_ssd = np.concatenate([r["o_ssd"].reshape(4, DEPTH, 2, 16, 64, P) for r in pr], 0)
    new_gla = np.concatenate([r["o_gla"].reshape(4, DEPTH, 2, 2, 64, 2, P).transpose(0, 1, 2, 5, 3, 4, 6).reshape(4, DEPTH, 2, 4, 64, P)
                              for r in pr], 0)
    new_lru = np.concatenate([r["o_lru"].reshape(DEPTH, P, 4, 4, 2).transpose(3, 0, 4, 2, 1).reshape(4, DEPTH, 2, 512) for r in pr], 0)
    return (y_prompt, y_sample, np.ascontiguousarray(new_ssd), np.ascontiguousarray(new_gla), np.ascontiguousarray(new_lru))


def kernel(**inputs):
    nc = build_program()
    in_maps = make_in_maps(inputs)
    res = run_bass_kernel_spmd(nc, in_maps, core_ids=list(range(8)))
    return assemble(res.results)
```

```python
from contextlib import ExitStack
import numpy as np
import concourse.bass as bass
import concourse.mybir as mybir
from concourse.bass_utils import run_bass_kernel_spmd

F32 = mybir.dt.float32
BF16 = mybir.dt.bfloat16
AF = mybir.ActivationFunctionType
ALU = mybir.AluOpType
AX = mybir.AxisListType

P = 128
T = 1024
D = 2048
DT = 16
NCH = 8
DEPTH = 2
IN_COLS = 5152
FF_DENSE = 5504
FF_EXP = 7168
NEXP = 8
EPS = 1e-6
ND = 48
ARENA0 = 16512
KB = 1024
import os
STOPX = int(os.environ.get('STOPX', '0'))
STOP_AFTER = None
C_Z, C_XBC, C_DT, C_Q, C_K, C_V, C_G, C_GRAW, C_XB, C_GB = 0, 1024, 2560, 2576, 2832, 3088, 3600, 4112, 4128, 4640


class FW:
    def __init__(self, nc):
        self.nc = nc
        self.es = ExitStack()
        self.eng = {'pe': nc.tensor, 'dve': nc.vector, 'act': nc.scalar, 'pool': nc.gpsimd, 'sp': nc.sync}
        self.sem = {k: self.es.enter_context(nc.semaphore("s_" + k)) for k in ['pe', 'dve', 'act', 'pool']}
        self.cnt = {k: 0 for k in self.sem}
        self.dsem = [self.es.enter_context(nc.semaphore("d%d" % i)) for i in range(ND)]
        self.dcnt = [0] * ND
        self.dnext = 0
        self.seen = {e: {} for e in self.eng}
        self.lastw = {}
        self.readers = {}
        self.nbuf = 0

    def ps(self, shape, dtype=F32, name=None):
        self.nbuf += 1
        return self.es.enter_context(self.nc.psum_tensor(name or ("p%d" % self.nbuf), list(shape), dtype))

    def _semh(self, sk):
        return self.sem[sk[1]] if sk[0] == 'e' else self.dsem[sk[1]]

    def _need(self, E, deps):
        for sk, v in deps:
            if sk[0] == 'd':
                v = 16 * self.dcnt[sk[1]]
            if sk == ('e', 'pe') and E == 'pe':
                continue
            if self.seen[E].get(sk, 0) >= v:
                continue
            self.eng[E].wait_ge(self._semh(sk), v)
            self.seen[E][sk] = v

    def _deps(self, reads, writes):
        deps = []
        for r in reads:
            lw = self.lastw.get(r)
            if lw:
                deps.append(lw)
        for w in writes:
            lw = self.lastw.get(w)
            if lw:
                deps.append(lw)
            deps.extend(self.readers.get(w, {}).items())
        return deps

    def _commit(self, key, val, reads, writes):
        for r in reads:
            self.readers.setdefault(r, {})[key] = val
        for w in writes:
            self.lastw[w] = (key, val)
            self.readers[w] = {}

    def op(self, E, fn, reads=(), writes=(), inc=True):
        self._need(E, self._deps(reads, writes))
        ins = fn(self.eng[E])
        val = self.cnt[E] + 1
        if inc:
            ins.then_inc(self.sem[E], 1)
            self.cnt[E] = val
        self._commit(('e', E), val, reads, writes)

    def dma(self, Q, out, in_, reads=(), writes=()):
        self._need(Q, self._deps(reads, writes))
        i = self.dnext
        self.dnext = (i + 1) % ND
        self.eng[Q].dma_start(out=out, in_=in_).then_inc(self.dsem[i], 16)
        self.dcnt[i] += 1
        self._commit(('d', i), 16 * self.dcnt[i], reads, writes)

    def barrier(self):
        for E in self.eng:
            deps = [(('e', k), self.cnt[k]) for k in self.sem if self.cnt[k] and k != E]
            deps += [(('d', i), 16 * self.dcnt[i]) for i in range(ND) if self.dcnt[i]]
            self._need(E, deps)

    def finish(self):
        for i in range(ND):
            if self.dcnt[i]:
                self.nc.sync.wait_ge(self.dsem[i], 16 * self.dcnt[i])
        for k in self.sem:
            if self.cnt[k]:
                self.nc.sync.wait_ge(self.sem[k], self.cnt[k])
        self.es.close()


class Arena:
    uid = 0

    def __init__(self, nc, base, size):
        self.nc, self.base, self.size, self.off = nc, base, size, 0

    def alloc(self, shape, dtype, name):
        n = 1
        for s in shape[1:]:
            n *= s
        nbytes = (n * (4 if dtype == F32 else 2) + 31) // 32 * 32
        assert self.off + nbytes <= self.size, (name, self.off, nbytes, self.size)
        Arena.uid += 1
        t = self.nc.alloc_sbuf_tensor_at("%s_%d" % (name, Arena.uid), list(shape), dtype, offset=self.base + self.off)
        self.off += nbytes
        return t

    def reset(self):
        self.off = 0


def _param_layout():
    off = {}
    n = 0

    def add(name, w):
        nonlocal n
        off[name] = (n, w)
        n += w
    add('cvec', DT)
    add('final_g', DT)
    add('flag', 1)
    for i in range(DEPTH):
        add('modb%d' % i, 96)
        add('n1g%d' % i, DT)
        add('n2g%d' % i, DT)
        add('lru_cw%d' % i, 16)
        add('lru_cb%d' % i, 4)
        for d in range(2):
            add('lru_ba%d%d' % (i, d), 4)
            add('lru_bx%d%d' % (i, d), 4)
            add('lru_lam%d%d' % (i, d), 4)
            add('lru_h0%d%d' % (i, d), 4)
        add('ssd_cw%d' % i, 48)
        add('ssd_cb%d' % i, 12)
        add('ssd_ng%d' % i, 8)
        add('ssd_D%d' % i, 16)
        for d in range(2):
            add('ssd_dtb%d%d' % (i, d), 16)
            add('ssd_alog%d%d' % (i, d), 16)
        add('gla_ng%d' % i, 1)
    return off, n


PAR_OFF, NPAR = _param_layout()


def build_program():
    nc = bass.Bass("TRN2", target_bir_lowering=False)
    fw = FW(nc)

    def din(name, shape, dt=F32):
        return nc.dram_tensor(name, list(shape), dt, kind="ExternalInput").ap()

    def dout(name, shape, dt=F32):
        return nc.dram_tensor(name, list(shape), dt, kind="ExternalOutput").ap()

    xT_in = din("xT", [D, T])
    par_in = din("par", [P, NPAR])
    cst_in = din("cst", [P, 4, P])
    msk_in = din("msk", [P, 3, T], BF16)
    mod_w = din("mod_w", [DEPTH, D, 6 * D])
    in_w = din("in_w", [DEPTH, D, IN_COLS])
    out_w = din("out_w", [DEPTH, D, D])
    w1 = din("ffn_w1", [D, FF_DENSE])
    w3 = din("ffn_w3", [D, FF_DENSE])
    w2 = din("ffn_w2", [FF_DENSE, D])
    rt_in = din("router", [D, NEXP])
    full = STOP_AFTER is None
    mw1 = din("moe_w1", [NEXP, D, FF_EXP]) if full else None
    mw3 = din("moe_w3", [NEXP, D, FF_EXP]) if full else None
    mw2 = din("moe_w2", [NEXP, FF_EXP, D]) if full else None
    gw_in = din("gla_gw", [DEPTH, 2, 32, 256])
    lw_in = din("lru_w", [DEPTH, 2, 2, 4, P, P])
    ssd_h0 = din("ssd_h0", [DEPTH, 2, P, 1024])
    gla_h0 = din("gla_h0", [DEPTH, 2, P, 256])
    yT_out = dout("yT", [D, T])
    o_ssd = dout("o_ssd", [4, DEPTH, 2, 1024, P])
    o_gla = dout("o_gla", [4, DEPTH, 2, P, 256])
    o_lru = dout("o_lru", [DEPTH, P, 32])
    dbg_ym = dout("dbg_ym", [P, DT, T], BF16) if STOP_AFTER else None
    xspill = nc.dram_tensor("xspill", [D, T], F32, kind="Internal").ap()

    b = ARENA0
    A_sm = Arena(nc, b, 11 * KB); b += 11 * KB
    A_h = Arena(nc, b, 32 * KB); b += 32 * KB
    A_ra = Arena(nc, b, 32 * KB); b += 32 * KB
    U_BASE = b
    A_u = Arena(nc, b, 42 * KB); b += 42 * KB
    A_x = Arena(nc, b, 64 * KB); b += 64 * KB
    A_nt = Arena(nc, b, 26 * KB); b += 26 * KB
    assert b <= 229376 - 64, b
    A_mix = Arena(nc, U_BASE + 32 * KB, 74 * KB)
    A_ffn = Arena(nc, U_BASE, 42 * KB)

    par = A_sm.alloc([P, NPAR], F32, "par")
    cst = A_sm.alloc([P, 4, P], F32, "cst")
    ident_b = A_sm.alloc([P, P], BF16, "identb")
    ones_b = A_sm.alloc([P, P], BF16, "onesb")
    mfb = A_sm.alloc([P, 2, P], BF16, "mfb")
    mods = [A_sm.alloc([P, 96], F32, "mods%d" % i) for i in range(DEPTH)]
    coef = [A_sm.alloc([P, 6, DT], F32, "coef%d" % i) for i in range(DEPTH)]
    csil = A_sm.alloc([P, DT], F32, "csil")
    epsb = A_sm.alloc([P, 1], F32, "epsb")
    oneb = A_sm.alloc([P, 1], F32, "oneb")
    lruF = A_sm.alloc([P, 32], F32, "lruF")
    rt = A_sm.alloc([P, DT, NEXP], F32, "rt")
    lg = A_sm.alloc([P, NCH, NEXP], F32, "lg")
    mx = A_sm.alloc([P, NCH, 8], F32, "mx")
    gt = A_sm.alloc([P, NCH, 4], F32, "gt")
    cb = A_sm.alloc([P, NCH, NEXP], F32, "cb")
    ident_f, Mf, Mb, ones_f = cst[:, 0, :], cst[:, 1, :], cst[:, 2, :], cst[:, 3, :]

    hT = A_h.alloc([P, DT, T], BF16, "hT")
    ring = [A_ra.alloc([P, DT, 512], BF16, "ring%d" % i) for i in range(2)]
    ymix = A_u.alloc([P, DT, T], BF16, "ymix")
    rstd = A_nt.alloc([P, T], F32, "rstd")
    tmpf = [A_nt.alloc([P, T], F32, "tmpf%d" % i) for i in range(3)]
    sqb = [A_nt.alloc([P, T], BF16, "sqb%d" % i) for i in range(2)]
    msk = A_nt.alloc([P, 3, T], BF16, "msk")
    psb = [fw.ps([P, 512], F32, "psb%d" % i) for i in range(8)]
    PSK = ['ps%d' % i for i in range(8)]

    def pcol(name, j=0, w=1):
        o, _ = PAR_OFF[name]
        return par[:, o + j:o + j + w]

    def V(fn, reads=(), writes=()):
        fw.op('dve', fn, reads, writes)

    def Aop(fn, reads=(), writes=()):
        fw.op('act', fn, reads, writes)

    def MM(out, lhsT, rhs, start, stop, reads, writes, inc=None):
        fw.op('pe', lambda e: e.matmul(out, lhsT, rhs, start=start, stop=stop), reads, writes,
              inc=(stop if inc is None else inc))

    fw.dma('sp', par[:], par_in, writes=['par'])
    for q in range(4):
        fw.dma('sp', cst[:, q, :], cst_in[:, q, :], writes=['cst'])
    for q in range(3):
        fw.dma('sp', msk[:, q, :], msk_in[:, q, :], writes=['msk'])
    for kt in range(DT):
        fw.dma('sp', rt[:, kt, :], rt_in[kt * P:(kt + 1) * P, :], writes=['rt'])
    V(lambda e: e.tensor_copy(out=ident_b[:], in_=ident_f), ['cst'], ['identb'])
    V(lambda e: e.memset(ones_b[:], 1.0), [], ['onesb'])
    V(lambda e: e.tensor_copy(out=mfb[:], in_=cst[:, 1:3, :]), ['cst'], ['mfb'])
    V(lambda e: e.memset(epsb[:], EPS), [], ['epsb'])
    V(lambda e: e.memset(oneb[:], 1.0), [], ['oneb'])
    Aop(lambda e: e.activation(out=csil[:], in_=pcol('cvec', 0, DT), func=AF.Silu), ['par'], ['csil'])

    MC = 384
    mwb = [A_x.alloc([P, DT, MC], F32, "mwb%d" % i) for i in range(2)]
    kk = 0
    for i in range(DEPTH):
        for ch in range(6 * D // MC):
            bb = kk % 2
            kk += 1
            for kt in range(DT):
                fw.dma('sp', mwb[bb][:, kt, :], mod_w[i, kt * P:(kt + 1) * P, ch * MC:(ch + 1) * MC], writes=[('mwb', bb, kt)])
            rb = 2 + kk % 2
            for kt in range(DT):
                MM(psb[rb][0:1, 0:MC], csil[:, kt:kt + 1], mwb[bb][:, kt, :], kt == 0, kt == DT - 1,
                   [('mwb', bb, kt), 'csil'], [PSK[rb]])
            Aop(lambda e, rb=rb: e.activation(out=tmpf[0][0:1, (kk % 2) * MC:(kk % 2) * MC + MC], in_=psb[rb][0:1, 0:MC], func=AF.Copy),
                [], [('mrow', kk % 2), PSK[rb]])
            for mm in range(MC // P):
                m = ch * (MC // P) + mm
                c0 = (kk % 2) * MC + mm * P
                MM(psb[0][:, m:m + 1], tmpf[0][0:1, c0:c0 + P], oneb[0:1, 0:1], True, True, [('mrow', kk % 2), 'oneb'], ['ps0'])
        V(lambda e, i=i: e.tensor_tensor(out=mods[i][:], in0=psb[0][:, 0:96], in1=pcol('modb%d' % i, 0, 96), op=ALU.add),
          ['par'], [('mods', i), 'ps0'])
        for half, (ng, sh_i, sc_i, g_i) in enumerate([('n1g%d' % i, 0, 1, 2), ('n2g%d' % i, 3, 4, 5)]):
            V(lambda e, i=i, half=half, ng=ng, sc_i=sc_i: e.scalar_tensor_tensor(
                out=coef[i][:, 3 * half + 0, :], in0=mods[i][:, sc_i * DT:(sc_i + 1) * DT], scalar=1.0,
                in1=pcol(ng, 0, DT), op0=ALU.add, op1=ALU.mult), [('mods', i), 'par'], ['coefs'])
            V(lambda e, i=i, half=half, sh_i=sh_i: e.tensor_copy(
                out=coef[i][:, 3 * half + 1, :], in_=mods[i][:, sh_i * DT:(sh_i + 1) * DT]), [('mods', i)], ['coefs'])
            V(lambda e, i=i, half=half, g_i=g_i: e.tensor_copy(
                out=coef[i][:, 3 * half + 2, :], in_=mods[i][:, g_i * DT:(g_i + 1) * DT]), [('mods', i)], ['coefs'])
    fw.barrier()
    A_x.reset()
    xT = A_x.alloc([P, DT, T], F32, "xT")
    for dt in range(DT):
        fw.dma('sp', xT[:, dt, :], xT_in[dt * P:(dt + 1) * P, :], writes=[('x', dt)])

    def rms_stats(src_fn, ntiles, key_fn, width):
        for j in range(ntiles):
            bb = j % 2
            Aop(lambda e, j=j, bb=bb: e.activation(out=sqb[bb][:], in_=src_fn(j), func=AF.Square), [key_fn(j)], [('sqb', bb)])
            for tc in range(2):
                MM(psb[tc][:], ones_b[:], sqb[bb][:, tc * 512:(tc + 1) * 512], j == 0, j == ntiles - 1,
                   [('sqb', bb), 'onesb'], [PSK[tc]], inc=True)
        for tc in range(2):
            Aop(lambda e, tc=tc: e.activation(out=tmpf[0][:, tc * 512:(tc + 1) * 512], in_=psb[tc][:], func=AF.Sqrt,
                                              scale=1.0 / width, bias=epsb[:]), ['epsb'], [('tmpf', 0), PSK[tc]])
        V(lambda e: e.reciprocal(out=rstd[:], in_=tmpf[0][:]), [('tmpf', 0)], ['rstd'])

    def norm_to_hT(i, half):
        rms_stats(lambda j: xT[:, j, :], DT, lambda j: ('x', j), D)
        for dt in range(DT):
            bb = 1 + dt % 2
            V(lambda e, dt=dt, bb=bb: e.tensor_tensor(out=tmpf[bb][:], in0=xT[:, dt, :], in1=rstd[:], op=ALU.mult),
              [('x', dt), 'rstd'], [('tmpf', bb)])
            V(lambda e, dt=dt, bb=bb: e.tensor_scalar(out=hT[:, dt, :], in0=tmpf[bb][:], scalar1=coef[i][:, 3 * half, dt:dt + 1],
                                                      scalar2=coef[i][:, 3 * half + 1, dt:dt + 1], op0=ALU.mult, op1=ALU.add),
              [('tmpf', bb), 'coefs'], [('h', dt)])

    rstate = {'k': 0}

    def load_cols(src2d, c0, ncols):
        s = ring[rstate['k'] % 2]
        rstate['k'] += 1
        for kt in range(DT):
            fw.dma('pool', s[:, kt, 0:ncols], src2d[kt * P:(kt + 1) * P, c0:c0 + ncols], writes=[('rg', id(s), kt)])
        return s

    def proj_fm(wslot, cofs, m, pi):
        for tc in range(2):
            for kt in range(DT):
                MM(psb[pi + tc][0:m, :], wslot[:, kt, cofs:cofs + m], hT[:, kt, tc * 512:(tc + 1) * 512], kt == 0, kt == DT - 1,
                   [('rg', id(wslot), kt), ('h', kt)], [PSK[pi + tc]])

    def proj_tm(wslot, cofs, n, tt, pi):
        for kt in range(DT):
            MM(psb[pi][:, 0:n], hT[:, kt, tt * P:(tt + 1) * P], wslot[:, kt, cofs:cofs + n], kt == 0, kt == DT - 1,
               [('rg', id(wslot), kt), ('h', kt)], [PSK[pi]])

    def evac_fm(pi, dst_fn, wkeys, func=AF.Copy, m=P):
        for tc in range(2):
            Aop(lambda e, tc=tc: e.activation(out=dst_fn(tc), in_=psb[pi + tc][0:m, :], func=func), [], list(wkeys) + [PSK[pi + tc]])

    def conv_tile(skey, dst_fn, cw, cb_, j):
        src, acc, tmp = tmpf[0], tmpf[1], tmpf[2]
        V(lambda e: e.tensor_scalar(out=acc[:], in0=src[:], scalar1=pcol(cw, 4 * j + 1), scalar2=pcol(cb_, j), op0=ALU.mult, op1=ALU.add),
          [skey, 'par'], [('tmpf', 1)])
        for (k, lo, hi, sh, mi) in [(0, 1, T, -1, 0), (2, 0, T - 1, 1, 1), (3, 0, T - 2, 2, 2)]:
            V(lambda e, lo=lo, hi=hi, sh=sh, mi=mi: e.tensor_tensor(out=tmp[:, lo:hi], in0=src[:, lo + sh:hi + sh], in1=msk[:, mi, lo:hi], op=ALU.mult),
              [skey, 'msk'], [('tmpf', 2)])
            V(lambda e, lo=lo, hi=hi, k=k: e.scalar_tensor_tensor(out=acc[:, lo:hi], in0=tmp[:, lo:hi], scalar=pcol(cw, 4 * j + k),
                                                                  in1=acc[:, lo:hi], op0=ALU.mult, op1=ALU.add),
              [('tmpf', 2), 'par'], [('tmpf', 1)])
        dst_fn(acc)

    def lru_mixer(i):
        A_mix.reset()
        inw = in_w[i]
        wbd = A_mix.alloc([P, 2, 2, P], BF16, "lruw")
        xr = A_mix.alloc([P, T], F32, "xr")
        xrb = A_mix.alloc([P, T], BF16, "xrb")
        gl = A_mix.alloc([P, T], F32, "gl")
        gx = A_mix.alloc([P, T], F32, "gx")
        hs = A_mix.alloc([P, T], F32, "hs")
        rr = A_mix.alloc([P, T], F32, "rr")
        ii = A_mix.alloc([P, T], F32, "ii")
        aa = A_mix.alloc([P, T], F32, "aa")
        uu = A_mix.alloc([P, T], F32, "uu")
        hh = A_mix.alloc([P, T], F32, "hh")
        cc = A_mix.alloc([P, 2], F32, "cc")
        for j in range(4):
            wx = load_cols(inw, C_XB + j * P, P)
            wg = load_cols(inw, C_GB + j * P, P)
            for d in range(2):
                for ax in range(2):
                    fw.dma('pool', wbd[:, d, ax, :], lw_in[i, d, ax, j], writes=[('lruw', d, ax)])
            proj_fm(wx, 0, P, 2)
            evac_fm(2, lambda tc: tmpf[0][:, tc * 512:(tc + 1) * 512], [('tmpf', 0)])

            def fin(acc):
                V(lambda e: e.tensor_copy(out=xr[:], in_=acc[:]), [('tmpf', 1)], ['xr'])
                V(lambda e: e.tensor_copy(out=xrb[:], in_=acc[:]), [('tmpf', 1)], ['xrb'])
            conv_tile(('tmpf', 0), fin, 'lru_cw%d' % i, 'lru_cb%d' % i, j)
            proj_fm(wg, 0, P, 4)
            evac_fm(4, lambda tc: gx[:, tc * 512:(tc + 1) * 512], ['gx'])
            V(lambda e: e.tensor_tensor(out=gl[:], in0=gx[:], in1=gx[:], op=ALU.mult), ['gx'], ['gl'])
            V(lambda e: e.tensor_scalar(out=gl[:], in0=gl[:], scalar1=0.044715, scalar2=1.0, op0=ALU.mult, op1=ALU.add), [], ['gl'])
            V(lambda e: e.tensor_tensor(out=gl[:], in0=gl[:], in1=gx[:], op=ALU.mult), ['gx'], ['gl'])
            Aop(lambda e: e.activation(out=gl[:], in_=gl[:], func=AF.Sigmoid, scale=1.5957691216057308), [], ['gl'])
            V(lambda e: e.tensor_tensor(out=gl[:], in0=gl[:], in1=gx[:], op=ALU.mult), ['gx'], ['gl'])
            for d in range(2):
                Aop(lambda e, d=d: e.activation(out=cc[:, 0:1], in_=pcol('lru_lam%d%d' % (i, d), j), func=AF.Exp, scale=-1.0), ['par'], ['cc'])
                Aop(lambda e: e.activation(out=cc[:, 0:1], in_=cc[:, 0:1], func=AF.Ln, bias=oneb[:]), ['oneb'], ['cc'])
                V(lambda e: e.tensor_scalar(out=cc[:, 1:2], in0=cc[:, 0:1], scalar1=-8.0, scalar2=None, op0=ALU.mult), [], ['cc'])
                for ax, (dst, dk, bn, ps0) in enumerate([(rr, 'rr', 'lru_ba%d%d' % (i, d), 2), (ii, 'ii', 'lru_bx%d%d' % (i, d), 4)]):
                    for tc in range(2):
                        MM(psb[ps0 + tc][:], wbd[:, d, ax, :], xrb[:, tc * 512:(tc + 1) * 512], True, True,
                           [('lruw', d, ax), 'xrb'], [PSK[ps0 + tc]])
                        Aop(lambda e, tc=tc, dst=dst, bn=bn, ps0=ps0: e.activation(
                            out=dst[:, tc * 512:(tc + 1) * 512], in_=psb[ps0 + tc][:], func=AF.Sigmoid, bias=pcol(bn, j)),
                            ['par'], [dk, PSK[ps0 + tc]])
                Aop(lambda e: e.activation(out=aa[:], in_=rr[:], func=AF.Exp, scale=cc[:, 1:2]), ['rr', 'cc'], ['aa'])
                V(lambda e: e.tensor_tensor(out=uu[:], in0=aa[:], in1=aa[:], op=ALU.mult), ['aa'], ['uu'])
                V(lambda e: e.tensor_scalar(out=uu[:], in0=uu[:], scalar1=-1.0, scalar2=1.0, op0=ALU.mult, op1=ALU.add), [], ['uu'])
                V(lambda e: e.tensor_scalar(out=uu[:], in0=uu[:], scalar1=0.0, scalar2=None, op0=ALU.max), [], ['uu'])
                Aop(lambda e: e.activation(out=uu[:], in_=uu[:], func=AF.Sqrt), [], ['uu'])
                V(lambda e: e.tensor_tensor(out=uu[:], in0=uu[:], in1=ii[:], op=ALU.mult), ['ii'], ['uu'])
                V(lambda e: e.tensor_tensor(out=uu[:], in0=uu[:], in1=xr[:], op=ALU.mult), ['xr'], ['uu'])
                av = aa[:].rearrange("p (s l) -> p s l", l=256)
                if d == 0:
                    V(lambda e: e.tensor_scalar(out=av[:, 1:4, 0:1], in0=av[:, 1:4, 0:1], scalar1=pcol('flag'), scalar2=None, op0=ALU.mult),
                      ['par'], ['aa'])
                    V(lambda e, d=d: e.tensor_tensor_scan(out=hh[:], data0=aa[:], data1=uu[:], initial=pcol('lru_h0%d%d' % (i, d), j),
                                                          op0=ALU.mult, op1=ALU.add), ['aa', 'uu', 'par'], ['hh'])
                    V(lambda e: e.tensor_copy(out=hs[:], in_=hh[:]), ['hh'], ['hs'])
                else:
                    V(lambda e: e.tensor_scalar(out=av[:, 0:3, 255:256], in0=av[:, 0:3, 255:256], scalar1=pcol('flag'), scalar2=None, op0=ALU.mult),
                      ['par'], ['aa'])
                    V(lambda e, d=d: e.tensor_tensor_scan(out=hh[:, ::-1], data0=aa[:, ::-1], data1=uu[:, ::-1],
                                                          initial=pcol('lru_h0%d%d' % (i, d), j), op0=ALU.mult, op1=ALU.add),
                      ['aa', 'uu', 'par'], ['hh'])
                    V(lambda e: e.tensor_tensor(out=hs[:], in0=hs[:], in1=hh[:], op=ALU.add), ['hh'], ['hs'])
                hv = hh[:].rearrange("p (s l) -> p s l", l=256)
                col = 255 if d == 0 else 0
                fv = lruF[:].rearrange("p (j s d) -> p j s d", j=4, s=4)
                V(lambda e, d=d, col=col, j=j: e.tensor_copy(out=fv[:, j, :, d:d + 1], in_=hv[:, :, col:col + 1]), ['hh'], ['lruF'])
            V(lambda e, j=j: e.tensor_tensor(out=ymix[:, 12 + j, :], in0=hs[:], in1=gl[:], op=ALU.mult), ['hs', 'gl'], [('ym', 12 + j)])
        fw.dma('sp', o_lru[i], lruF[:], reads=['lruF'])

    def gla_mixer(i):
        A_mix.reset()
        inw = in_w[i]
        qT = A_mix.alloc([P, 2, T], F32, "qT")
        kT = A_mix.alloc([P, 2, T], F32, "kT")
        vtm = A_mix.alloc([P, NCH, 512], BF16, "vtm")
        gT_ = A_mix.alloc([P, 4, T], BF16, "gTs")
        grA = A_mix.alloc([32, T], F32, "grA")
        gw = A_mix.alloc([32, 256], F32, "gw")
        ltm = A_mix.alloc([P, NCH, 256], F32, "ltm")
        oT = A_mix.alloc([P, 4, T], F32, "oT")
        eb = A_mix.alloc([P, 2, P], F32, "eb")
        enb = A_mix.alloc([P, 2, P], F32, "enb")
        qt = A_mix.alloc([P, 2, P], BF16, "qt")
        kt_ = A_mix.alloc([P, 2, P], BF16, "kt")
        qtm = A_mix.alloc([P, 4, P], BF16, "qtm")
        ktm = A_mix.alloc([P, 256], BF16, "ktm")
        atT = A_mix.alloc([P, 4, P], BF16, "atT")
        S = A_mix.alloc([P, 2, P], F32, "S")
        Sb = A_mix.alloc([P, 2, P], BF16, "Sb")
        stg = [A_mix.alloc([P, 2, P], F32, "stg%d" % q) for q in range(2)]
        w = load_cols(inw, C_Q, 512)
        for t2 in range(2):
            for (dst, cofs, nm) in [(qT, 0, 'qT'), (kT, 256, 'kT')]:
                proj_fm(w, cofs + t2 * P, P, 2)
                evac_fm(2, lambda tc, dst=dst, t2=t2: dst[:, t2, tc * 512:(tc + 1) * 512], [nm])
        w = load_cols(inw, C_G, 512)
        for t4 in range(4):
            proj_fm(w, t4 * P, P, 4)
            evac_fm(4, lambda tc, t4=t4: gT_[:, t4, tc * 512:(tc + 1) * 512], [('gTs', t4)], func=AF.Silu)
        w = load_cols(inw, C_V, 512)
        for tt in range(NCH):
            pi = 6 + tt % 2
            proj_tm(w, 0, 512, tt, pi)
            V(lambda e, tt=tt, pi=pi: e.tensor_copy(out=vtm[:, tt, :], in_=psb[pi][:]), [], [('vtm', tt), PSK[pi]])
        w = load_cols(inw, C_GRAW, 16)
        V(lambda e: e.memset(grA[:], 1.0), [], ['grA'])
        proj_fm(w, 0, 16, 2)
        evac_fm(2, lambda tc: grA[0:16, tc * 512:(tc + 1) * 512], ['grA'], m=16)
        V(lambda e: e.memset(oT[:], 0.0), [], ['oT'])
        V(lambda e: e.memset(qtm[:], 0.0), [], ['qtm'])
        if STOPX == 1:
            return
        for d in range(2):
            fw.dma('sp', gw[:], gw_in[i, d], writes=['gw'])
            for tt in range(NCH):
                pi = 6 + tt % 2
                MM(psb[pi][:, 0:256], grA[0:32, tt * P:(tt + 1) * P], gw[0:32, :], True, True, ['grA', 'gw'], [PSK[pi]])
                Aop(lambda e, tt=tt, pi=pi: e.activation(out=ltm[:, tt, :], in_=psb[pi][:, 0:256], func=AF.Exp, scale=-1.0), [], [('ltm', tt), PSK[pi]])
                Aop(lambda e, tt=tt: e.activation(out=ltm[:, tt, :], in_=ltm[:, tt, :], func=AF.Ln, bias=oneb[:]), ['oneb'], [('ltm', tt)])
            if STOPX == 2:
                return
            fw.dma('sp', S[:].rearrange("p a b -> p (a b)"), gla_h0[i, d], writes=['S'])
            V(lambda e: e.tensor_copy(out=Sb[:], in_=S[:]), ['S'], ['Sb'])
            Mm = Mf if d == 0 else Mb
            order = list(range(NCH)) if d == 0 else list(range(NCH - 1, -1, -1))
            for step, c in enumerate(order):
                ts = slice(c * P, (c + 1) * P)
                last = P - 1 if d == 0 else 0
                if step > 0 and step % 2 == 0:
                    V(lambda e: e.tensor_scalar(out=S[:], in0=S[:], scalar1=pcol('flag'), scalar2=None, op0=ALU.mult), ['par'], ['S'])
                    V(lambda e: e.tensor_copy(out=Sb[:], in_=S[:]), ['S'], ['Sb'])
                for t2 in range(2):
                    MM(psb[2][:, t2 * P:(t2 + 1) * P], ltm[:, c, t2 * P:(t2 + 1) * P], Mm, True, True, [('ltm', c), 'cst'], [PSK[2]])
                Aop(lambda e: e.activation(out=eb[:].rearrange("p a b -> p (a b)"), in_=psb[2][:, 0:256], func=AF.Exp, scale=-1.0 / 16), [], ['eb', PSK[2]])
                Aop(lambda e: e.activation(out=enb[:].rearrange("p a b -> p (a b)"), in_=psb[2][:, 0:256], func=AF.Exp, scale=1.0 / 16), [], ['enb', PSK[2]])
                V(lambda e, ts=ts: e.scalar_tensor_tensor(out=qt[:], in0=qT[:, :, ts], scalar=0.125, in1=eb[:], op0=ALU.mult, op1=ALU.mult),
                  ['qT', 'eb'], ['qt'])
                V(lambda e, ts=ts: e.tensor_tensor(out=kt_[:], in0=kT[:, :, ts], in1=enb[:], op=ALU.mult), ['kT', 'enb'], ['kt'])
                for h in range(4):
                    rs = slice((h % 2) * 64, (h % 2) * 64 + 64)
                    V(lambda e, h=h, rs=rs: e.tensor_copy(out=qtm[rs, h, :], in_=qt[rs, h // 2, :]), ['qt'], ['qtm'])
                if STOPX == 3:
                    return
                for t2 in range(2):
                    MM(psb[3][:, t2 * P:(t2 + 1) * P], kt_[:, t2, :], ident_b[:], True, True, ['kt', 'identb'], [PSK[3]])
                V(lambda e: e.tensor_copy(out=ktm[:], in_=psb[3][:, 0:256]), [], ['ktm', PSK[3]])
                for h in range(4):
                    rs = slice((h % 2) * 64, (h % 2) * 64 + 64)
                    MM(psb[4][:, h * P:(h + 1) * P], kt_[:, h // 2, :], qtm[:, h, :], True, True, ['kt', 'qtm'], [PSK[4]])
                V(lambda e, d=d: e.tensor_tensor(out=atT[:], in0=psb[4][:].rearrange("p (h t) -> p h t", h=4),
                                                 in1=mfb[:, d, :].unsqueeze(1).to_broadcast([P, 4, P]), op=ALU.mult), ['mfb'], ['atT', PSK[4]])
                if STOPX == 4:
                    return
                for h in range(4):
                    rs = slice((h % 2) * 64, (h % 2) * 64 + 64)
                    MM(psb[5][:, h * P:(h + 1) * P], vtm[:, c, h * P:(h + 1) * P], atT[:, h, :], True, False, [('vtm', c), 'atT'], [PSK[5]], inc=False)
                    MM(psb[5][:, h * P:(h + 1) * P], Sb[:, h // 2, :], qtm[:, h, :], False, True, ['Sb', 'qtm'], [PSK[5]], inc=True)
                V(lambda e, ts=ts: e.tensor_tensor(out=oT[:, :, ts], in0=oT[:, :, ts], in1=psb[5][:].rearrange("p (h t) -> p h t", h=4), op=ALU.add),
                  [], ['oT', PSK[5]])
                if STOPX == 5:
                    return
                for t2 in range(2):
                    MM(psb[6][:, 0:256], ktm[:, t2 * P:(t2 + 1) * P], vtm[:, c, t2 * 256:(t2 + 1) * 256], True, True, ['ktm', ('vtm', c)], [PSK[6]])
                    for hl in range(2):
                        rs = slice(hl * 64, hl * 64 + 64)
                        V(lambda e, t2=t2, hl=hl, rs=rs: e.tensor_tensor(out=S[rs, t2, :], in0=S[rs, t2, :], in1=psb[6][rs, hl * P:(hl + 1) * P], op=ALU.add),
                          [], ['S', PSK[6]])
                    V(lambda e, t2=t2, last=last: e.tensor_scalar(out=S[:, t2, :], in0=S[:, t2, :], scalar1=eb[:, t2, last:last + 1], scalar2=None, op0=ALU.mult),
                      ['eb'], ['S'])
                V(lambda e: e.tensor_copy(out=Sb[:], in_=S[:]), ['S'], ['Sb'])
                if step % 2 == 1:
                    seg = c // 2
                    q = (step // 2) % 2
                    V(lambda e, q=q: e.tensor_copy(out=stg[q][:], in_=S[:]), ['S'], [('stg', q)])
                    fw.dma('sp', o_gla[seg, i, d], stg[q][:].rearrange("p a b -> p (a b)"), reads=[('stg', q)])
        for h in range(4):
            bb = h % 2
            Aop(lambda e, h=h, bb=bb: e.activation(out=sqb[bb][:], in_=oT[:, h, :], func=AF.Square), ['oT'], [('sqb', bb)])
            for tc in range(2):
                MM(psb[tc][:], ones_b[:], sqb[bb][:, tc * 512:(tc + 1) * 512], True, True, [('sqb', bb), 'onesb'], [PSK[tc]])
                Aop(lambda e, tc=tc: e.activation(out=tmpf[0][:, tc * 512:(tc + 1) * 512], in_=psb[tc][:], func=AF.Sqrt,
                                                  scale=1.0 / P, bias=epsb[:]), ['epsb'], [('tmpf', 0), PSK[tc]])
            V(lambda e: e.reciprocal(out=tmpf[1][:], in_=tmpf[0][:]), [('tmpf', 0)], [('tmpf', 1)])
            V(lambda e, h=h: e.tensor_tensor(out=tmpf[2][:], in0=oT[:, h, :], in1=tmpf[1][:], op=ALU.mult), ['oT', ('tmpf', 1)], [('tmpf', 2)])
            V(lambda e, h=h: e.scalar_tensor_tensor(out=ymix[:, 8 + h, :], in0=tmpf[2][:], scalar=pcol('gla_ng%d' % i), in1=gT_[:, h, :],
                                                    op0=ALU.mult, op1=ALU.mult), [('tmpf', 2), 'par', ('gTs', h)], [('ym', 8 + h)])

    def ssd_mixer(i):
        A_mix.reset()
        inw = in_w[i]
        xtm = A_mix.alloc([P, NCH, 1024], BF16, "xtm")
        BCT = A_mix.alloc([P, 4, T], BF16, "BCT")
        Btm = A_mix.alloc([P, NCH, 256], BF16, "Btm")
        yf = A_mix.alloc([P, NCH, 1024], BF16, "yf")
        dtr = A_mix.alloc([P, NCH, 16], F32, "dtr")
        dtd = A_mix.alloc([P, NCH, 16], F32, "dtd")
        ad = A_mix.alloc([P, NCH, 16], F32, "ad")
        nA = A_mix.alloc([P, 16], F32, "nA")
        abc = A_mix.alloc([P, 16, P], F32, "abc")
        DTm = A_mix.alloc([P, 16, P], F32, "DTm")
        WT = A_mix.alloc([P, 16, P], BF16, "WT")
        ST = A_mix.alloc([P, 1024], F32, "ST")
        STb = A_mix.alloc([P, 1024], BF16, "STb")
        sm = A_mix.alloc([P, 4, 16], F32, "ssm")
        rrep, yc, stg = tmpf[1], tmpf[2], tmpf[0]
        xdt, xdd = sqb[0], sqb[1]
        for j in range(12):
            w = load_cols(inw, C_XBC + j * P, P)
            proj_fm(w, 0, P, 2)
            evac_fm(2, lambda tc: tmpf[0][:, tc * 512:(tc + 1) * 512], [('tmpf', 0)])
            if j < 8:
                def fin(acc, j=j):
                    Aop(lambda e: e.activation(out=sqb[0][:], in_=acc[:], func=AF.Silu), [('tmpf', 1)], [('sqb', 0)])
                    for c in range(NCH):
                        pi = 4 + c % 2
                        MM(psb[pi][:, 0:P], sqb[0][:, c * P:(c + 1) * P], ident_b[:], True, True, [('sqb', 0), 'identb'], [PSK[pi]])
                        V(lambda e, c=c, pi=pi: e.tensor_copy(out=xtm[:, c, j * P:(j + 1) * P], in_=psb[pi][:, 0:P]), [], [('xtm', c), PSK[pi]])
            else:
                def fin(acc, j=j):
                    Aop(lambda e: e.activation(out=BCT[:, j - 8, :], in_=acc[:], func=AF.Silu), [('tmpf', 1)], [('BCT', j - 8)])
                    if j < 10:
                        for c in range(NCH):
                            pi = 4 + c % 2
                            MM(psb[pi][:, 0:P], BCT[:, j - 8, c * P:(c + 1) * P], ident_b[:], True, True, [('BCT', j - 8), 'identb'], [PSK[pi]])
                            V(lambda e, c=c, pi=pi: e.tensor_copy(out=Btm[:, c, (j - 8) * P:(j - 7) * P], in_=psb[pi][:, 0:P]), [], [('Btm', c), PSK[pi]])
            conv_tile(('tmpf', 0), fin, 'ssd_cw%d' % i, 'ssd_cb%d' % i, j)
        w = load_cols(inw, C_DT, 16)
        for tt in range(NCH):
            pi = 6 + tt % 2
            proj_tm(w, 0, 16, tt, pi)
            V(lambda e, tt=tt, pi=pi: e.tensor_copy(out=dtr[:, tt, :], in_=psb[pi][:, 0:16]), [], ['dtr', PSK[pi]])
        for d in (1, 0):
            if d == 0:
                wz = [load_cols(inw, C_Z, 512), load_cols(inw, C_Z + 512, 512)]
            V(lambda e, d=d: e.tensor_tensor(out=dtd[:], in0=dtr[:], in1=pcol('ssd_dtb%d%d' % (i, d), 0, 16).unsqueeze(1).to_broadcast([P, NCH, 16]), op=ALU.add),
              ['dtr', 'par'], ['dtd'])
            Aop(lambda e: e.activation(out=dtd[:], in_=dtd[:], func=AF.Exp), [], ['dtd'])
            Aop(lambda e: e.activation(out=dtd[:], in_=dtd[:], func=AF.Ln, bias=oneb[:]), ['oneb'], ['dtd'])
            Aop(lambda e, d=d: e.activation(out=nA[:], in_=pcol('ssd_alog%d%d' % (i, d), 0, 16), func=AF.Exp), ['par'], ['nA'])
            V(lambda e: e.scalar_tensor_tensor(out=ad[:], in0=dtd[:], scalar=-1.0, in1=nA[:].unsqueeze(1).to_broadcast([P, NCH, 16]), op0=ALU.mult, op1=ALU.mult),
              ['dtd', 'nA'], ['ad'])
            fw.dma('sp', ST[:], ssd_h0[i, d], writes=['ST'])
            V(lambda e: e.tensor_copy(out=STb[:], in_=ST[:]), ['ST'], ['STb'])
            Mm = Mf if d == 0 else Mb
            order = list(range(NCH)) if d == 0 else list(range(NCH - 1, -1, -1))
            for step, c in enumerate(order):
                ts = slice(c * P, (c + 1) * P)
                if step > 0 and step % 2 == 0:
                    V(lambda e: e.tensor_scalar(out=ST[:], in0=ST[:], scalar1=pcol('flag'), scalar2=None, op0=ALU.mult), ['par'], ['ST'])
                    V(lambda e: e.tensor_copy(out=STb[:], in_=ST[:]), ['ST'], ['STb'])
                for hf in range(2):
                    V(lambda e, hf=hf, c=c: e.tensor_tensor(out=rrep[:].rearrange("p (h t) -> p h t", h=8),
                                                            in0=ad[:, c, hf * 8:(hf + 1) * 8].unsqueeze(2).to_broadcast([P, 8, P]),
                                                            in1=ident_f.unsqueeze(1).to_broadcast([P, 8, P]), op=ALU.mult), ['ad', 'cst'], [('tmpf', 1)])
                    for q in range(2):
                        MM(psb[2 + q][:], ones_f, rrep[:, q * 512:(q + 1) * 512], True, True, [('tmpf', 1), 'cst'], [PSK[2 + q]])
                        Aop(lambda e, hf=hf, q=q: e.activation(out=abc[:, hf * 8 + q * 4:hf * 8 + q * 4 + 4, :].rearrange("p h t -> p (h t)"),
                                                               in_=psb[2 + q][:], func=AF.Exp), [], ['abc', PSK[2 + q]])
                MM(psb[4][:, 0:16], Mm, ad[:, c, :], True, True, ['cst', 'ad'], [PSK[4]])
                Aop(lambda e: e.activation(out=sm[:, 3, :], in_=psb[4][:, 0:16], func=AF.Exp), [], ['sm3', PSK[4]])
                MM(psb[4][:, 16:32], ones_f, ad[:, c, :], True, True, ['cst', 'ad'], [PSK[4]])
                Aop(lambda e: e.activation(out=sm[:, 1, :], in_=psb[4][:, 16:32], func=AF.Exp), [], ['sm1', PSK[4]])
                edge = 0 if d == 0 else P - 1
                V(lambda e, edge=edge: e.memset(abc[:, :, edge:edge + 1], 0.0), [], ['abc'])
                a2 = abc[:].rearrange("p h t -> p (h t)")
                d2 = DTm[:].rearrange("p h t -> p (h t)")
                V(lambda e: e.tensor_copy(out=DTm[:], in_=ident_f.unsqueeze(1).to_broadcast([P, 16, P])), ['cst'], ['DTm'])
                if d == 0:
                    V(lambda e: e.tensor_tensor_scan(out=d2, data0=a2, data1=d2, initial=0.0, op0=ALU.mult, op1=ALU.add), ['abc'], ['DTm'])
                else:
                    V(lambda e: e.tensor_tensor_scan(out=d2[:, ::-1], data0=a2[:, ::-1], data1=d2[:, ::-1], initial=0.0, op0=ALU.mult, op1=ALU.add),
                      ['abc'], ['DTm'])
                for g in range(2):
                    MM(psb[5][:, g * P:(g + 1) * P], BCT[:, g, ts], BCT[:, 2 + g, ts], True, True, [('BCT', g), ('BCT', 2 + g)], [PSK[5]])
                for g in range(2):
                    V(lambda e, g=g: e.tensor_tensor(out=WT[:, g * 8:(g + 1) * 8, :], in0=DTm[:, g * 8:(g + 1) * 8, :],
                                                     in1=psb[5][:, g * P:(g + 1) * P].unsqueeze(1).to_broadcast([P, 8, P]), op=ALU.mult),
                      ['DTm'], ['WT', PSK[5]])
                V(lambda e, c=c: e.tensor_tensor(out=xdt[:].rearrange("p (h q) -> p h q", h=16), in0=xtm[:, c, :].rearrange("p (h q) -> p h q", h=16),
                                                 in1=dtd[:, c, :].unsqueeze(2).to_broadcast([P, 16, 64]), op=ALU.mult), [('xtm', c), 'dtd'], [('sqb', 0)])
                for h in range(16):
                    MM(psb[6 + h // 8][:, (h % 8) * 64:(h % 8) * 64 + 64], WT[:, h, :], xdt[:, h * 64:(h + 1) * 64], True, True,
                       ['WT', ('sqb', 0)], [PSK[6 + h // 8]])
                for g in range(2):
                    MM(psb[2 + g][:], BCT[:, 2 + g, ts], STb[:, g * 512:(g + 1) * 512], True, True, [('BCT', 2 + g), 'STb'], [PSK[2 + g]])
                for g in range(2):
                    V(lambda e, g=g: e.tensor_tensor(out=yc[:, g * 512:(g + 1) * 512].rearrange("p (h q) -> p h q", h=8),
                                                     in0=psb[2 + g][:].rearrange("p (h q) -> p h q", h=8),
                                                     in1=sm[:, 3, g * 8:(g + 1) * 8].unsqueeze(2).to_broadcast([P, 8, 64]), op=ALU.mult),
                      ['sm3'], [('tmpf', 2), PSK[2 + g]])
                    V(lambda e, g=g: e.tensor_tensor(out=yc[:, g * 512:(g + 1) * 512], in0=yc[:, g * 512:(g + 1) * 512], in1=psb[6 + g][:], op=ALU.add),
                      [], [('tmpf', 2), PSK[6 + g]])
                ecol = P - 1 if d == 0 else 0
                V(lambda e, ecol=ecol: e.tensor_tensor(out=xdd[:].rearrange("p (h q) -> p h q", h=16), in0=xdt[:].rearrange("p (h q) -> p h q", h=16),
                                                       in1=DTm[:, :, ecol:ecol + 1].to_broadcast([P, 16, 64]), op=ALU.mult), [('sqb', 0), 'DTm'], [('sqb', 1)])
                for g in range(2):
                    MM(psb[4 + g][:], Btm[:, c, g * P:(g + 1) * P], xdd[:, g * 512:(g + 1) * 512], True, True, [('Btm', c), ('sqb', 1)], [PSK[4 + g]])
                V(lambda e: e.tensor_tensor(out=ST[:].rearrange("p (h q) -> p h q", h=16), in0=ST[:].rearrange("p (h q) -> p h q", h=16),
                                            in1=sm[:, 1, :].unsqueeze(2).to_broadcast([P, 16, 64]), op=ALU.mult), ['sm1'], ['ST'])
                for g in range(2):
                    V(lambda e, g=g: e.tensor_tensor(out=ST[:, g * 512:(g + 1) * 512], in0=ST[:, g * 512:(g + 1) * 512], in1=psb[4 + g][:], op=ALU.add),
                      [], ['ST', PSK[4 + g]])
                V(lambda e: e.tensor_copy(out=STb[:], in_=ST[:]), ['ST'], ['STb'])
                if step % 2 == 1:
                    seg = c // 2
                    for q in range(8):
                        MM(psb[2 + q % 2][:, 0:P], ST[:, q * P:(q + 1) * P], ident_f, True, True, ['ST', 'cst'], [PSK[2 + q % 2]])
                        V(lambda e, q=q: e.tensor_copy(out=stg[:, q * P:(q + 1) * P], in_=psb[2 + q % 2][:, 0:P]), [], [('tmpf', 0), PSK[2 + q % 2]])
                    for q in range(8):
                        fw.dma('sp', o_ssd[seg, i, d, q * P:(q + 1) * P, :], stg[:, q * P:(q + 1) * P], reads=[('tmpf', 0)])
                if d == 1:
                    V(lambda e, c=c: e.tensor_copy(out=yf[:, c, :], in_=yc[:]), [('tmpf', 2)], [('yf', c)])
                else:
                    V(lambda e, c=c: e.tensor_tensor(out=yc[:], in0=yc[:], in1=yf[:, c, :], op=ALU.add), [('yf', c)], [('tmpf', 2)])
                    V(lambda e, c=c: e.tensor_tensor(out=xdd[:].rearrange("p (h q) -> p h q", h=16), in0=xtm[:, c, :].rearrange("p (h q) -> p h q", h=16),
                                                     in1=pcol('ssd_D%d' % i, 0, 16).unsqueeze(2).to_broadcast([P, 16, 64]), op=ALU.mult),
                      [('xtm', c), 'par'], [('sqb', 1)])
                    V(lambda e: e.tensor_tensor(out=yc[:], in0=yc[:], in1=xdd[:], op=ALU.add), [('sqb', 1)], [('tmpf', 2)])
                    for zq in range(2):
                        proj_tm(wz[zq], 0, 512, c, 2 + zq)
                        Aop(lambda e, zq=zq: e.activation(out=tmpf[0][:, zq * 512:(zq + 1) * 512], in_=psb[2 + zq][:], func=AF.Silu),
                            [], [('tmpf', 0), PSK[2 + zq]])
                    V(lambda e: e.tensor_tensor(out=yc[:], in0=yc[:], in1=tmpf[0][:], op=ALU.mult), [('tmpf', 0)], [('tmpf', 2)])
                    Aop(lambda e: e.activation(out=tmpf[0][:], in_=yc[:], func=AF.Square), [('tmpf', 2)], [('tmpf', 0)])
                    V(lambda e: e.reduce_sum(out=sm[:, 0, 0:1], in_=tmpf[0][:], axis=AX.X), [('tmpf', 0)], ['sm0'])
                    Aop(lambda e: e.activation(out=sm[:, 0, 1:2], in_=sm[:, 0, 0:1], func=AF.Sqrt, scale=1.0 / 1024, bias=epsb[:]), ['epsb'], ['sm0'])
                    V(lambda e: e.reciprocal(out=sm[:, 0, 2:3], in_=sm[:, 0, 1:2]), [], ['sm0'])
                    V(lambda e: e.tensor_scalar(out=xdd[:], in0=yc[:], scalar1=sm[:, 0, 2:3], scalar2=None, op0=ALU.mult), [('tmpf', 2), 'sm0'], [('sqb', 1)])
                    for q in range(8):
                        pi = 4 + q % 2
                        MM(psb[pi][:, 0:P], xdd[:, q * P:(q + 1) * P], ident_b[:], True, True, [('sqb', 1), 'identb'], [PSK[pi]])
                        V(lambda e, q=q, ts=ts, pi=pi: e.tensor_scalar(out=ymix[:, q, ts], in0=psb[pi][:, 0:P], scalar1=pcol('ssd_ng%d' % i, q),
                                                                       scalar2=None, op0=ALU.mult), ['par'], [('ym', q), PSK[pi]])

    def ffn_pass(w1_, w3_, w2_, FF, g2cols, comb_fn, slots, gT, silb):
        ntiles = FF // P
        sl = [0]
        for g0 in range(0, ntiles, 4):
            J = min(4, ntiles - g0)
            nco = J * P
            k = rstate['k']
            rstate['k'] += 3
            s1, s3, s2 = slots[k % 4], slots[(k + 1) % 4], slots[(k + 2) % 4]
            for kt in range(DT):
                fw.dma('pool', s1[:, kt, 0:nco], w1_[kt * P:(kt + 1) * P, g0 * P:g0 * P + nco], writes=[('rg', id(s1), kt)])
            for kt in range(DT):
                fw.dma('pool', s3[:, kt, 0:nco], w3_[kt * P:(kt + 1) * P, g0 * P:g0 * P + nco], writes=[('rg', id(s3), kt)])
            w2v = s2[:].rearrange("p a b -> p (a b)").rearrange("p (j d) -> p j d", j=4)
            for j in range(J):
                for q in range(4):
                    fw.dma('pool', w2v[:, j, q * 512:(q + 1) * 512], w2_[(g0 + j) * P:(g0 + j + 1) * P, q * 512:(q + 1) * 512],
                           writes=[('rg', id(s2), 4 * j + q)])
            for j in range(J):
                for tc in range(2):
                    a_, b_ = 2 + 2 * (sl[0] % 2), 3 + 2 * (sl[0] % 2)
                    sb_ = sl[0] % 2
                    sl[0] += 1
                    for kt in range(DT):
                        MM(psb[a_][:], s1[:, kt, j * P:(j + 1) * P], hT[:, kt, tc * 512:(tc + 1) * 512], kt == 0, kt == DT - 1,
                           [('rg', id(s1), kt), ('h', kt)], [PSK[a_]])
                    for kt in range(DT):
                        MM(psb[b_][:], s3[:, kt, j * P:(j + 1) * P], hT[:, kt, tc * 512:(tc + 1) * 512], kt == 0, kt == DT - 1,
                           [('rg', id(s3), kt), ('h', kt)], [PSK[b_]])
                    Aop(lambda e, a_=a_, sb_=sb_: e.activation(out=silb[sb_][:], in_=psb[a_][:], func=AF.Silu), [], [('silb', sb_), PSK[a_]])
                    if comb_fn is None:
                        V(lambda e, b_=b_, sb_=sb_, j=j, tc=tc: e.tensor_tensor(out=gT[:, j, tc * 512:(tc + 1) * 512], in0=silb[sb_][:], in1=psb[b_][:], op=ALU.mult),
                          [('silb', sb_)], [('gT', j, tc), PSK[b_]])
                    else:
                        V(lambda e, b_=b_, sb_=sb_: e.tensor_tensor(out=silb[sb_][:], in0=silb[sb_][:], in1=psb[b_][:], op=ALU.mult),
                          [], [('silb', sb_), PSK[b_]])
                        V(lambda e, sb_=sb_, j=j, tc=tc: e.tensor_tensor(out=gT[:, j, tc * 512:(tc + 1) * 512], in0=silb[sb_][:], in1=comb_fn(tc), op=ALU.mult),
                          [('silb', sb_), 'rstd'], [('gT', j, tc)])
            for dt in range(DT):
                for tc in range(2):
                    o_ = 6 + (sl[0] % 2)
                    sl[0] += 1
                    for j in range(J):
                        MM(psb[o_][:], w2v[:, j, dt * P:(dt + 1) * P], gT[:, j, tc * 512:(tc + 1) * 512], j == 0, j == J - 1,
                           [('rg', id(s2), 4 * j + dt // 4), ('gT', j, tc)], [PSK[o_]])
                    V(lambda e, dt=dt, tc=tc, o_=o_: e.scalar_tensor_tensor(
                        out=xT[:, dt, tc * 512:(tc + 1) * 512], in0=psb[o_][:], scalar=g2cols[dt],
                        in1=xT[:, dt, tc * 512:(tc + 1) * 512], op0=ALU.mult, op1=ALU.add), ['coefs'], [('x', dt), PSK[o_]])

    def moe_layer(i, g2cols, slots, gTb, silb):
        comb = rstd
        cbT = tmpf[1]
        sel = tmpf[2]
        for dt in range(DT):
            bb = 1 + dt % 2
            V(lambda e, dt=dt, bb=bb: e.tensor_tensor(out=tmpf[bb][:], in0=xT[:, dt, :], in1=rstd[:], op=ALU.mult), [('x', dt), 'rstd'], [('tmpf', bb)])
            V(lambda e, dt=dt, bb=bb: e.tensor_scalar(out=tmpf[bb][:], in0=tmpf[bb][:], scalar1=coef[i][:, 3, dt:dt + 1], scalar2=coef[i][:, 4, dt:dt + 1],
                                                      op0=ALU.mult, op1=ALU.add), ['coefs'], [('tmpf', bb)])
            for tt in range(NCH):
                MM(psb[tt][:, 0:NEXP], tmpf[bb][:, tt * P:(tt + 1) * P], rt[:, dt, :], dt == 0, dt == DT - 1,
                   [('tmpf', bb), 'rt'], [PSK[tt]], inc=True)
        for tt in range(NCH):
            V(lambda e, tt=tt: e.tensor_copy(out=lg[:, tt, :], in_=psb[tt][:, 0:NEXP]), [], ['lg', PSK[tt]])
        for tt in range(NCH):
            V(lambda e, tt=tt: e.max(out=mx[:, tt, :], in_=lg[:, tt, :]), ['lg'], ['mx'])
        V(lambda e: e.tensor_tensor(out=gt[:, :, 0:1], in0=mx[:, :, 1:2], in1=mx[:, :, 0:1], op=ALU.subtract), ['mx'], ['gt'])
        Aop(lambda e: e.activation(out=gt[:, :, 1:2], in_=gt[:, :, 0:1], func=AF.Exp), [], ['gt'])
        V(lambda e: e.tensor_scalar(out=gt[:, :, 1:2], in0=gt[:, :, 1:2], scalar1=1.0, scalar2=None, op0=ALU.add), [], ['gt'])
        V(lambda e: e.reciprocal(out=gt[:, :, 2:3], in_=gt[:, :, 1:2]), [], ['gt'])
        V(lambda e: e.tensor_scalar(out=gt[:, :, 3:4], in0=gt[:, :, 2:3], scalar1=-1.0, scalar2=1.0, op0=ALU.mult, op1=ALU.add), [], ['gt'])
        V(lambda e: e.tensor_tensor(out=cb[:], in0=lg[:], in1=mx[:, :, 0:1].to_broadcast([P, NCH, NEXP]), op=ALU.is_equal), ['lg', 'mx'], ['cb'])
        V(lambda e: e.tensor_tensor(out=cb[:], in0=cb[:], in1=gt[:, :, 2:3].to_broadcast([P, NCH, NEXP]), op=ALU.mult), ['gt'], ['cb'])
        V(lambda e: e.tensor_tensor(out=lg[:], in0=lg[:], in1=mx[:, :, 1:2].to_broadcast([P, NCH, NEXP]), op=ALU.is_equal), ['mx'], ['lg'])
        V(lambda e: e.tensor_tensor(out=lg[:], in0=lg[:], in1=gt[:, :, 3:4].to_broadcast([P, NCH, NEXP]), op=ALU.mult), ['gt'], ['lg'])
        V(lambda e: e.tensor_tensor(out=cb[:], in0=cb[:], in1=lg[:], op=ALU.add), ['lg'], ['cb'])
        if os.environ.get('MOEDBG'):
            for nm_, t_, k_ in [('dbg_cb', cb, 'cb'), ('dbg_mx', mx, 'mx')]:
                fw.dma('sp', dout(nm_, [P, NCH, 8]), t_[:], reads=[k_])
        for hf in range(2):
            for q in range(4):
                MM(psb[1][0:NEXP, q * P:(q + 1) * P], cb[:, hf * 4 + q, :], ident_f, True, True, ['cb', 'cst'], [PSK[1]])
            V(lambda e, hf=hf: e.tensor_copy(out=cbT[0:NEXP, hf * 512:(hf + 1) * 512], in_=psb[1][0:NEXP, :]), [], [('tmpf', 1), PSK[1]])
        V(lambda e: e.tensor_copy(out=sel[0:NEXP, :].rearrange("p (a b) -> p a b", a=NEXP),
                                  in_=cst[0:NEXP, 0, 0:NEXP].unsqueeze(2).to_broadcast([NEXP, NEXP, P])), ['cst'], [('tmpf', 2)])
        for ex in range(NEXP):
            for tc in range(2):
                MM(psb[tc][:], sel[0:NEXP, ex * P:(ex + 1) * P], cbT[0:NEXP, tc * 512:(tc + 1) * 512], True, True, [('tmpf', 2), ('tmpf', 1)], [PSK[tc]])
                V(lambda e, tc=tc: e.tensor_copy(out=comb[:, tc * 512:(tc + 1) * 512], in_=psb[tc][:]), [], ['rstd', PSK[tc]])
            ffn_pass(mw1[ex], mw3[ex], mw2[ex], FF_EXP, g2cols, lambda tc: comb[:, tc * 512:(tc + 1) * 512], slots, gTb, silb)

    def stop_here():
        fw.barrier()
        for kt in range(DT):
            fw.dma('sp', dbg_ym[:, kt, :], ymix[:, kt, :], reads=[('ym', kt)])
        for dt in range(DT):
            fw.dma('sp', yT_out[dt * P:(dt + 1) * P, :], xT[:, dt, :], reads=[('x', dt)])
        fw.finish()
        return nc

    for i in range(DEPTH):
        norm_to_hT(i, 0)
        for dt in range(DT):
            fw.dma('sp', xspill[dt * P:(dt + 1) * P, :], xT[:, dt, :], reads=[('x', dt)])
        fw.barrier()
        for nm, fn in [('lru', lru_mixer), ('gla', gla_mixer), ('ssd', ssd_mixer)]:
            if STOP_AFTER and STOP_AFTER[1] == i and STOP_AFTER[0] in ('lru', 'gla', 'ssd') and STOP_AFTER[0] != nm and STOP_AFTER[2:] == ('only',):
                continue
            fn(i)
            fw.barrier()
            if STOP_AFTER == (nm, i) or STOP_AFTER == (nm, i, 'only'):
                for dt in range(DT):
                    fw.dma('sp', xT[:, dt, :], xspill[dt * P:(dt + 1) * P, :], writes=[('x', dt)])
                return stop_here()
        for dt in range(DT):
            fw.dma('sp', xT[:, dt, :], xspill[dt * P:(dt + 1) * P, :], writes=[('x', dt)])
        for dt4 in range(4):
            w = load_cols(out_w[i], dt4 * 512, 512)
            for dq in range(4):
                dt = dt4 * 4 + dq
                for tc in range(2):
                    o_ = 6 + tc
                    for kt in range(DT):
                        MM(psb[o_][:], w[:, kt, dq * P:(dq + 1) * P], ymix[:, kt, tc * 512:(tc + 1) * 512], kt == 0, kt == DT - 1,
                           [('rg', id(w), kt), ('ym', kt)], [PSK[o_]])
                    V(lambda e, dt=dt, tc=tc, o_=o_: e.scalar_tensor_tensor(
                        out=xT[:, dt, tc * 512:(tc + 1) * 512], in0=psb[o_][:], scalar=coef[i][:, 2, dt:dt + 1],
                        in1=xT[:, dt, tc * 512:(tc + 1) * 512], op0=ALU.mult, op1=ALU.add), ['coefs'], [('x', dt), PSK[o_]])
        if STOP_AFTER == ('mix', i):
            return stop_here()
        norm_to_hT(i, 1)
        fw.barrier()
        A_ffn.reset()
        slots = ring + [A_ffn.alloc([P, DT, 512], BF16, "ringB%d" % q) for q in range(2)]
        gTb = A_ffn.alloc([P, 4, T], BF16, "gT")
        silb = [A_ffn.alloc([P, 512], BF16, "silb%d" % q) for q in range(2)]
        g2cols = [coef[i][:, 5, q:q + 1] for q in range(DT)]
        if i % 2 == 0:
            ffn_pass(w1, w3, w2, FF_DENSE, g2cols, None, slots, gTb, silb)
        else:
            moe_layer(i, g2cols, slots, gTb, silb)
        fw.barrier()
        if STOP_AFTER == ('ffn', i):
            return stop_here()

    rms_stats(lambda j: xT[:, j, :], DT, lambda j: ('x', j), D)
    for dt in range(DT):
        bb = 1 + dt % 2
        V(lambda e, dt=dt, bb=bb: e.tensor_tensor(out=tmpf[bb][:], in0=xT[:, dt, :], in1=rstd[:], op=ALU.mult), [('x', dt), 'rstd'], [('tmpf', bb)])
        V(lambda e, dt=dt, bb=bb: e.tensor_scalar(out=tmpf[bb][:], in0=tmpf[bb][:], scalar1=pcol('final_g', dt), scalar2=None, op0=ALU.mult),
          ['par'], [('tmpf', bb)])
        fw.dma('sp', yT_out[dt * P:(dt + 1) * P, :], tmpf[bb][:], reads=[('tmpf', bb)])
    fw.finish()
    return nc


def _host_params(core, inp):
    par = np.zeros((P, NPAR), np.float32)

    def put(name, vec):
        o, w = PAR_OFF[name]
        par[:, o:o + w] = np.asarray(vec, np.float32).reshape(w, P).T

    def rep(name, vec):
        o, w = PAR_OFF[name]
        par[:, o:o + w] = np.asarray(vec, np.float32).reshape(1, w)
    sample = core < 4
    put('cvec', inp['c'][core] if sample else inp['c_ctx'])
    put('final_g', inp['final_norm_g'])
    rep('flag', [1.0 if sample else 0.0])
    for i in range(DEPTH):
        put('modb%d' % i, inp['mod_b'][i])
        put('n1g%d' % i, inp['norm1_g'][i])
        put('n2g%d' % i, inp['norm2_g'][i])
        o, w = PAR_OFF['lru_cw%d' % i]
        par[:, o:o + w] = inp['lru_conv_w'][i].reshape(4, 4, P).transpose(2, 1, 0).reshape(P, 16)
        put('lru_cb%d' % i, inp['lru_conv_b'][i])
        for d in range(2):
            put('lru_ba%d%d' % (i, d), inp['lru_ba'][i, d])
            put('lru_bx%d%d' % (i, d), inp['lru_bx'][i, d])
            put('lru_lam%d%d' % (i, d), inp['lru_lambda'][i, d])
            put('lru_h0%d%d' % (i, d), inp['state_lru'][core, i, d] if sample else np.zeros(512, np.float32))
        o, w = PAR_OFF['ssd_cw%d' % i]
        par[:, o:o + w] = inp['ssd_conv_w'][i].reshape(4, 12, P).transpose(2, 1, 0).reshape(P, 48)
        put('ssd_cb%d' % i, inp['ssd_conv_b'][i])
        put('ssd_ng%d' % i, inp['ssd_norm_g'][i])
        rep('ssd_D%d' % i, inp['ssd_D'][i])
        for d in range(2):
            rep('ssd_dtb%d%d' % (i, d), inp['ssd_dt_bias'][i, d])
            rep('ssd_alog%d%d' % (i, d), inp['ssd_A_log'][i, d])
        put('gla_ng%d' % i, inp['gla_norm_g'][i])
    return par


def _host_consts(core):
    import ml_dtypes
    s = np.arange(P)
    cst = np.zeros((P, 4, P), np.float32)
    cst[:, 0] = np.eye(P)
    cst[:, 1] = (s[:, None] <= s[None, :])
    cst[:, 2] = (s[:, None] >= s[None, :])
    cst[:, 3] = 1.0
    L = 64 if core < 4 else 256
    t = np.arange(T)
    m = np.stack([(t % L != 0), (t % L != L - 1), (t % L < L - 2)], 0).astype(np.float32)
    msk = np.broadcast_to(m[None], (P, 3, T)).astype(ml_dtypes.bfloat16)
    return cst, np.ascontiguousarray(msk)


def make_in_maps(inp, cores=range(8)):
    inp = {k: np.asarray(v) for k, v in inp.items()}
    gw = np.zeros((DEPTH, 2, 32, 256), np.float32)
    gw[:, :, 0:16] = inp['gla_gate_w']
    gw[:, :, 16] = inp['gla_gate_b']
    lw = np.zeros((DEPTH, 2, 2, 4, P, P), np.float32)
    for ax, nm in enumerate(['lru_wa', 'lru_wx']):
        wv = inp[nm]
        for j in range(4):
            lw[:, :, ax, j, 0:64, 0:64] = wv[:, :, 2 * j]
            lw[:, :, ax, j, 64:128, 64:128] = wv[:, :, 2 * j + 1]
    maps = []
    for core in cores:
        sample = core < 4
        x = inp['x_sample'][core] if sample else inp['x_prompt'][4 * (core - 4):4 * (core - 3)].reshape(T, D)
        cst, msk = _host_consts(core)
        if sample:
            sh = inp['state_ssd'][core]
            ssd_h0 = np.ascontiguousarray(sh.reshape(DEPTH, 2, 1024, P).transpose(0, 1, 3, 2))
            gh = inp['state_gla'][core]
            gla_h0 = np.ascontiguousarray(gh.reshape(DEPTH, 2, 2, 2, 64, P).transpose(0, 1, 3, 4, 2, 5).reshape(DEPTH, 2, P, 256))
        else:
            ssd_h0 = np.zeros((DEPTH, 2, P, 1024), np.float32)
            gla_h0 = np.zeros((DEPTH, 2, P, 256), np.float32)
        maps.append({
            "xT": np.ascontiguousarray(x.T), "par": _host_params(core, inp), "cst": cst, "msk": msk,
            "mod_w": inp['mod_w'], "in_w": inp['in_w'], "out_w": inp['out_w'],
            "ffn_w1": inp['ffn_w1'][0], "ffn_w3": inp['ffn_w3'][0], "ffn_w2": inp['ffn_w2'][0],
            "router": inp['moe_router'][0], "moe_w1": inp['moe_w1'][0], "moe_w3": inp['moe_w3'][0], "moe_w2": inp['moe_w2'][0],
            "gla_gw": gw, "lru_w": lw, "ssd_h0": ssd_h0, "gla_h0": gla_h0,
        })
    return maps


def assemble(results):
    ys = [np.ascontiguousarray(r["yT"].T) for r in results]
    y_sample = np.stack(ys[:4], 0)
    y_prompt = np.concatenate(ys[4:], 0).reshape(16, 256, D)
    pr = results[4:]
    new_ssd = np.concatenate([r["o_ssd"].reshape(4, DEPTH, 2, 16, 64, P) for r in pr], 0)
    new_gla = np.concatenate([r["o_gla"].reshape(4, DEPTH, 2, 2, 64, 2, P).transpose(0, 1, 2, 5, 3, 4, 6).reshape(4, DEPTH, 2, 4, 64, P)
                              for r in pr], 0)
    new_lru = np.concatenate([r["o_lru"].reshape(DEPTH, P, 4, 4, 2).transpose(3, 0, 4, 2, 1).reshape(4, DEPTH, 2, 512) for r in pr], 0)
    return (y_prompt, y_sample, np.ascontiguousarray(new_ssd), np.ascontiguousarray(new_gla), np.ascontiguousarray(new_lru))


def kernel(**inputs):
    nc = build_program()
    in_maps = make_in_maps(inputs)
    res = run_bass_kernel_spmd(nc, in_maps, core_ids=list(range(8)))
    return assemble(res.results)
```
